# Optimizing a Trainium2 kernel written in Bass

```python
import math
import jax, jax.numpy as jnp
from jax import lax
import numpy as np

D_MODEL = 1024
BATCH = 4
SEQ = 4096
DEPTH = 2

GRID_W = 64
CTX_LEN = 256
Q_BLOCK = 128
EPS = 1e-6
ROPE_THETA = 10000.0

A_HEAD_DIM = 64
A_HEADS = D_MODEL // 128
A_KV_HEADS = A_HEADS // 4
A_GROUP = A_HEADS // A_KV_HEADS
A_Q = A_HEADS * A_HEAD_DIM
A_KV = A_KV_HEADS * A_HEAD_DIM
A_OUT = A_Q

B_CHANNELS = D_MODEL // 2
B_CONV_W = 31
B_IN = 2 * B_CHANNELS

C_HEADS = D_MODEL // 256
C_HEAD_DIM = 64
C_V_DIM = 2 * C_HEAD_DIM
C_QK = C_HEADS * 2 * C_HEAD_DIM
C_V = C_HEADS * C_V_DIM

N_BRANCH = 3
IN_SPLITS = [A_Q, A_KV, A_KV, B_IN, C_QK, C_QK, C_V, N_BRANCH * D_MODEL]
IN_WIDTH = sum(IN_SPLITS)
IN_OFFSETS = [int(v) for v in np.cumsum(IN_SPLITS)[:-1]]

D_FF = 256 * math.ceil(8 * D_MODEL / 3 / 256)
N_EXPERTS = 8
TOP_K = 2
D_FF_EXPERT = 7 * D_MODEL // 2
N_DENSE = (DEPTH + 1) // 2
N_MOE = DEPTH // 2

kernel_name = "hybrid_gqa_conformer_diffattn_moe_dit"


def rms_norm(x, g):
    xf = x.astype(jnp.float32)
    y = xf * lax.rsqrt(jnp.mean(xf * xf, axis=-1, keepdims=True) + EPS)
    return (y * g.astype(jnp.float32)).astype(x.dtype)


def layer_norm(x, g, b):
    xf = x.astype(jnp.float32)
    mu = jnp.mean(xf, axis=-1, keepdims=True)
    var = jnp.mean(jnp.square(xf - mu), axis=-1, keepdims=True)
    y = (xf - mu) * lax.rsqrt(var + EPS)
    return (y * g.astype(jnp.float32) + b.astype(jnp.float32)).astype(x.dtype)


def axial_rope_tables(seq, head_dim):
    rows = seq // GRID_W
    row = jnp.repeat(jnp.arange(rows), GRID_W)
    col = jnp.tile(jnp.arange(GRID_W), rows)
    n_freq = head_dim // 4
    inv = ROPE_THETA ** (-jnp.arange(n_freq, dtype=jnp.float32) / n_freq)
    pos = jnp.stack([row, col], axis=-1).astype(jnp.float32)
    ang = pos[:, :, None] * inv
    return jnp.cos(ang), jnp.sin(ang)


def apply_axial_rope(x, cos, sin):
    shp = x.shape
    xr = x.astype(jnp.float32).reshape(shp[:-1] + (2, 2, shp[-1] // 4))
    extra = x.ndim - 3
    cos = cos.reshape((cos.shape[0],) + (1,) * extra + cos.shape[1:])
    sin = sin.reshape((sin.shape[0],) + (1,) * extra + sin.shape[1:])
    x1, x2 = xr[..., 0, :], xr[..., 1, :]
    out = jnp.stack([x1 * cos - x2 * sin, x2 * cos + x1 * sin], axis=-2)
    return out.reshape(shp).astype(x.dtype)


def map_query_blocks(fn, q):
    b, s = q.shape[0], q.shape[1]
    qb = jnp.moveaxis(q.reshape((b, s // Q_BLOCK, Q_BLOCK) + q.shape[2:]), 1, 0)
    out = lax.map(fn, qb)
    out = jnp.moveaxis(out, 0, 1)
    return out.reshape((b, s) + out.shape[3:])


def gqa_attend(q, k, v):
    s = jnp.einsum('bqkgd,bskd->bkgqs', q, k).astype(jnp.float32) * (A_HEAD_DIM ** -0.5)
    p = jax.nn.softmax(s, axis=-1).astype(v.dtype)
    return jnp.einsum('bkgqs,bskd->bqkgd', p, v)


def diff_attend(q, k, v, lam):
    s = jnp.einsum('bqhmd,bshmd->bhmqs', q, k).astype(jnp.float32) * (C_HEAD_DIM ** -0.5)
    p = jax.nn.softmax(s, axis=-1)
    a = (p[:, :, 0] - lam * p[:, :, 1]).astype(v.dtype)
    return jnp.einsum('bhqs,bshd->bqhd', a, v)


def project_in(u, qn_g, kn_g):
    aq, ak, av, bz, cq, ck, cv, gt = jnp.split(u, IN_OFFSETS, axis=-1)
    b, t = u.shape[0], u.shape[1]
    aq = rms_norm(aq.reshape(b, t, A_KV_HEADS, A_GROUP, A_HEAD_DIM), qn_g)
    ak = rms_norm(ak.reshape(b, t, A_KV_HEADS, A_HEAD_DIM), kn_g)
    av = av.reshape(b, t, A_KV_HEADS, A_HEAD_DIM)
    cq = cq.reshape(b, t, C_HEADS, 2, C_HEAD_DIM)
    ck = ck.reshape(b, t, C_HEADS, 2, C_HEAD_DIM)
    cv = cv.reshape(b, t, C_HEADS, C_V_DIM)
    return aq, ak, av, bz, cq, ck, cv, gt


def conformer_conv(z, dw_w, dw_b, ln_g, ln_b):
    a, g = jnp.split(z, 2, axis=-1)
    y = a * jax.nn.sigmoid(g)
    pad = B_CONV_W // 2
    y = lax.conv_general_dilated(y, dw_w[:, None, :], window_strides=(1,), padding=[(pad, pad)],
                                 dimension_numbers=('NWC', 'WIO', 'NWC'),
                                 feature_group_count=B_CHANNELS) + dw_b
    return jax.nn.silu(layer_norm(y, ln_g, ln_b))


def mixer_merge(o_a, o_c, bz, gt, lam_init, subln_g, dw_w, dw_b, ln_g, ln_b, b_gate, w_pa, w_pb, w_pc, w_out):
    b, t = o_a.shape[0], o_a.shape[1]
    o_a = o_a.reshape(b, t, A_OUT)
    o_c = (rms_norm(o_c, subln_g) * (1.0 - lam_init)).reshape(b, t, C_V)
    o_b = conformer_conv(bz, dw_w, dw_b, ln_g, ln_b)
    g_a, g_b, g_c = jnp.split(jax.nn.sigmoid(gt + b_gate), N_BRANCH, axis=-1)
    m = g_a * (o_a @ w_pa) + g_b * (o_b @ w_pb) + g_c * (o_c @ w_pc)
    return m @ w_out


def swiglu(h, w1, w3, w2):
    return (jax.nn.silu(h @ w1) * (h @ w3)) @ w2


def moe_swiglu(h, w_r, b_r, w1, w3, w2):
    logits = (h @ w_r).astype(jnp.float32) + b_r.astype(jnp.float32)
    top_v, top_i = lax.top_k(logits, TOP_K)
    top_w = jax.nn.softmax(top_v, axis=-1)
    gates = jnp.sum(jax.nn.one_hot(top_i, N_EXPERTS, dtype=jnp.float32) * top_w[..., None], axis=-2).astype(h.dtype)
    out = jnp.zeros_like(h)
    for e in range(N_EXPERTS):
        out = out + gates[..., e:e + 1] * swiglu(h, w1[e], w3[e], w2[e])
    return out


def setup_inputs(seed: int = 0) -> dict:
    key = jax.random.key(seed)
    ks = iter(jax.random.split(key, 48))

    def nrm(shape, scale):
        return jax.random.normal(next(ks), shape, jnp.float32) * scale

    d = D_MODEL
    return {
        "x": nrm((BATCH, SEQ, d), 1.0),
        "c": nrm((BATCH, d), 1.0),
        "ctx": nrm((BATCH, CTX_LEN, d), 1.0),
        "c_ctx": nrm((d,), 1.0),
        "w_mod": nrm((DEPTH, d, 6 * d), 0.5 * d ** -0.5),
        "b_mod": nrm((DEPTH, 6 * d), 0.02),
        "norm1_g": 1.0 + nrm((DEPTH, d), 0.02),
        "norm2_g": 1.0 + nrm((DEPTH, d), 0.02),
        "w_in": nrm((DEPTH, d, IN_WIDTH), d ** -0.5),
        "b_gate": nrm((DEPTH, N_BRANCH * d), 0.02),
        "a_qn_g": 1.0 + nrm((DEPTH, A_HEAD_DIM), 0.02),
        "a_kn_g": 1.0 + nrm((DEPTH, A_HEAD_DIM), 0.02),
        "b_dw_w": nrm((DEPTH, B_CONV_W, B_CHANNELS), B_CONV_W ** -0.5),
        "b_dw_b": nrm((DEPTH, B_CHANNELS), 0.02),
        "b_ln_g": 1.0 + nrm((DEPTH, B_CHANNELS), 0.02),
        "b_ln_b": nrm((DEPTH, B_CHANNELS), 0.02),
        "c_lq1": nrm((DEPTH, C_HEAD_DIM), 0.1),
        "c_lk1": nrm((DEPTH, C_HEAD_DIM), 0.1),
        "c_lq2": nrm((DEPTH, C_HEAD_DIM), 0.1),
        "c_lk2": nrm((DEPTH, C_HEAD_DIM), 0.1),
        "c_subln_g": 1.0 + nrm((DEPTH, C_V_DIM), 0.02),
        "w_pa": nrm((DEPTH, A_OUT, d), A_OUT ** -0.5),
        "w_pb": nrm((DEPTH, B_CHANNELS, d), B_CHANNELS ** -0.5),
        "w_pc": nrm((DEPTH, C_V, d), C_V ** -0.5),
        "w_out": nrm((DEPTH, d, d), d ** -0.5),
        "ffn_w1": nrm((N_DENSE, d, D_FF), d ** -0.5),
        "ffn_w3": nrm((N_DENSE, d, D_FF), d ** -0.5),
        "ffn_w2": nrm((N_DENSE, D_FF, d), D_FF ** -0.5),
        "moe_router": nrm((N_MOE, d, N_EXPERTS), d ** -0.5),
        "moe_router_b": nrm((N_MOE, N_EXPERTS), 0.01),
        "moe_w1": nrm((N_MOE, N_EXPERTS, d, D_FF_EXPERT), d ** -0.5),
        "moe_w3": nrm((N_MOE, N_EXPERTS, d, D_FF_EXPERT), d ** -0.5),
        "moe_w2": nrm((N_MOE, N_EXPERTS, D_FF_EXPERT, d), D_FF_EXPERT ** -0.5),
        "final_g": 1.0 + nrm((d,), 0.02),
    }


def reference(x, c, ctx, c_ctx, w_mod, b_mod, norm1_g, norm2_g, w_in, b_gate,
              a_qn_g, a_kn_g, b_dw_w, b_dw_b, b_ln_g, b_ln_b,
              c_lq1, c_lk1, c_lq2, c_lk2, c_subln_g,
              w_pa, w_pb, w_pc, w_out,
              ffn_w1, ffn_w3, ffn_w2,
              moe_router, moe_router_b, moe_w1, moe_w3, moe_w2, final_g):
    seq = x.shape[1]
    cos, sin = axial_rope_tables(seq, A_HEAD_DIM)
    xc = ctx
    for i in range(DEPTH):
        last = i == DEPTH - 1
        mod = jnp.split((jax.nn.silu(c) @ w_mod[i] + b_mod[i])[:, None, :], 6, axis=-1)
        modc = jnp.split((jax.nn.silu(c_ctx) @ w_mod[i] + b_mod[i])[None, None, :], 6, axis=-1)

        h = rms_norm(x, norm1_g[i]) * (1.0 + mod[1]) + mod[0]
        hc = rms_norm(xc, norm1_g[i]) * (1.0 + modc[1]) + modc[0]
        aq, ak, av, bz, cq, ck, cv, gt = project_in(h @ w_in[i], a_qn_g[i], a_kn_g[i])
        aqc, akc, avc, bzc, cqc, ckc, cvc, gtc = project_in(hc @ w_in[i], a_qn_g[i], a_kn_g[i])
        aq = apply_axial_rope(aq, cos, sin)
        ak = apply_axial_rope(ak, cos, sin)
        cq = apply_axial_rope(cq, cos, sin)
        ck = apply_axial_rope(ck, cos, sin)

        lam_init = 0.8 - 0.6 * math.exp(-0.3 * i)
        lam = (jnp.exp(jnp.sum(c_lq1[i].astype(jnp.float32) * c_lk1[i].astype(jnp.float32)))
               - jnp.exp(jnp.sum(c_lq2[i].astype(jnp.float32) * c_lk2[i].astype(jnp.float32)))
               + lam_init)

        k_a = jnp.concatenate([ak, akc], axis=1)
        v_a = jnp.concatenate([av, avc], axis=1)
        o_a = map_query_blocks(lambda qb: gqa_attend(qb, k_a, v_a), aq)
        k_c = jnp.concatenate([ck, ckc], axis=1)
        v_c = jnp.concatenate([cv, cvc], axis=1)
        o_c = map_query_blocks(lambda qb: diff_attend(qb, k_c, v_c, lam), cq)
        y = mixer_merge(o_a, o_c, bz, gt, lam_init, c_subln_g[i], b_dw_w[i], b_dw_b[i], b_ln_g[i], b_ln_b[i],
                        b_gate[i], w_pa[i], w_pb[i], w_pc[i], w_out[i])
        x = x + mod[2] * y

        if not last:
            o_ac = gqa_attend(aqc, akc, avc)
            o_cc = diff_attend(cqc, ckc, cvc, lam)
            yc = mixer_merge(o_ac, o_cc, bzc, gtc, lam_init, c_subln_g[i], b_dw_w[i], b_dw_b[i], b_ln_g[i],
                             b_ln_b[i], b_gate[i], w_pa[i], w_pb[i], w_pc[i], w_out[i])
            xc = xc + modc[2] * yc

        j = i // 2
        h2 = rms_norm(x, norm2_g[i]) * (1.0 + mod[4]) + mod[3]
        if i % 2 == 0:
            x = x + mod[5] * swiglu(h2, ffn_w1[j], ffn_w3[j], ffn_w2[j])
        else:
            x = x + mod[5] * moe_swiglu(h2, moe_router[j], moe_router_b[j], moe_w1[j], moe_w3[j], moe_w2[j])
        if not last:
            h2c = rms_norm(xc, norm2_g[i]) * (1.0 + modc[4]) + modc[3]
            if i % 2 == 0:
                xc = xc + modc[5] * swiglu(h2c, ffn_w1[j], ffn_w3[j], ffn_w2[j])
            else:
                xc = xc + modc[5] * moe_swiglu(h2c, moe_router[j], moe_router_b[j], moe_w1[j], moe_w3[j],
                                               moe_w2[j])
    return rms_norm(x, final_g)
```

```python
import math
import numpy as np
from contextlib import ExitStack
import concourse.bass as bass
import concourse.mybir as mybir
from concourse.bass_utils import run_bass_kernel_spmd

F32 = mybir.dt.float32
BF16 = mybir.dt.bfloat16
AF = mybir.ActivationFunctionType
ALU = mybir.AluOpType
AX = mybir.AxisListType

D = 1024
SEQ = 4096
HALF = 2048
CTX = 256
NTOK = SEQ + CTX
DEPTH = 2
EPS = 1e-6
QB = 256
EXT = QB + 30
NKB = NTOK // 128
D_FF = 2816
N_EXP = 8
D_FFE = 3584
OFF_AQ, OFF_AK, OFF_AV, OFF_BZ, OFF_CQ, OFF_CK, OFF_CV, OFF_GT = 0, 512, 640, 768, 1792, 2304, 2816, 3328
FUSED = True
DEBUG = False
ARENA = 99500

VL = dict(b_mod=(0, 48), n1g=(48, 8), n2g=(56, 8), b_gate=(64, 24), qng=(88, 2), kng=(90, 2), dww=(92, 124),
          dwb=(216, 4), lng=(220, 4), lnb=(224, 4), subg=(228, 1), lamv=(229, 256))
VLN = 485
VG = dict(hmask=(2 * VLN, 5), fing=(2 * VLN + 5, 8), cfm=(2 * VLN + 13, 16), rb=(2 * VLN + 29, 8))
NV = 2 * VLN + 37
C_ONES, C_BD, C_SEL, C_ID, C_OH = 0, 128, 256, 320, 448
NCONST = 448 + 1024


class Buf:
    __slots__ = ("name", "w", "r")

    def __init__(self, name):
        self.name = name
        self.w = None
        self.r = {}


class Ev:
    __slots__ = ("sem", "val", "clk")

    def __init__(self, sem, val, clk):
        self.sem = sem
        self.val = val
        self.clk = clk


class K:
    ENGS = ("pe", "act", "dve", "pool", "sp")

    def __init__(self, nc, stack, n_dma_sems=32):
        self.nc = nc
        self.stack = stack
        self.e = dict(pe=nc.tensor, act=nc.scalar, dve=nc.vector, pool=nc.gpsimd, sp=nc.sync)
        self.sem, self.cnt, self.clk = {}, {}, {}
        for k in self.ENGS:
            self.sem[k] = stack.enter_context(nc.semaphore("s_" + k))
            self.cnt[k] = 0
            self.clk[k] = {}
        self.dsem = [stack.enter_context(nc.semaphore("d%d" % i)) for i in range(n_dma_sems)]
        self.dcnt = [0] * n_dma_sems
        self.dlast = [None] * n_dma_sems
        self.dnext = 0
        self.nwait = 0
        self.nins = 0
        self.dry = False

    def sb(self, name, shape, dt):
        return self.stack.enter_context(self.nc.sbuf_tensor(name, list(shape), dt))

    def ps(self, name, shape, dt=F32):
        return self.stack.enter_context(self.nc.psum_tensor(name, list(shape), dt))

    def _need(self, eng, ev):
        if ev is None:
            return
        c = self.clk[eng]
        if c.get(ev.sem, 0) >= ev.val:
            return
        if eng == "pe" and ev.sem is self.sem["pe"]:
            return
        self.e[eng].wait_ge(ev.sem, ev.val)
        self.nwait += 1
        for s, v in ev.clk.items():
            if c.get(s, 0) < v:
                c[s] = v
        if c.get(ev.sem, 0) < ev.val:
            c[ev.sem] = ev.val

    def _deps(self, eng, reads, writes):
        for b in reads:
            self._need(eng, b.w)
        for b in writes:
            self._need(eng, b.w)
            for ev in b.r.values():
                self._need(eng, ev)

    def _commit(self, ev, reads, writes):
        for b in writes:
            b.w = ev
            b.r = {}
        for b in reads:
            if b.w is ev:
                continue
            b.r[ev.sem] = ev

    def op(self, eng, fn, reads=(), writes=()):
        if self.dry:
            return None
        self._deps(eng, reads, writes)
        ins = fn(self.e[eng])
        self.cnt[eng] += 1
        ins.then_inc(self.sem[eng], 1)
        self.nins += 1
        clk = dict(self.clk[eng])
        clk[self.sem[eng]] = self.cnt[eng]
        ev = Ev(self.sem[eng], self.cnt[eng], clk)
        self._commit(ev, reads, writes)
        return ev

    def dma(self, eng, out, in_, reads=(), writes=(), **kw):
        if self.dry:
            return None
        i = self.dnext
        self.dnext = (self.dnext + 1) % len(self.dsem)
        self._need(eng, self.dlast[i])
        self._deps(eng, reads, writes)
        ins = self.e[eng].dma_start(out=out, in_=in_, **kw)
        self.dcnt[i] += 16
        ins.then_inc(self.dsem[i], 16)
        self.nins += 1
        clk = dict(self.clk[eng])
        clk[self.dsem[i]] = self.dcnt[i]
        ev = Ev(self.dsem[i], self.dcnt[i], clk)
        self.dlast[i] = ev
        self._commit(ev, reads, writes)
        return ev

    def barrier(self, engs=None):
        if self.dry:
            return
        for e in (engs or self.ENGS):
            for f in self.ENGS:
                if f != e and self.cnt[f] > 0:
                    self._need(e, Ev(self.sem[f], self.cnt[f], {}))
            for ev in self.dlast:
                self._need(e, ev)

    def finish(self):
        self.barrier(engs=("sp",))


class Ring:
    def __init__(self, items):
        self.items = items
        self.i = 0

    def next(self):
        it = self.items[self.i % len(self.items)]
        self.i += 1
        return it


class WStream:
    def __init__(self, k, pf):
        self.k = k
        self.pf = pf
        self.reqs = []
        self.issued = 0
        self.pos = 0
        self.phase = 0
        self.pstart = 0
        self.slots = None
        self.scr = None
        self.sidx = {}
        self.sbuf = {}
        self.done = set()

    def rewind(self):
        self.issued = 0
        self.pos = 0
        self.phase = 0
        self.pstart = 0
        self.pend = {}
        for j, r in enumerate(self.reqs):
            self.pend[r[3]] = j + 1
        self.done = set()

    def new_phase(self, slots, pf):
        self.phase += 1
        self.pf = pf
        self.slots = slots
        self.pstart = self.pos
        assert self.issued <= self.pos or self.k.dry

    def _issue(self, j):
        ap, kc, n, ph, key = self.reqs[j]
        assert ph == self.phase
        t, B = self.slots[(j - self.pstart) % len(self.slots)]
        view = t[:, 0:kc * n].rearrange("p (c n) -> p c n", n=n)
        if key is None:
            self.k.dma("pool", view, ap, writes=[B])
            return
        if key not in self.sidx:
            self.sidx[key] = len(self.sidx)
            self.sbuf[key] = Buf("wsc_%d" % self.sidx[key])
        SB = self.sbuf[key]
        sap = self.scr[self.sidx[key]][:, 0:kc * n]
        if key in self.done:
            self.k.dma("sp", t[:, 0:kc * n], sap, reads=[SB], writes=[B])
        else:
            self.k.dma("pool", view, ap, writes=[B])
            self.k.dma("sp", sap, t[:, 0:kc * n], reads=[B], writes=[SB])
            self.done.add(key)

    def get(self, ap, kc, n, key=None):
        if self.k.dry:
            self.reqs.append((ap, kc, n, self.phase, key))
            self.pos += 1
            t, B = self.slots[0]
            return t[:, 0:kc * n].rearrange("p (c n) -> p c n", n=n), B
        j = self.pos
        self.pos += 1
        lim = min(self.pend[self.phase], j + 1 + self.pf)
        while self.issued < lim:
            self._issue(self.issued)
            self.issued += 1
        t, B = self.slots[(j - self.pstart) % len(self.slots)]
        return t[:, 0:kc * n].rearrange("p (c n) -> p c n", n=n), B


def build_program(layers, fused):
    nc = bass.Bass("TRN2", target_bir_lowering=False)
    dt = nc.dram_tensor
    I = {}

    def inp(name, shape):
        I[name] = dt(name, list(shape), F32, kind="ExternalInput").ap()
        return I[name]

    xT = inp("xT", [D, NTOK])
    cosk = inp("cosk", [128, NTOK])
    sink = inp("sink", [128, NTOK])
    vecs = inp("vecs", [128, NV])
    consts = inp("consts", [128, NCONST])
    w_mod = inp("w_mod", [DEPTH, D, 6 * D])
    w_in = inp("w_in", [DEPTH, D, 6400])
    w_pa = inp("w_pa", [DEPTH, 512, D])
    w_pb = inp("w_pb", [DEPTH, 512, D])
    w_pc = inp("w_pc", [DEPTH, 512, D])
    w_out = inp("w_out", [DEPTH, D, D])
    if 0 in layers:
        ffn_w1 = inp("ffn_w1", [1, D, D_FF])
        ffn_w3 = inp("ffn_w3", [1, D, D_FF])
        ffn_w2 = inp("ffn_w2", [1, D_FF, D])
    if 1 in layers:
        moe_r = inp("moe_router", [1, D, N_EXP])
        moe_w1 = inp("moe_w1", [1, N_EXP, D, D_FFE])
        moe_w3 = inp("moe_w3", [1, N_EXP, D, D_FFE])
        moe_w2 = inp("moe_w2", [1, N_EXP, D_FFE, D])
    last = layers[-1] == DEPTH - 1
    if last:
        outT = dt("outT", [D, HALF], F32, kind="ExternalOutput").ap()
    else:
        outT = dt("x1T", [D, HALF + CTX], F32, kind="ExternalOutput").ap()
    if DEBUG:
        dbg16 = dt("dbg16", [128, 36 * QB], BF16, kind="ExternalOutput").ap()
        dbg32 = dt("dbg32", [128, 16 * EXT], F32, kind="ExternalOutput").ap()
    xm = dt("xm_scr", [D, NTOK], F32).ap()
    xb = dt("xb_scr", [D, NTOK], F32).ap()
    wsc = dt("wsc_scr", [DEPTH * 44, 128, 4096], BF16).ap()

    dbuf = {}

    def DB(name, c0, c1):
        out = []
        for c in range(c0 // 128, (c1 + 127) // 128):
            key = (name, c)
            if key not in dbuf:
                dbuf[key] = Buf("%s_%d" % key)
            out.append(dbuf[key])
        return out

    def fm(ap2d):
        return ap2d.rearrange("(c p) n -> p c n", p=128)

    with ExitStack() as st:
        k = K(nc, st)
        vec = k.sb("vec", [128, NV], F32); Bvec = Buf("vec")
        cst = k.sb("cst", [128, NCONST], F32); Bcst = Buf("cst")
        ones16 = k.sb("ones16", [128, 128], BF16); Bo16 = Buf("ones16")
        modT = k.sb("modT", [128, DEPTH * 96], F32); Bmod = Buf("modT")
        dv = k.sb("dv", [128, DEPTH * 2 * 6 * 8], F32); Bdv = Buf("dv")
        lamt = k.sb("lamt", [128, DEPTH * 8 + 64], F32); Blam = Buf("lamt")
        csb = k.sb("csb", [128, 16], BF16); Bcsb = Buf("csb")
        arena = k.sb("arena", [128, ARENA], BF16)
        banks = [(k.ps("bank%d" % i, [128, 512]), Buf("bank%d" % i)) for i in range(8)]

        k.dma("sp", vec[:], vecs, writes=[Bvec])
        k.dma("sp", cst[:], consts, writes=[Bcst])
        k.op("dve", lambda e: e.memset(ones16[:], 1.0), writes=[Bo16])
        ones32 = cst[:, C_ONES:C_ONES + 128]
        bd32 = cst[:, C_BD:C_BD + 128]
        sel65 = cst[:, C_SEL:C_SEL + 64]
        ident = cst[:, C_ID:C_ID + 128]

        def V(l, name, j=0, n=1):
            o, ln = VL[name]
            return vec[:, l * VLN + o + j: l * VLN + o + j + n]

        def VGl(name, j=0, n=1):
            o, ln = VG[name]
            return vec[:, o + j: o + j + n]

        def DV(l, which, kind, c):
            o = ((l * 2 + which) * 6 + kind) * 8 + c
            return dv[:, o:o + 1]

        class Carver:
            def __init__(self):
                self.off = 0

            def take(self, nbytes, dtype, name):
                nb = (nbytes + 63) // 64 * 64
                a = arena[:, self.off // 2:(self.off + nb) // 2]
                self.off += nb
                assert self.off <= ARENA * 2, (name, self.off)
                if dtype is F32:
                    a = a.bitcast(F32)
                    return a[:, 0:nbytes // 4]
                return a[:, 0:nbytes // 2]

        epsc = k.sb("epsc", [128, 1], F32); Beps = Buf("epsc")
        k.op("dve", lambda e: e.memset(epsc[:], EPS), writes=[Beps])

        def rsqrt_from(out, Bout, src_, Bsrc, scale, P):
            k.op("act", lambda e: e.activation(out, src_, AF.Ln, bias=epsc[0:P, :], scale=scale), reads=[Bsrc, Beps],
                 writes=[Bout])
            k.op("act", lambda e: e.activation(out, out, AF.Exp, scale=-0.5), reads=[Bout], writes=[Bout])

        def mm(bank_ap, lhsT, rhs, start, stop, reads, Bbank, skip=False):
            k.op("pe", lambda e: e.matmul(bank_ap, lhsT, rhs, start=start, stop=stop, skip_group_check=skip), reads=reads,
                 writes=[Bbank])

        def mod_phase(ws, l):
            bank, Bb = banks[0]
            for g in range(12):
                wt, Bw = ws.get(fm(w_mod[l])[:, :, g * 512:(g + 1) * 512], 8, 512)
                for nl in range(4):
                    j = g * 4 + nl
                    for c in range(8):
                        mm(bank[:, 2 * j:2 * j + 2], wt[:, c, nl * 128:(nl + 1) * 128], csb[:, 2 * c:2 * c + 2],
                           c == 0, c == 7, [Bw, Bcsb], Bb)
            mv = modT[:, l * 96:(l + 1) * 96].rearrange("p (j w) -> p j w", w=2)
            bv = bank[:, 0:96].rearrange("p (j w) -> p j w", w=2)
            for w in range(2):
                k.op("dve", lambda e: e.tensor_tensor(mv[:, :, w], bv[:, :, w], V(l, "b_mod", 0, 48), ALU.add),
                     reads=[Bb, Bvec], writes=[Bmod])
            for w in range(2):
                for sub, (sh, sc, gt, ng) in enumerate(((0, 1, 2, "n1g"), (3, 4, 5, "n2g"))):
                    o = ((l * 2 + w) * 6 + sub * 3) * 8
                    k.op("dve", lambda e: e.scalar_tensor_tensor(dv[:, o:o + 8], mv[:, sc * 8:(sc + 1) * 8, w], 1.0,
                                                                 V(l, ng, 0, 8), ALU.add, ALU.mult),
                         reads=[Bmod, Bvec], writes=[Bdv])
                    k.op("dve", lambda e: e.tensor_copy(dv[:, o + 8:o + 16], mv[:, sh * 8:(sh + 1) * 8, w]),
                         reads=[Bmod], writes=[Bdv])
                    k.op("dve", lambda e: e.tensor_copy(dv[:, o + 16:o + 24], mv[:, gt * 8:(gt + 1) * 8, w]),
                         reads=[Bmod], writes=[Bdv])
            lam_init = 0.8 - 0.6 * math.exp(-0.3 * l)
            lo = l * 8
            tmp = lamt[:, DEPTH * 8:DEPTH * 8 + 64]
            for j in range(2):
                k.op("dve", lambda e: e.tensor_tensor(tmp, V(l, "lamv", j * 128, 64), V(l, "lamv", j * 128 + 64, 64),
                                                      ALU.mult), reads=[Bvec], writes=[Blam])
                k.op("dve", lambda e: e.reduce_sum(lamt[:, lo + j:lo + j + 1], tmp, AX.X), reads=[Blam], writes=[Blam])
            k.op("act", lambda e: e.activation(lamt[:, lo + 2:lo + 4], lamt[:, lo:lo + 2], AF.Exp), reads=[Blam],
                 writes=[Blam])
            k.op("dve", lambda e: e.tensor_tensor(lamt[:, lo + 4:lo + 5], lamt[:, lo + 3:lo + 4], lamt[:, lo + 2:lo + 3],
                                                  ALU.subtract), reads=[Blam], writes=[Blam])
            k.op("dve", lambda e: e.tensor_scalar(lamt[:, lo + 5:lo + 6], lamt[:, lo + 4:lo + 5], -lam_init, None,
                                                  ALU.add), reads=[Blam], writes=[Blam])
            k.op("dve", lambda e: e.tensor_scalar(lamt[:, lo + 6:lo + 7], V(l, "subg"), 1.0 - lam_init, None, ALU.mult),
                 reads=[Bvec], writes=[Blam])

        def norm_block(T, xv, Bx, n, Acol, Bcol, out, Bout, out2=None, Bout2=None):
            bank, Bb = T["pp"].next()
            for c in range(8):
                sq, Bs = T["sq"].next()
                k.op("act", lambda e: e.activation(sq[:, :n], xv[:, c, :], AF.Square), reads=[Bx], writes=[Bs])
                mm(bank[:, :n], ones32, sq[:, :n], c == 0, c == 7, [Bs, Bcst], Bb)
            rs, Br = T["rs"].next()
            rsqrt_from(rs[:, :n], Br, bank[:, :n], Bb, 1.0 / D, 128)
            for c in range(8):
                if Bcol is None:
                    k.op("dve", lambda e: e.scalar_tensor_tensor(out[:, c, :], xv[:, c, :], Acol(c), rs[:, :n], ALU.mult,
                                                                 ALU.mult), reads=[Bx, Br, Bdv, Bvec], writes=[Bout])
                    continue
                tmp, Bt = T["tmp"].next()
                k.op("dve", lambda e: e.scalar_tensor_tensor(tmp[:, :n], xv[:, c, :], Acol(c), rs[:, :n], ALU.mult,
                                                             ALU.mult), reads=[Bx, Br, Bdv], writes=[Bt])
                k.op("act", lambda e: e.activation(out[:, c, :], tmp[:, :n], AF.Identity, bias=Bcol(c)),
                     reads=[Bt, Bdv], writes=[Bout])
                if out2 is not None:
                    k.op("act", lambda e: e.activation(out2[:, c, :], tmp[:, :n], AF.Identity, bias=Bcol(c)),
                         reads=[Bt, Bdv], writes=[Bout2])

        def rope_pair(T, bA, BA, bB, BB, P, n, cs, sn, Bcs, norm_g, dests):
            if norm_g is not None:
                gA, gB = norm_g
                sA, BsA = T["sq"].next()
                sB, BsB = T["sq"].next()
                k.op("act", lambda e: e.activation(sA[0:P, :n], bA[0:P, :n], AF.Square), reads=[BA], writes=[BsA])
                k.op("act", lambda e: e.activation(sB[0:P, :n], bB[0:P, :n], AF.Square), reads=[BB], writes=[BsB])
                bank, Bb = T["pp"].next()
                mm(bank[0:P, :n], bd32[0:P, 0:P], sA[0:P, :n], True, False, [BsA, Bcst], Bb)
                mm(bank[0:P, :n], bd32[0:P, 0:P], sB[0:P, :n], False, True, [BsB, Bcst], Bb)
                rs, Br = T["rs"].next()
                rsqrt_from(rs[0:P, :n], Br, bank[0:P, :n], Bb, 1.0 / 64, P)
                nA, BnA = T["tmp"].next()
                nB, BnB = T["tmp"].next()
                k.op("dve", lambda e: e.scalar_tensor_tensor(nA[0:P, :n], bA[0:P, :n], gA[0:P], rs[0:P, :n], ALU.mult,
                                                             ALU.mult), reads=[BA, Br, Bvec], writes=[BnA])
                k.op("dve", lambda e: e.scalar_tensor_tensor(nB[0:P, :n], bB[0:P, :n], gB[0:P], rs[0:P, :n], ALU.mult,
                                                             ALU.mult), reads=[BB, Br, Bvec], writes=[BnB])
                srcA, BsrcA, srcB, BsrcB = nA, BnA, nB, BnB
            else:
                srcA, BsrcA, srcB, BsrcB = bA, BA, bB, BB
            t1, B1 = T["rp"].next()
            t2, B2 = T["rp"].next()
            t3, B3 = T["rp"].next()
            t4, B4 = T["rp"].next()
            k.op("dve", lambda e: e.tensor_tensor(t1[0:P, :n], srcA[0:P, :n], cs[0:P, :n], ALU.mult),
                 reads=[BsrcA, Bcs], writes=[B1])
            k.op("dve", lambda e: e.tensor_tensor(t2[0:P, :n], srcB[0:P, :n], sn[0:P, :n], ALU.mult),
                 reads=[BsrcB, Bcs], writes=[B2])
            k.op("dve", lambda e: e.tensor_tensor(t3[0:P, :n], srcB[0:P, :n], cs[0:P, :n], ALU.mult),
                 reads=[BsrcB, Bcs], writes=[B3])
            k.op("dve", lambda e: e.tensor_tensor(t4[0:P, :n], srcA[0:P, :n], sn[0:P, :n], ALU.mult),
                 reads=[BsrcA, Bcs], writes=[B4])
            for (slo, dl) in dests:
                for (dtile, Bd, dlo) in dl:
                    k.op("pool", lambda e: e.tensor_tensor(dtile[dlo:dlo + 32, :n], t1[slo:slo + 32, :n],
                                                           t2[slo:slo + 32, :n], ALU.subtract),
                         reads=[B1, B2], writes=[Bd])
                    k.op("pool", lambda e: e.tensor_tensor(dtile[dlo + 32:dlo + 64, :n], t3[slo:slo + 32, :n],
                                                           t4[slo:slo + 32, :n], ALU.add),
                         reads=[B3, B4], writes=[Bd])

        def mixer_phase(ws, l, src, sname, qsegs):
            cv = Carver()
            slots = [(cv.take(8192, BF16, "ws%d" % i), Buf("ws%d" % i)) for i in range(3)]
            kTA = [(cv.take(NTOK * 2, BF16, "kTA"), Buf("kTA%d" % i)) for i in range(2)]
            kTC = [(cv.take(NTOK * 2, BF16, "kTC"), Buf("kTC%d" % i)) for i in range(4)]
            VAf = cv.take(NKB * 2 * 65 * 2, BF16, "VA")
            VA = VAf.rearrange("p (b v d) -> p b v d", v=2, d=65); BVA = Buf("VA")
            VC = cv.take(NKB * 512 * 2, BF16, "VC").rearrange("p (b n) -> p b n", n=512); BVC = Buf("VC")
            T = {}
            T["xe"] = Ring([(cv.take(8 * EXT * 4, F32, "xe").rearrange("p (c n) -> p c n", n=EXT), Buf("xe%d" % i))
                            for i in range(1)])
            hT = cv.take(8 * EXT * 2, BF16, "hT").rearrange("p (c n) -> p c n", n=EXT); BhT = Buf("hT")
            cs_t = cv.take(EXT * 4, F32, "cs"); sn_t = cv.take(EXT * 4, F32, "sn"); Bcs = Buf("cs")
            yx = cv.take(4 * EXT * 4, F32, "yx").rearrange("p (c n) -> p c n", n=EXT); Byx = Buf("yx")
            cacc = cv.take(4 * QB * 4, F32, "cacc").rearrange("p (c n) -> p c n", n=QB); Bcacc = [Buf("cacc%d" % i) for i in range(4)]
            qTA = [(cv.take(2 * QB * 2, BF16, "qTA"), Buf("qTA%d" % i)) for i in range(4)]
            qTC = [(cv.take(2 * QB * 2, BF16, "qTC"), Buf("qTC%d" % i)) for i in range(4)]
            oaT = [(cv.take(QB * 2, BF16, "oaT"), Buf("oaT%d" % i)) for i in range(4)]
            obT = [(cv.take(QB * 2, BF16, "obT"), Buf("obT%d" % i)) for i in range(4)]
            ocT = [(cv.take(QB * 2, BF16, "ocT"), Buf("ocT%d" % i)) for i in range(4)]
            mT = cv.take(8 * QB * 2, BF16, "mT").rearrange("p (c n) -> p c n", n=QB); BmT = Buf("mT")
            macc = cv.take(8 * QB * 4, F32, "macc").rearrange("p (c n) -> p c n", n=QB); Bmacc = Buf("macc")
            T["sq"] = Ring([(cv.take(EXT * 4, F32, "sq"), Buf("sq%d" % i)) for i in range(2)])
            T["tmp"] = Ring([(cv.take(EXT * 4, F32, "tmp"), Buf("tmp%d" % i)) for i in range(3)])
            T["rs"] = Ring([(cv.take(EXT * 4, F32, "rs"), Buf("rs%d" % i)) for i in range(2)])
            ri_l = [(cv.take(512 * 4, F32, "ri"), Buf("ri%d" % i)) for i in range(2)]
            T["ri"] = Ring(ri_l)
            T["rp"] = Ring([(ri_l[i // 2][0][:, (i % 2) * QB:(i % 2 + 1) * QB], ri_l[i // 2][1]) for i in range(4)])
            T["pT"] = Ring([(cv.take(512 * 2, BF16, "pT"), Buf("pT%d" % i)) for i in range(4)])
            T["osb"] = Ring([(cv.take(512 * 4, F32, "osb"), Buf("osb%d" % i)) for i in range(2)])
            T["om"] = Ring([(cv.take(512 * 4, F32, "om"), Buf("om%d" % i)) for i in range(2)])
            T["dd"] = Ring([(cv.take(QB * 4, F32, "dd"), Buf("dd%d" % i)) for i in range(2)])
            T["pp"] = Ring(banks[0:8])
            wl = fm(w_in[l])
            srcv = fm(src)

            k.op("pool", lambda e: e.memset(VA[:, :, :, 64:65], 1.0), writes=[BVA])
            for (t_, B_) in qTA + qTC:
                k.op("pool", lambda e: e.memset(t_[:, :], 0.0), writes=[B_])

            wkv = [(slots[0][0][:, 0:2048].rearrange("p (c n) -> p c n", n=256), Buf("wkv_akv"), wl[:, :, OFF_AK:OFF_AK + 256]),
                   (slots[0][0][:, 2048:4096].rearrange("p (c n) -> p c n", n=256), Buf("wkv_ck0"), wl[:, :, OFF_CK:OFF_CK + 256]),
                   (slots[1][0][:, 0:2048].rearrange("p (c n) -> p c n", n=256), Buf("wkv_ck1"),
                    wl[:, :, OFF_CK + 256:OFF_CK + 512]),
                   (slots[2][0][:, 0:4096].rearrange("p (c n) -> p c n", n=512), Buf("wkv_cv"), wl[:, :, OFF_CV:OFF_CV + 512])]
            for (wt_, Bw_, ap_) in wkv:
                k.dma("pool", wt_, ap_, writes=[Bw_])
            n = QB
            xe0, Bxe0 = T["xe"].items[0]
            xring = [(xe0[:, :, 0:n], Bxe0), (macc[:, :, :], Buf("xe_kv2"))]
            hring = [(hT[:, :, 0:n], BhT), (mT[:, :, :], Buf("hT_kv2"))]
            csring = [(cs_t, sn_t, Bcs), (yx[:, 0, :], yx[:, 1, :], Buf("cs_kv2"))]
            NKV = NTOK // QB

            def kv_load(kb):
                c0 = kb * QB
                xv_, Bx_ = xring[kb % 2]
                k.dma("sp", xv_, srcv[:, :, c0:c0 + n], reads=DB(sname, c0, c0 + n), writes=[Bx_])
                cs_, sn_, Bc_ = csring[kb % 2]
                k.dma("sp", cs_[:, 0:n], cosk[:, c0:c0 + n], writes=[Bc_])
                k.dma("sp", sn_[:, 0:n], sink[:, c0:c0 + n], writes=[Bc_])

            def kv_norm(kb):
                which = 1 if kb * QB >= SEQ else 0
                xv_, Bx_ = xring[kb % 2]
                hv_, Bh_ = hring[kb % 2]
                norm_block(T, xv_, Bx_, n, lambda c: DV(l, which, 0, c), lambda c: DV(l, which, 1, c), hv_, Bh_)

            kv_load(0)
            kv_norm(0)
            for kb in range(NKV):
                c0 = kb * QB
                hv, Bh = hring[kb % 2]
                cs_k, sn_k, Bcs_k = csring[kb % 2]
                if kb + 1 < NKV:
                    kv_load(kb + 1)
                wt, Bw, _ = wkv[0]
                (bA, BA), (bB, BB) = T["pp"].next(), T["pp"].next()
                for part, (bk, Bk) in enumerate(((bA, BA), (bB, BB))):
                    for c in range(8):
                        mm(bk[0:64, :n], wt[:, c, part * 64:(part + 1) * 64], hv[:, c, :], c == 0, c == 7, [Bw, Bh], Bk)
                dests = []
                for kvh in range(2):
                    t, Bt = kTA[kvh]
                    dests.append((32 * kvh, [(t[:, c0:c0 + n], Bt, 0), (t[:, c0:c0 + n], Bt, 64)]))
                rope_pair(T, bA, BA, bB, BB, 64, n, cs_k, sn_k, Bcs_k, (V(l, "kng", 0), V(l, "kng", 1)), dests)
                for sbk in range(n // 128):
                    bk, Bk = T["pp"].next()
                    for c in range(8):
                        mm(bk[:, 0:128], hv[:, c, sbk * 128:(sbk + 1) * 128], wt[:, c, 128:256], c == 0, c == 7, [Bw, Bh], Bk)
                    blk = c0 // 128 + sbk
                    k.op("act", lambda e: e.activation(VA[:, blk, :, 0:64], bk[:, 0:128].rearrange("p (v d) -> p v d", d=64),
                                                       AF.Copy), reads=[Bk], writes=[BVA])
                for pr in range(2):
                    wt, Bw, _ = wkv[1 + pr]
                    (bA, BA), (bB, BB) = T["pp"].next(), T["pp"].next()
                    for part, (bk, Bk) in enumerate(((bA, BA), (bB, BB))):
                        for c in range(8):
                            mm(bk[:, :n], wt[:, c, part * 128:(part + 1) * 128], hv[:, c, :], c == 0, c == 7, [Bw, Bh], Bk)
                    dests = []
                    for ul in range(4):
                        u = pr * 4 + ul
                        t, Bt = kTC[u // 2]
                        dests.append((32 * ul, [(t[:, c0:c0 + n], Bt, 64 * (u % 2))]))
                    rope_pair(T, bA, BA, bB, BB, 128, n, cs_k, sn_k, Bcs_k, None, dests)
                    if pr == 0 and kb + 1 < NKV:
                        kv_norm(kb + 1)
                wt, Bw, _ = wkv[3]
                for sbk in range(n // 128):
                    bk, Bk = T["pp"].next()
                    for c in range(8):
                        mm(bk[:, 0:512], hv[:, c, sbk * 128:(sbk + 1) * 128], wt[:, c, :], c == 0, c == 7, [Bw, Bh], Bk)
                    blk = c0 // 128 + sbk
                    k.op("act", lambda e: e.activation(VC[:, blk, :], bk[:, 0:512], AF.Copy), reads=[Bk], writes=[BVC])

            k.barrier()
            qslots = [(slots[i // 2][0][:, (i % 2) * 2048:(i % 2 + 1) * 2048], Buf("qws%d" % i)) for i in range(6)]
            ws.new_phase(qslots, 4)
            for seg in qsegs:
                which = seg["which"]
                nblk = seg["n"] // QB
                for qb in range(nblk):
                    c0 = seg["c0"] + qb * QB
                    n = QB
                    xe, Bxe = T["xe"].next()
                    k.dma("sp", xe[:, :, 15:15 + n], srcv[:, :, c0:c0 + n], reads=DB(sname, c0, c0 + n), writes=[Bxe])
                    lcol = seg["hl_col"] if qb == 0 else c0 - 15
                    rcol = seg["hr_col"] if qb == nblk - 1 else c0 + n
                    k.dma("sp", xe[:, :, 0:15], srcv[:, :, lcol:lcol + 15], reads=DB(sname, lcol, lcol + 15), writes=[Bxe])
                    k.dma("sp", xe[:, :, 15 + n:30 + n], srcv[:, :, rcol:rcol + 15], reads=DB(sname, rcol, rcol + 15),
                          writes=[Bxe])
                    k.dma("sp", cs_t[:, 0:n], cosk[:, c0:c0 + n], writes=[Bcs])
                    k.dma("sp", sn_t[:, 0:n], sink[:, c0:c0 + n], writes=[Bcs])
                    norm_block(T, xe, Bxe, EXT, lambda c: DV(l, which, 0, c), lambda c: DV(l, which, 1, c), hT, BhT)
                    hm = hT[:, :, 15:15 + n]
                    for j in range(4):
                        if j % 2 == 0:
                            jj = j // 2
                            wa, Bwa = ws.get(wl[:, :, OFF_BZ + jj * 256:OFF_BZ + (jj + 1) * 256], 8, 256, key=(l, "bza", jj))
                            wg, Bwg = ws.get(wl[:, :, OFF_BZ + 512 + jj * 256:OFF_BZ + 512 + (jj + 1) * 256], 8, 256,
                                             key=(l, "bzg", jj))
                        j2 = j % 2
                        (ba, Ba), (bg, Bg) = T["pp"].next(), T["pp"].next()
                        for c in range(8):
                            mm(ba[:, :EXT], wa[:, c, j2 * 128:(j2 + 1) * 128], hT[:, c, :], c == 0, c == 7, [Bwa, BhT], Ba)
                        for c in range(8):
                            mm(bg[:, :EXT], wg[:, c, j2 * 128:(j2 + 1) * 128], hT[:, c, :], c == 0, c == 7, [Bwg, BhT], Bg)
                        sg, Bsg = T["tmp"].next()
                        k.op("act", lambda e: e.activation(sg[:, :EXT], bg[:, :EXT], AF.Sigmoid), reads=[Bg], writes=[Bsg])
                        k.op("dve", lambda e: e.tensor_tensor(yx[:, j, :], ba[:, :EXT], sg[:, :EXT], ALU.mult),
                             reads=[Ba, Bsg], writes=[Byx])
                    if qb == 0:
                        mcol = VGl("hmask", seg["hl_mask"])
                        k.op("pool", lambda e: e.tensor_scalar(yx[:, :, 0:15], yx[:, :, 0:15], mcol, None, ALU.mult),
                             reads=[Byx, Bvec], writes=[Byx])
                    if qb == nblk - 1:
                        mcol = VGl("hmask", seg["hr_mask"])
                        k.op("pool", lambda e: e.tensor_scalar(yx[:, :, 15 + n:30 + n], yx[:, :, 15 + n:30 + n], mcol, None,
                                                               ALU.mult), reads=[Byx, Bvec], writes=[Byx])
                    side = []

                    def conv_tap(j, t):
                        if t == 0:
                            k.op("dve", lambda e: e.tensor_scalar(cacc[:, j, :], yx[:, j, 0:n], V(l, "dww", j * 31),
                                                                  V(l, "dwb", j), ALU.mult, ALU.add),
                                 reads=[Byx, Bvec], writes=[Bcacc[j]])
                        else:
                            k.op("dve", lambda e: e.scalar_tensor_tensor(cacc[:, j, :], yx[:, j, t:t + n],
                                                                         V(l, "dww", j * 31 + t), cacc[:, j, :], ALU.mult,
                                                                         ALU.add), reads=[Byx, Bvec, Bcacc[j]], writes=[Bcacc[j]])
                    for t in range(31):
                        for j in range(4):
                            side.append(lambda j=j, t=t: conv_tap(j, t))
                    mean, rstd, m2, d1 = yx[:, 0, 0:n], yx[:, 1, 0:n], yx[:, 2, 0:n], yx[:, 3, 0:n]

                    lnst = {}

                    def ln_sq(j0):
                        if j0 == 0:
                            lnst["bank"] = T["pp"].next()
                        for j in (j0, j0 + 1):
                            sq, Bs = T["sq"].next()
                            lnst[j] = (sq, Bs)
                            k.op("act", lambda e: e.activation(sq[:, :n], cacc[:, j, :], AF.Square), reads=[Bcacc[j]], writes=[Bs])

                    def ln_mm(j0):
                        bfull, B1 = lnst["bank"]
                        b1, b2, B2 = bfull[:, 0:n], bfull[:, n:2 * n], B1
                        for j in (j0, j0 + 1):
                            sq, Bs = lnst[j]
                            mm(b1[:, :n], ones32, cacc[:, j, :], j == 0, j == 3, [Bcacc[j], Bcst], B1, skip=True)
                            mm(b2[:, :n], ones32, sq[:, :n], False, j == 3, [Bs, Bcst], B2, skip=True)

                    def ln_var():
                        bfull, B1 = lnst["bank"]
                        b1, b2, B2 = bfull[:, 0:n], bfull[:, n:2 * n], B1
                        k.op("dve", lambda e: e.tensor_scalar(mean, b1[:, :n], 1.0 / 512, None, ALU.mult),
                             reads=[B1], writes=[Byx])
                        k.op("dve", lambda e: e.tensor_tensor(m2, mean, mean, ALU.mult), reads=[Byx], writes=[Byx])
                        k.op("dve", lambda e: e.scalar_tensor_tensor(rstd, b2[:, :n], 1.0 / 512, m2, ALU.mult,
                                                                     ALU.subtract), reads=[B2, Byx], writes=[Byx])

                    def ln_rs():
                        rsqrt_from(rstd, Byx, rstd, Byx, 1.0, 128)

                    def ln_sub():
                        for j in range(4):
                            k.op("pool", lambda e: e.tensor_tensor(cacc[:, j, :], cacc[:, j, :], mean, ALU.subtract),
                                 reads=[Bcacc[j], Byx], writes=[Bcacc[j]])

                    def ln_mul():
                        for j in range(4):
                            k.op("dve", lambda e: e.tensor_tensor(cacc[:, j, :], cacc[:, j, :], rstd, ALU.mult),
                                 reads=[Bcacc[j], Byx], writes=[Bcacc[j]])

                    def ln_act():
                        for j in range(4):
                            ot, Bot = obT[j]
                            k.op("act", lambda e: e.activation(ot[:, :n], cacc[:, j, :], AF.Silu, bias=V(l, "lnb", j),
                                                               scale=V(l, "lng", j)), reads=[Bcacc[j], Bvec], writes=[Bot])

                    def ln_mm0_sq2():
                        ln_mm(0)
                        ln_sq(2)
                    side += [None] * 21
                    side += [lambda: ln_sq(0), None, ln_mm0_sq2, None, lambda: ln_mm(2), None, None, ln_var, None, None, ln_rs,
                             None, ln_sub, None, None, ln_mul, None, None, ln_act]
                    for (off, qT, ng) in ((OFF_AQ, qTA, (V(l, "qng", 0), V(l, "qng", 1))), (OFF_CQ, qTC, None)):
                        for pr in range(2):
                            wt, Bw = ws.get(wl[:, :, off + pr * 256:off + (pr + 1) * 256], 8, 256, key=(l, "q", off, pr))
                            (bA, BA), (bB, BB) = T["pp"].next(), T["pp"].next()
                            for part, (bk, Bk) in enumerate(((bA, BA), (bB, BB))):
                                for c in range(8):
                                    mm(bk[:, :n], wt[:, c, part * 128:(part + 1) * 128], hm[:, c, :], c == 0, c == 7,
                                       [Bw, BhT], Bk)
                            dests = []
                            for ul in range(4):
                                u = pr * 4 + ul
                                t, Bt = qT[u // 2]
                                dests.append((32 * ul, [(t[:, (u % 2) * n:(u % 2 + 1) * n], Bt, 64 * (u % 2))]))
                            rope_pair(T, bA, BA, bB, BB, 128, n, cs_t, sn_t, Bcs, ng, dests)
                    kbl = [2 * kp + i for kp in seg["keys"] for i in range(2)]
                    NP = len(kbl)
                    n2 = 2 * n
                    units = [("A", hp) for hp in range(4)] + [("C", hc) for hc in range(4)]
                    flat = [(ui, pi) for ui in range(len(units)) for pi in range(NP)]
                    pp_saved = T["pp"]
                    T["pp"] = Ring(banks[0:1]); T["st"] = Ring(banks[1:4]); T["ac"] = Ring(banks[4:8])
                    stq = {}

                    def emit_qk(s_):
                        ui, pi = flat[s_]
                        kind, a_ = units[ui]
                        (qt, Bq) = qTA[a_] if kind == "A" else qTC[a_]
                        (kt, Bkt) = kTA[a_ // 2] if kind == "A" else kTC[a_]
                        stb, Bst = T["st"].next()
                        kbk = kbl[pi]
                        mm(stb[:, 0:n2], kt[:, kbk * 128:(kbk + 1) * 128], qt[:, 0:n2], True, True, [Bkt, Bq], Bst)
                        stq[s_] = (stb, Bst)

                    pend = []

                    def flush(all_=False):
                        for it in pend:
                            it[0] -= 1
                        while pend and (all_ or pend[0][0] <= 0):
                            pend.pop(0)[1]()

                    def epi_A(hp, acc, Bacc):
                        if pend:
                            flush(True)
                        osb, Bosb = T["osb"].next()
                        k.op("dve", lambda e: e.tensor_copy(osb[0:65, 0:n2], acc[0:65, 0:n2]), reads=[Bacc], writes=[Bosb])
                        st_ = {}

                        def p1():
                            rb_, Brb = T["pp"].next()
                            st_["rb"] = (rb_, Brb)
                            mm(rb_[0:64, 0:n2], sel65[0:65, :], osb[0:65, 0:n2], True, True, [Bosb, Bcst], Brb)

                        def p2():
                            rb_, Brb = st_["rb"]
                            ri, Bri = T["ri"].next()
                            k.op("act", lambda e: e.activation(ri[0:64, 0:n2], rb_[0:64, 0:n2], AF.Ln), reads=[Brb], writes=[Bri])
                            k.op("act", lambda e: e.activation(ri[0:64, 0:n2], ri[0:64, 0:n2], AF.Exp, scale=-1.0),
                                 reads=[Bri], writes=[Bri])
                            ot, Bot = oaT[hp]
                            for hh in range(2):
                                k.op("dve", lambda e: e.tensor_tensor(ot[64 * hh:64 * hh + 64, :n], osb[0:64, hh * n:(hh + 1) * n],
                                                                      ri[0:64, hh * n:(hh + 1) * n], ALU.mult),
                                     reads=[Bosb, Bri], writes=[Bot])
                        pend.append([4, p1])
                        pend.append([6, p2])

                    def epi_C(hc, acc, Bacc, rsk, Brsk):
                        if pend:
                            flush(True)
                        st_ = {}

                        def p0():
                            ri, Bri = T["ri"].next()
                            k.op("act", lambda e: e.activation(ri[:, 0:n2], rsk[:, 0:n2], AF.Ln), reads=[Brsk], writes=[Bri])
                            k.op("act", lambda e: e.activation(ri[:, 0:n2], ri[:, 0:n2], AF.Exp, scale=-1.0), reads=[Bri],
                                 writes=[Bri])
                            om, Bom = T["om"].next()
                            k.op("dve", lambda e: e.tensor_tensor(om[:, 0:n2], acc[:, 0:n2], ri[:, 0:n2], ALU.mult),
                                 reads=[Bacc, Bri], writes=[Bom])
                            dd, Bdd = T["dd"].next()
                            st_["dd"] = (dd, Bdd)
                            k.op("dve", lambda e: e.scalar_tensor_tensor(dd[:, :n], om[:, n:n2], lamt[:, l * 8 + 5:l * 8 + 6],
                                                                         om[:, 0:n], ALU.mult, ALU.add),
                                 reads=[Bom, Blam], writes=[Bdd])
                            sq, Bs = T["sq"].next()
                            st_["sq"] = (sq, Bs)
                            k.op("pool", lambda e: e.tensor_tensor(sq[:, :n], dd[:, :n], dd[:, :n], ALU.mult), reads=[Bdd],
                                 writes=[Bs])

                        def p1():
                            sq, Bs = st_["sq"]
                            bk, Bk = T["pp"].next()
                            st_["bk"] = (bk, Bk)
                            mm(bk[:, :n], ones32, sq[:, :n], True, True, [Bs, Bcst], Bk)

                        def p2():
                            bk, Bk = st_["bk"]
                            dd, Bdd = st_["dd"]
                            rs, Br = T["rs"].next()
                            rsqrt_from(rs[:, :n], Br, bk[:, :n], Bk, 1.0 / 128, 128)
                            ot, Bot = ocT[hc]
                            k.op("dve", lambda e: e.scalar_tensor_tensor(ot[:, :n], dd[:, :n], lamt[:, l * 8 + 6:l * 8 + 7],
                                                                         rs[:, :n], ALU.mult, ALU.mult),
                                 reads=[Bdd, Br, Blam], writes=[Bot])
                        pend.append([1, p0])
                        pend.append([4, p1])
                        pend.append([7, p2])

                    SK = 2
                    for s_ in range(min(SK, len(flat))):
                        emit_qk(s_)
                    per_step = (len(side) + max(1, len(flat) - 6) - 1) // max(1, len(flat) - 6)
                    cur = None
                    for s_, (ui, pi) in enumerate(flat):
                        if s_ + SK < len(flat):
                            emit_qk(s_ + SK)
                        kind, a_ = units[ui]
                        stb, Bst = stq.pop(s_)
                        pT, BpT = T["pT"].next()
                        k.op("act", lambda e: e.activation(pT[:, 0:n2], stb[:, 0:n2], AF.Exp, scale=0.125),
                             reads=[Bst], writes=[BpT])
                        if pi == 0:
                            cur = [T["ac"].next()]
                            if kind == "C":
                                cur.append(T["ac"].next())
                        acc, Bacc = cur[0]
                        kbk = kbl[pi]
                        first = pi == 0
                        lastk = pi == NP - 1
                        if kind == "A":
                            mm(acc[0:65, 0:n2], VA[:, kbk, a_ // 2, :], pT[:, 0:n2], first, lastk, [BVA, BpT], Bacc)
                        else:
                            rsk, Brsk = cur[1]
                            mm(acc[:, 0:n2], VC[:, kbk, a_ * 128:(a_ + 1) * 128], pT[:, 0:n2], first, lastk, [BVC, BpT], Bacc)
                            mm(rsk[:, 0:n2], ones16[:], pT[:, 0:n2], first, lastk, [Bo16, BpT], Brsk)
                        flush()
                        if pi == NP - 1:
                            if kind == "A":
                                epi_A(a_, acc, Bacc)
                            else:
                                epi_C(a_, acc, Bacc, cur[1][0], cur[1][1])
                        for _ in range(per_step):
                            if side:
                                f_ = side.pop(0)
                                if f_ is not None:
                                    f_()
                    flush(True)
                    while side:
                        f_ = side.pop(0)
                        if f_ is not None:
                            f_()
                    T["pp"] = pp_saved
                    for a, (wp, oT) in enumerate(((w_pa, oaT), (w_pb, obT), (w_pc, ocT))):
                        wpv = wp[l].rearrange("(c p) n -> p c n", p=128)
                        for ng in range(4):
                            wg, Bwg = ws.get(wl[:, :, OFF_GT + a * 1024 + ng * 256:OFF_GT + a * 1024 + (ng + 1) * 256], 8, 256,
                                             key=(l, "gt", a, ng))
                            wpt, Bwp = ws.get(wpv[:, :, ng * 256:(ng + 1) * 256], 4, 256, key=(l, "wp", a, ng))
                            for nl in range(2):
                                nn = ng * 2 + nl
                                (bg, Bg), (bp, Bp) = T["pp"].next(), T["pp"].next()
                                for c in range(8):
                                    mm(bg[:, :n], wg[:, c, nl * 128:(nl + 1) * 128], hm[:, c, :], c == 0, c == 7, [Bwg, BhT], Bg)
                                for c in range(4):
                                    mm(bp[:, :n], wpt[:, c, nl * 128:(nl + 1) * 128], oT[c][0][:, :n], c == 0, c == 3,
                                       [Bwp, oT[c][1]], Bp)
                                gs, Bgs = T["tmp"].next()
                                k.op("act", lambda e: e.activation(gs[:, :n], bg[:, :n], AF.Sigmoid,
                                                                   bias=V(l, "b_gate", a * 8 + nn)),
                                     reads=[Bg, Bvec], writes=[Bgs])
                                if a == 0:
                                    k.op("dve", lambda e: e.tensor_tensor(macc[:, nn, :], gs[:, :n], bp[:, :n], ALU.mult),
                                         reads=[Bgs, Bp], writes=[Bmacc])
                                else:
                                    k.op("dve", lambda e: e.tensor_tensor(gs[:, :n], gs[:, :n], bp[:, :n], ALU.mult),
                                         reads=[Bgs, Bp], writes=[Bgs])
                                    if a == 1:
                                        k.op("dve", lambda e: e.tensor_tensor(macc[:, nn, :], macc[:, nn, :], gs[:, :n], ALU.add),
                                             reads=[Bgs, Bmacc], writes=[Bmacc])
                                    else:
                                        k.op("dve", lambda e: e.tensor_tensor(mT[:, nn, :], macc[:, nn, :], gs[:, :n], ALU.add),
                                             reads=[Bgs, Bmacc], writes=[BmT])
                    wov = fm(w_out[l])
                    for ng in range(4):
                        wt, Bw = ws.get(wov[:, :, ng * 256:(ng + 1) * 256], 8, 256, key=(l, "wo", ng))
                        for nl in range(2):
                            nn = ng * 2 + nl
                            bk, Bk = T["pp"].next()
                            for c in range(8):
                                mm(bk[:, :n], wt[:, c, nl * 128:(nl + 1) * 128], mT[:, c, :], c == 0, c == 7, [Bw, BmT], Bk)
                            k.op("dve", lambda e: e.scalar_tensor_tensor(macc[:, nn, :], bk[:, :n], DV(l, which, 2, nn),
                                                                         xe[:, nn, 15:15 + n], ALU.mult, ALU.add),
                                 reads=[Bk, Bdv, Bxe], writes=[Bmacc])
                    k.dma("sp", fm(xm)[:, :, c0:c0 + n], macc[:, :, :], reads=[Bmacc], writes=DB("xm_scr", c0, c0 + n))
                    if DEBUG and seg is qsegs[0] and qb == 0 and l == layers[0]:
                        o = 0
                        k.dma("sp", dbg16[:, 0:8 * n].rearrange("p (c n) -> p c n", n=n), hT[:, :, 15:15 + n], reads=[BhT]); o += 8 * n
                        for lst in (oaT, obT, ocT, qTA, qTC):
                            for (t_, B_) in lst:
                                k.dma("sp", dbg16[:, o:o + n], t_[:, :n], reads=[B_]); o += n
                        k.dma("sp", dbg16[:, o:o + 8 * n].rearrange("p (c n) -> p c n", n=n), mT[:, :, :], reads=[BmT])
                        k.dma("sp", dbg32[:, 0:8 * EXT].rearrange("p (c n) -> p c n", n=EXT), xe[:, :, :], reads=[Bxe])
                        k.dma("sp", dbg32[:, 8 * EXT:12 * EXT].rearrange("p (c n) -> p c n", n=EXT), yx[:, :, :], reads=[Byx])
                        k.dma("sp", dbg32[:, 12 * EXT:12 * EXT + 4 * n].rearrange("p (c n) -> p c n", n=n), cacc[:, :, :], reads=Bcacc)

        def ffn_phase(ws, l, tsegs, moe, final, dst, dname):
            cv = Carver()
            nslot = 6
            slots = [(cv.take(8192, BF16, "ws%d" % i), Buf("fws%d" % i)) for i in range(nslot)]
            ws.new_phase(slots, 2)
            NT = sum(s["n"] for s in tsegs)
            acc = cv.take(8 * NT * 4, F32, "acc").rearrange("p (c n) -> p c n", n=NT)
            h2T = cv.take(8 * NT * 2, BF16, "h2T").rearrange("p (c n) -> p c n", n=NT)
            nblk256 = NT // 256
            Bacc = [Buf("acc%d" % i) for i in range(nblk256)]
            Bh2 = [Buf("h2_%d" % i) for i in range(nblk256)]
            T = {}
            T["sq"] = Ring([(cv.take(512 * 4, F32, "sq"), Buf("fsq%d" % i)) for i in range(2)])
            T["tmp"] = Ring([(cv.take(512 * 4, F32, "tmp"), Buf("ftmp%d" % i)) for i in range(3)])
            T["rs"] = Ring([(cv.take(512 * 4, F32, "rs"), Buf("frs%d" % i)) for i in range(2)])
            hid = Ring([(cv.take(4 * 512 * 2, BF16, "hid").rearrange("p (c n) -> p c n", n=512), Buf("hid%d" % i))
                        for i in range(2)])
            T["pp"] = Ring(banks[0:2])
            pa = Ring(banks[0:2]); pb = Ring(banks[2:4]); py = Ring(banks[4:8])
            if moe:
                h2f = cv.take(8 * 256 * 4, F32, "h2f").rearrange("p (c n) -> p c n", n=256); Bh2f = Buf("h2f")
                gT = cv.take(NT * 4, F32, "gT"); BgT = Buf("gT")
                Ge = cv.take(NT * 4, F32, "Ge"); BGe = Buf("Ge")
                wr = cv.take(8 * 8 * 4, F32, "wr").rearrange("p (c n) -> p c n", n=8); Bwr = Buf("wr")
                sm = cv.take(64 * 4, F32, "sm"); Bsm = Buf("sm")
                k.dma("sp", wr, fm(moe_r[0]), writes=[Bwr])
            blocks = []
            lc = 0
            for s in tsegs:
                for j in range(s["n"] // 256):
                    blocks.append((lc, s, s["c0"] + j * 256, None if s["dcol"] is None else s["dcol"] + j * 256))
                    lc += 256
            xmv = fm(xm)
            for bi, (lc, s, c0, dcol) in enumerate(blocks):
                which = s["which"]
                k.dma("sp", acc[:, :, lc:lc + 256], xmv[:, :, c0:c0 + 256], reads=DB("xm_scr", c0, c0 + 256), writes=[Bacc[bi]])
                norm_block(T, acc[:, :, lc:lc + 256], Bacc[bi], 256, lambda c: DV(l, which, 3, c), lambda c: DV(l, which, 4, c),
                           h2T[:, :, lc:lc + 256], Bh2[bi], out2=h2f if moe else None, Bout2=Bh2f if moe else None)
                if moe:
                    for sbk in range(2):
                        bk, Bk = T["pp"].next()
                        for c in range(8):
                            mm(bk[:, 0:8], h2f[:, c, sbk * 128:(sbk + 1) * 128], wr[:, c, :], c == 0, c == 7, [Bh2f, Bwr], Bk)
                        lg = sm[:, 0:8]; m1 = sm[:, 8:9]; eq = sm[:, 16:24]; l2 = sm[:, 24:32]; m2 = sm[:, 9:10]
                        sel = sm[:, 32:40]; ex = sm[:, 40:48]; nm1 = sm[:, 10:11]; ssum = sm[:, 11:12]; gg = sm[:, 48:56]
                        R, W = [Bsm], [Bsm]
                        k.op("dve", lambda e: e.tensor_tensor(lg, bk[:, 0:8], VGl("rb", 0, 8), ALU.add), reads=[Bk, Bvec], writes=W)
                        k.op("dve", lambda e: e.reduce_max(m1, lg, AX.X), reads=R, writes=W)
                        k.op("dve", lambda e: e.tensor_scalar(eq, lg, m1, None, ALU.is_equal), reads=R, writes=W)
                        k.op("dve", lambda e: e.scalar_tensor_tensor(l2, eq, -1.0e30, lg, ALU.mult, ALU.add), reads=R, writes=W)
                        k.op("dve", lambda e: e.reduce_max(m2, l2, AX.X), reads=R, writes=W)
                        k.op("dve", lambda e: e.tensor_scalar(sel, lg, m2, None, ALU.is_ge), reads=R, writes=W)
                        k.op("dve", lambda e: e.tensor_scalar(nm1, m1, -1.0, None, ALU.mult), reads=R, writes=W)
                        k.op("act", lambda e: e.activation(ex, lg, AF.Exp, bias=nm1), reads=R, writes=W)
                        k.op("dve", lambda e: e.tensor_tensor(gg, sel, ex, ALU.mult), reads=R, writes=W)
                        k.op("dve", lambda e: e.reduce_sum(ssum, gg, AX.X), reads=R, writes=W)
                        k.op("dve", lambda e: e.reciprocal(ssum, ssum), reads=R, writes=W)
                        k.op("dve", lambda e: e.tensor_scalar(gg, gg, ssum, None, ALU.mult), reads=R, writes=W)
                        bt, Bbt = T["pp"].next()
                        k.op("pe", lambda e: e.transpose(bt[0:8, 0:128], gg, ident), reads=[Bsm, Bcst], writes=[Bbt])
                        cc = lc + sbk * 128
                        k.op("act", lambda e: e.activation(gT[0:8, cc:cc + 128], bt[0:8, 0:128], AF.Copy), reads=[Bbt], writes=[BgT])
            tbl = []
            lc = 0
            for s in tsegs:
                o = 0
                while o < s["n"]:
                    w = min(512, s["n"] - o)
                    tbl.append((lc + o, w, s["which"]))
                    o += w
                lc += s["n"]
            nexp = N_EXP if moe else 1
            dff = D_FFE if moe else D_FF
            work = []
            for ex_i in range(nexp):
                f0 = 0
                while f0 < dff:
                    fw = min(512, dff - f0)
                    for ti, (lc, w, which) in enumerate(tbl):
                        work.append((ex_i, f0, fw, ti, lc, w, which))
                    f0 += fw
            wstate = {}

            def stage_a(item):
                ex_i, f0, fw, ti, lc, w, which = item
                nfc = fw // 128
                if ti == 0:
                    if moe:
                        if f0 == 0:
                            for (lc_, w_, which_) in tbl:
                                bk, Bk = py.next()
                                mm(bk[:, :w_], cst[0:8, C_OH + ex_i * 128:C_OH + (ex_i + 1) * 128], gT[0:8, lc_:lc_ + w_], True, True,
                                   [BgT, Bcst], Bk)
                                k.op("act", lambda e: e.activation(Ge[:, lc_:lc_ + w_], bk[:, :w_], AF.Copy), reads=[Bk],
                                     writes=[BGe])
                        w1v, w3v, w2v = fm(moe_w1[0, ex_i]), fm(moe_w3[0, ex_i]), fm(moe_w2[0, ex_i])
                    else:
                        w1v, w3v, w2v = fm(ffn_w1[0]), fm(ffn_w3[0]), fm(ffn_w2[0])
                    w1t, Bw1 = ws.get(w1v[:, :, f0:f0 + fw], 8, fw)
                    w3t, Bw3 = ws.get(w3v[:, :, f0:f0 + fw], 8, fw)
                    w2t, Bw2 = ws.get(w2v[:, f0 // 128:f0 // 128 + nfc, :], nfc, 1024)
                    wstate["w"] = (w1t, Bw1, w3t, Bw3, w2t, Bw2)
                w1t, Bw1, w3t, Bw3, w2t, Bw2 = wstate["w"]
                bis = list(range(lc // 256, (lc + w) // 256))
                hd, Bhd = hid.next()
                for fc in range(nfc):
                    (ba, Ba), (bb, Bb) = pa.next(), pb.next()
                    for c in range(8):
                        mm(ba[:, :w], w1t[:, c, fc * 128:(fc + 1) * 128], h2T[:, c, lc:lc + w], c == 0, c == 7,
                           [Bw1] + [Bh2[i] for i in bis], Ba)
                    for c in range(8):
                        mm(bb[:, :w], w3t[:, c, fc * 128:(fc + 1) * 128], h2T[:, c, lc:lc + w], c == 0, c == 7,
                           [Bw3] + [Bh2[i] for i in bis], Bb)
                    sa, Bsa = T["tmp"].next()
                    k.op("act", lambda e: e.activation(sa[:, :w], ba[:, :w], AF.Silu), reads=[Ba], writes=[Bsa])
                    if moe:
                        k.op("dve", lambda e: e.tensor_tensor(sa[:, :w], sa[:, :w], bb[:, :w], ALU.mult),
                             reads=[Bsa, Bb], writes=[Bsa])
                        k.op("pool", lambda e: e.tensor_tensor(hd[:, fc, :w], sa[:, :w], Ge[:, lc:lc + w], ALU.mult),
                             reads=[Bsa, BGe], writes=[Bhd])
                    else:
                        k.op("dve", lambda e: e.tensor_tensor(hd[:, fc, :w], sa[:, :w], bb[:, :w], ALU.mult),
                             reads=[Bsa, Bb], writes=[Bhd])
                return (hd, Bhd, w2t, Bw2, nfc, lc, w, which, bis)

            def stage_b(ctx_):
                hd, Bhd, w2t, Bw2, nfc, lc, w, which, bis = ctx_
                for nn in range(8):
                    by, By = py.next()
                    for fc in range(nfc):
                        mm(by[:, :w], w2t[:, fc, nn * 128:(nn + 1) * 128], hd[:, fc, :w], fc == 0, fc == nfc - 1,
                           [Bw2, Bhd], By)
                    k.op("dve", lambda e: e.scalar_tensor_tensor(acc[:, nn, lc:lc + w], by[:, :w], DV(l, which, 5, nn),
                                                                 acc[:, nn, lc:lc + w], ALU.mult, ALU.add),
                         reads=[By, Bdv] + [Bacc[i] for i in bis], writes=[Bacc[i] for i in bis])

            prev_ = None
            for item in work:
                cur_ = stage_a(item)
                if prev_ is not None:
                    stage_b(prev_)
                prev_ = cur_
            stage_b(prev_)
            if final:
                k.barrier()
                Bstg = Buf("stg")
            for bi, (lc, s, c0, dcol) in enumerate(blocks):
                if dcol is None:
                    continue
                if final:
                    stg = h2f
                    norm_block(T, acc[:, :, lc:lc + 256], Bacc[bi], 256, lambda c: VGl("fing", c), None, stg, Bstg)
                    k.dma("sp", fm(dst)[:, :, dcol:dcol + 256], stg, reads=[Bstg], writes=DB(dname, dcol, dcol + 256))
                else:
                    k.dma("sp", fm(dst)[:, :, dcol:dcol + 256], acc[:, :, lc:lc + 256], reads=[Bacc[bi]],
                          writes=DB(dname, dcol, dcol + 256))

        all_keys = list(range(NKB // 2))
        ctx_keys = [SEQ // 256]
        seg_own = dict(c0=0, n=HALF, which=0, hl_col=SEQ - 15, hl_mask=0, hr_col=HALF, hr_mask=1, keys=all_keys)
        seg_oth = dict(c0=HALF, n=HALF, which=0, hl_col=HALF - 15, hl_mask=2, hr_col=0, hr_mask=3, keys=all_keys)
        seg_ctx = dict(c0=SEQ, n=CTX, which=1, hl_col=SEQ, hl_mask=4, hr_col=SEQ, hr_mask=4, keys=ctx_keys)

        def whole(ws):
            k.op("act", lambda e: e.activation(csb[:], VGl("cfm", 0, 16), AF.Silu), reads=[Bvec], writes=[Bcsb])
            cvm = Carver()
            ws.new_phase([(cvm.take(8192, BF16, "mws%d" % i), Buf("mws%d" % i)) for i in range(3)], 2)
            for l in layers:
                mod_phase(ws, l)
            for li, l in enumerate(layers):
                k.barrier()
                src, sname = (xT, "xT") if li == 0 else (xb, "xb_scr")
                lastl = l == DEPTH - 1
                if lastl:
                    qsegs = [seg_own]
                elif fused and len(layers) > 1:
                    qsegs = [seg_own, seg_oth, seg_ctx]
                else:
                    qsegs = [seg_own, seg_ctx]
                mixer_phase(ws, l, src, sname, qsegs)
                if lastl:
                    sbs = [[dict(c0=0, n=HALF, which=0, dcol=0)]]
                    dst, dname = outT, "outT"
                elif fused and len(layers) > 1:
                    sbs = [[dict(c0=0, n=HALF, which=0, dcol=0)],
                           [dict(c0=HALF, n=HALF, which=0, dcol=HALF), dict(c0=SEQ, n=CTX, which=1, dcol=SEQ)]]
                    dst, dname = xb, "xb_scr"
                else:
                    sbs = [[dict(c0=0, n=HALF, which=0, dcol=0), dict(c0=SEQ, n=CTX, which=1, dcol=HALF)]]
                    dst, dname = outT, "outT"
                for sb_ in sbs:
                    k.barrier()
                    ffn_phase(ws, l, sb_, moe=(l % 2 == 1), final=lastl, dst=dst, dname=dname)
            k.finish()

        ws = WStream(k, pf=2)
        ws.scr = wsc
        k.dry = True
        whole(ws)
        k.dry = False
        ws.rewind()
        whole(ws)
        print("program: nins=%d nwait=%d" % (k.nins, k.nwait))
    return nc


def _fm(v):
    v = np.asarray(v, np.float32)
    return np.ascontiguousarray(v.reshape(-1, 128).T)


def _qk_perm(nunits):
    cols = []
    for g0 in range(0, nunits, 4):
        us = list(range(g0, min(g0 + 4, nunits)))
        for half in range(2):
            for u in us:
                for axis in range(2):
                    for f in range(16):
                        cols.append(u * 64 + axis * 32 + half * 16 + f)
    return np.array(cols)


def _consts():
    c = np.zeros((128, NCONST), np.float32)
    c[:, C_ONES:C_ONES + 128] = 1.0
    for b in range(4):
        c[32 * b:32 * b + 32, C_BD + 32 * b:C_BD + 32 * b + 32] = 1.0
    c[64, C_SEL:C_SEL + 64] = 1.0
    c[:, C_ID:C_ID + 128] = np.eye(128, dtype=np.float32)
    for e in range(8):
        c[e, C_OH + e * 128:C_OH + (e + 1) * 128] = 1.0
    return c


def _tables(tok_pos):
    tok_pos = np.asarray(tok_pos)
    n_freq = 16
    inv = (10000.0 ** (-np.arange(n_freq, dtype=np.float32) / n_freq)).astype(np.float32)
    row = (tok_pos // 64).astype(np.float32)
    col = (tok_pos % 64).astype(np.float32)
    cos = np.ones((128, len(tok_pos)), np.float32)
    sin = np.zeros((128, len(tok_pos)), np.float32)
    valid = tok_pos >= 0
    for p in range(128):
        axis = (p % 32) // 16
        f = p % 16
        pos = row if axis == 0 else col
        ang = (pos * inv[f]).astype(np.float32)
        cos[p, valid] = np.cos(ang[valid])
        sin[p, valid] = np.sin(ang[valid])
    return cos, sin


def _prep_shared(inp):
    sh = {}
    w_in = np.asarray(inp["w_in"], np.float32)
    perm = np.arange(6400)
    perm[OFF_AQ:OFF_AQ + 512] = OFF_AQ + _qk_perm(8)
    perm[OFF_AK:OFF_AK + 128] = OFF_AK + _qk_perm(2)
    perm[OFF_CQ:OFF_CQ + 512] = OFF_CQ + _qk_perm(8)
    perm[OFF_CK:OFF_CK + 512] = OFF_CK + _qk_perm(8)
    sh["w_in"] = np.ascontiguousarray(w_in[:, :, perm])
    for nme in ("w_mod", "w_pa", "w_pb", "w_pc", "w_out", "ffn_w1", "ffn_w3", "ffn_w2", "moe_router", "moe_w1", "moe_w3",
                "moe_w2"):
        sh[nme] = np.ascontiguousarray(np.asarray(inp[nme], np.float32))
    sh["consts"] = _consts()
    return sh


def _vecs(inp, b, hf):
    v = np.zeros((128, NV), np.float32)
    p = np.arange(128)
    gidx = ((p % 32) // 16) * 32 + (p % 16)
    for l in range(DEPTH):
        o = l * VLN

        def put(name, arr):
            a, n = VL[name]
            v[:, o + a:o + a + n] = arr.reshape(128, n)
        put("b_mod", _fm(inp["b_mod"][l]))
        put("n1g", _fm(inp["norm1_g"][l]))
        put("n2g", _fm(inp["norm2_g"][l]))
        put("b_gate", _fm(inp["b_gate"][l]))
        qg = np.asarray(inp["a_qn_g"][l], np.float32)
        kg = np.asarray(inp["a_kn_g"][l], np.float32)
        put("qng", np.stack([qg[gidx], qg[gidx + 16]], axis=1))
        put("kng", np.stack([kg[gidx], kg[gidx + 16]], axis=1))
        dw = np.asarray(inp["b_dw_w"][l], np.float32)
        put("dww", np.ascontiguousarray(dw.T.reshape(4, 128, 31).transpose(1, 0, 2)))
        put("dwb", _fm(inp["b_dw_b"][l]))
        put("lng", _fm(inp["b_ln_g"][l]))
        put("lnb", _fm(inp["b_ln_b"][l]))
        put("subg", _fm(inp["c_subln_g"][l]))
        lv = np.concatenate([np.asarray(inp[n_][l], np.float32) for n_ in ("c_lq1", "c_lk1", "c_lq2", "c_lk2")])
        put("lamv", np.broadcast_to(lv[None, :], (128, 256)))
    a, n = VG["hmask"]
    hm = np.array([0, 1, 1, 0, 0] if hf == 0 else [1, 0, 0, 1, 0], np.float32)
    v[:, a:a + n] = hm[None, :]
    a, n = VG["fing"]
    v[:, a:a + n] = _fm(inp["final_g"])
    a, n = VG["cfm"]
    cf = np.stack([_fm(inp["c"][b]), _fm(inp["c_ctx"])], axis=2)
    v[:, a:a + n] = cf.reshape(128, 16)
    a, n = VG["rb"]
    v[:, a:a + n] = np.broadcast_to(np.asarray(inp["moe_router_b"][0], np.float32)[None, :], (128, 8))
    return v


def _core_order(hf):
    own = np.arange(hf * HALF, (hf + 1) * HALF)
    oth = np.arange((1 - hf) * HALF, (2 - hf) * HALF)
    return own, oth


_PROG_CACHE = {}


def _get_prog(layers, fused):
    key = (tuple(layers), fused)
    if key not in _PROG_CACHE:
        _PROG_CACHE[key] = build_program(list(layers), fused)
    return _PROG_CACHE[key]


def _run(layers, fused, inp, sh, xT_cores):
    in_maps = []
    for core in range(8):
        b, hf = core // 2, core % 2
        own, oth = _core_order(hf)
        pos = np.concatenate([own, oth, -np.ones(CTX, np.int64)])
        cos, sin = _tables(pos)
        m = dict(xT=xT_cores[core], cosk=cos, sink=sin, vecs=_vecs(inp, b, hf), consts=sh["consts"])
        for nme in ("w_mod", "w_in", "w_pa", "w_pb", "w_pc", "w_out"):
            m[nme] = sh[nme]
        if 0 in layers:
            for nme in ("ffn_w1", "ffn_w3", "ffn_w2"):
                m[nme] = sh[nme]
        if 1 in layers:
            for nme in ("moe_router", "moe_w1", "moe_w3", "moe_w2"):
                m[nme] = sh[nme]
        in_maps.append(m)
    nc = _get_prog(layers, fused)
    res = run_bass_kernel_spmd(nc, in_maps, core_ids=list(range(8)))
    return res.results


def kernel(**inp):
    x = np.asarray(inp["x"], np.float32)
    ctx = np.asarray(inp["ctx"], np.float32)
    sh = _prep_shared(inp)
    xT_cores = []
    for core in range(8):
        b, hf = core // 2, core % 2
        own, oth = _core_order(hf)
        xt = np.concatenate([x[b][own], x[b][oth], ctx[b]], axis=0)
        xT_cores.append(np.ascontiguousarray(xt.T))
    if FUSED:
        res = _run([0, 1], True, inp, sh, xT_cores)
    else:
        r0 = _run([0], False, inp, sh, xT_cores)
        xT1 = []
        for core in range(8):
            mate = core ^ 1
            xT1.append(np.ascontiguousarray(np.concatenate(
                [r0[core]["x1T"][:, :HALF], r0[mate]["x1T"][:, :HALF], r0[core]["x1T"][:, HALF:]], axis=1)))
        res = _run([1], False, inp, sh, xT1)
    out = np.zeros((4, SEQ, D), np.float32)
    for core in range(8):
        b, hf = core // 2, core % 2
        out[b, hf * HALF:(hf + 1) * HALF, :] = res[core]["outT"].T
    return out
```

```python
import math
import numpy as np
from contextlib import ExitStack
import concourse.bass as bass
import concourse.mybir as mybir
from concourse.bass_utils import run_bass_kernel_spmd

F32 = mybir.dt.float32
BF16 = mybir.dt.bfloat16
AF = mybir.ActivationFunctionType
ALU = mybir.AluOpType
AX = mybir.AxisListType

D = 1024
SEQ = 4096
HALF = 2048
CTX = 256
NTOK = SEQ + CTX
DEPTH = 2
EPS = 1e-6
QB = 256
EXT = QB + 30
NKB = NTOK // 128
D_FF = 2816
N_EXP = 8
D_FFE = 3584
OFF_AQ, OFF_AK, OFF_AV, OFF_BZ, OFF_CQ, OFF_CK, OFF_CV, OFF_GT = 0, 512, 640, 768, 1792, 2304, 2816, 3328
FUSED = True
DEBUG = False
ARENA = 99500

VL = dict(b_mod=(0, 48), n1g=(48, 8), n2g=(56, 8), b_gate=(64, 24), qng=(88, 2), kng=(90, 2), dww=(92, 124),
          dwb=(216, 4), lng=(220, 4), lnb=(224, 4), subg=(228, 1), lamv=(229, 256))
VLN = 485
VG = dict(hmask=(2 * VLN, 5), fing=(2 * VLN + 5, 8), cfm=(2 * VLN + 13, 16), rb=(2 * VLN + 29, 8))
NV = 2 * VLN + 37
C_ONES, C_BD, C_SEL, C_ID, C_OH = 0, 128, 256, 320, 448
NCONST = 448 + 1024


class Buf:
    __slots__ = ("name", "w", "r")

    def __init__(self, name):
        self.name = name
        self.w = None
        self.r = {}


class Ev:
    __slots__ = ("sem", "val", "clk")

    def __init__(self, sem, val, clk):
        self.sem = sem
        self.val = val
        self.clk = clk


class K:
    ENGS = ("pe", "act", "dve", "pool", "sp")

    def __init__(self, nc, stack, n_dma_sems=32):
        self.nc = nc
        self.stack = stack
        self.e = dict(pe=nc.tensor, act=nc.scalar, dve=nc.vector, pool=nc.gpsimd, sp=nc.sync)
        self.sem, self.cnt, self.clk = {}, {}, {}
        for k in self.ENGS:
            self.sem[k] = stack.enter_context(nc.semaphore("s_" + k))
            self.cnt[k] = 0
            self.clk[k] = {}
        self.dsem = [stack.enter_context(nc.semaphore("d%d" % i)) for i in range(n_dma_sems)]
        self.dcnt = [0] * n_dma_sems
        self.dlast = [None] * n_dma_sems
        self.dnext = 0
        self.nwait = 0
        self.nins = 0
        self.dry = False

    def sb(self, name, shape, dt):
        return self.stack.enter_context(self.nc.sbuf_tensor(name, list(shape), dt))

    def ps(self, name, shape, dt=F32):
        return self.stack.enter_context(self.nc.psum_tensor(name, list(shape), dt))

    def _need(self, eng, ev):
        if ev is None:
            return
        c = self.clk[eng]
        if c.get(ev.sem, 0) >= ev.val:
            return
        if eng == "pe" and ev.sem is self.sem["pe"]:
            return
        self.e[eng].wait_ge(ev.sem, ev.val)
        self.nwait += 1
        for s, v in ev.clk.items():
            if c.get(s, 0) < v:
                c[s] = v
        if c.get(ev.sem, 0) < ev.val:
            c[ev.sem] = ev.val

    def _deps(self, eng, reads, writes):
        for b in reads:
            self._need(eng, b.w)
        for b in writes:
            self._need(eng, b.w)
            for ev in b.r.values():
                self._need(eng, ev)

    def _commit(self, ev, reads, writes):
        for b in writes:
            b.w = ev
            b.r = {}
        for b in reads:
            if b.w is ev:
                continue
            b.r[ev.sem] = ev

    def op(self, eng, fn, reads=(), writes=()):
        if self.dry:
            return None
        self._deps(eng, reads, writes)
        ins = fn(self.e[eng])
        self.cnt[eng] += 1
        ins.then_inc(self.sem[eng], 1)
        self.nins += 1
        clk = dict(self.clk[eng])
        clk[self.sem[eng]] = self.cnt[eng]
        ev = Ev(self.sem[eng], self.cnt[eng], clk)
        self._commit(ev, reads, writes)
        return ev

    def dma(self, eng, out, in_, reads=(), writes=(), **kw):
        if self.dry:
            return None
        i = self.dnext
        self.dnext = (self.dnext + 1) % len(self.dsem)
        self._need(eng, self.dlast[i])
        self._deps(eng, reads, writes)
        ins = self.e[eng].dma_start(out=out, in_=in_, **kw)
        self.dcnt[i] += 16
        ins.then_inc(self.dsem[i], 16)
        self.nins += 1
        clk = dict(self.clk[eng])
        clk[self.dsem[i]] = self.dcnt[i]
        ev = Ev(self.dsem[i], self.dcnt[i], clk)
        self.dlast[i] = ev
        self._commit(ev, reads, writes)
        return ev

    def barrier(self, engs=None):
        if self.dry:
            return
        for e in (engs or self.ENGS):
            for f in self.ENGS:
                if f != e and self.cnt[f] > 0:
                    self._need(e, Ev(self.sem[f], self.cnt[f], {}))
            for ev in self.dlast:
                self._need(e, ev)

    def finish(self):
        self.barrier(engs=("sp",))


class Ring:
    def __init__(self, items):
        self.items = items
        self.i = 0

    def next(self):
        it = self.items[self.i % len(self.items)]
        self.i += 1
        return it


class WStream:
    def __init__(self, k, pf):
        self.k = k
        self.pf = pf
        self.reqs = []
        self.issued = 0
        self.pos = 0
        self.phase = 0
        self.pstart = 0
        self.slots = None
        self.scr = None
        self.sidx = {}
        self.sbuf = {}
        self.done = set()

    def rewind(self):
        self.issued = 0
        self.pos = 0
        self.phase = 0
        self.pstart = 0
        self.pend = {}
        for j, r in enumerate(self.reqs):
            self.pend[r[3]] = j + 1
        self.done = set()

    def new_phase(self, slots, pf):
        self.phase += 1
        self.pf = pf
        self.slots = slots
        self.pstart = self.pos
        assert self.issued <= self.pos or self.k.dry

    def _issue(self, j):
        ap, kc, n, ph, key = self.reqs[j]
        assert ph == self.phase
        t, B = self.slots[(j - self.pstart) % len(self.slots)]
        view = t[:, 0:kc * n].rearrange("p (c n) -> p c n", n=n)
        if key is None:
            self.k.dma("pool", view, ap, writes=[B])
            return
        if key not in self.sidx:
            self.sidx[key] = len(self.sidx)
            self.sbuf[key] = Buf("wsc_%d" % self.sidx[key])
        SB = self.sbuf[key]
        sap = self.scr[self.sidx[key]][:, 0:kc * n]
        if key in self.done:
            self.k.dma("sp", t[:, 0:kc * n], sap, reads=[SB], writes=[B])
        else:
            self.k.dma("pool", view, ap, writes=[B])
            self.k.dma("sp", sap, t[:, 0:kc * n], reads=[B], writes=[SB])
            self.done.add(key)

    def get(self, ap, kc, n, key=None):
        if self.k.dry:
            self.reqs.append((ap, kc, n, self.phase, key))
            self.pos += 1
            t, B = self.slots[0]
            return t[:, 0:kc * n].rearrange("p (c n) -> p c n", n=n), B
        j = self.pos
        self.pos += 1
        lim = min(self.pend[self.phase], j + 1 + self.pf)
        while self.issued < lim:
            self._issue(self.issued)
            self.issued += 1
        t, B = self.slots[(j - self.pstart) % len(self.slots)]
        return t[:, 0:kc * n].rearrange("p (c n) -> p c n", n=n), B


def build_program(layers, fused):
    nc = bass.Bass("TRN2", target_bir_lowering=False)
    dt = nc.dram_tensor
    I = {}

    def inp(name, shape):
        I[name] = dt(name, list(shape), F32, kind="ExternalInput").ap()
        return I[name]

    xT = inp("xT", [D, NTOK])
    cosk = inp("cosk", [128, NTOK])
    sink = inp("sink", [128, NTOK])
    vecs = inp("vecs", [128, NV])
    consts = inp("consts", [128, NCONST])
    w_mod = inp("w_mod", [DEPTH, D, 6 * D])
    w_in = inp("w_in", [DEPTH, D, 6400])
    w_pa = inp("w_pa", [DEPTH, 512, D])
    w_pb = inp("w_pb", [DEPTH, 512, D])
    w_pc = inp("w_pc", [DEPTH, 512, D])
    w_out = inp("w_out", [DEPTH, D, D])
    if 0 in layers:
        ffn_w1 = inp("ffn_w1", [1, D, D_FF])
        ffn_w3 = inp("ffn_w3", [1, D, D_FF])
        ffn_w2 = inp("ffn_w2", [1, D_FF, D])
    if 1 in layers:
        moe_r = inp("moe_router", [1, D, N_EXP])
        moe_w1 = inp("moe_w1", [1, N_EXP, D, D_FFE])
        moe_w3 = inp("moe_w3", [1, N_EXP, D, D_FFE])
        moe_w2 = inp("moe_w2", [1, N_EXP, D_FFE, D])
    last = layers[-1] == DEPTH - 1
    if last:
        outT = dt("outT", [D, HALF], F32, kind="ExternalOutput").ap()
    else:
        outT = dt("x1T", [D, HALF + CTX], F32, kind="ExternalOutput").ap()
    if DEBUG:
        dbg16 = dt("dbg16", [128, 36 * QB], BF16, kind="ExternalOutput").ap()
        dbg32 = dt("dbg32", [128, 16 * EXT], F32, kind="ExternalOutput").ap()
    xm = dt("xm_scr", [D, NTOK], F32).ap()
    xb = dt("xb_scr", [D, NTOK], F32).ap()
    wsc = dt("wsc_scr", [DEPTH * 44, 128, 4096], BF16).ap()

    dbuf = {}

    def DB(name, c0, c1):
        out = []
        for c in range(c0 // 128, (c1 + 127) // 128):
            key = (name, c)
            if key not in dbuf:
                dbuf[key] = Buf("%s_%d" % key)
            out.append(dbuf[key])
        return out

    def fm(ap2d):
        return ap2d.rearrange("(c p) n -> p c n", p=128)

    with ExitStack() as st:
        k = K(nc, st)
        vec = k.sb("vec", [128, NV], F32); Bvec = Buf("vec")
        cst = k.sb("cst", [128, NCONST], F32); Bcst = Buf("cst")
        ones16 = k.sb("ones16", [128, 128], BF16); Bo16 = Buf("ones16")
        modT = k.sb("modT", [128, DEPTH * 96], F32); Bmod = Buf("modT")
        dv = k.sb("dv", [128, DEPTH * 2 * 6 * 8], F32); Bdv = Buf("dv")
        lamt = k.sb("lamt", [128, DEPTH * 8 + 64], F32); Blam = Buf("lamt")
        csb = k.sb("csb", [128, 16], BF16); Bcsb = Buf("csb")
        arena = k.sb("arena", [128, ARENA], BF16)
        banks = [(k.ps("bank%d" % i, [128, 512]), Buf("bank%d" % i)) for i in range(8)]

        k.dma("sp", vec[:], vecs, writes=[Bvec])
        k.dma("sp", cst[:], consts, writes=[Bcst])
        k.op("dve", lambda e: e.memset(ones16[:], 1.0), writes=[Bo16])
        ones32 = cst[:, C_ONES:C_ONES + 128]
        bd32 = cst[:, C_BD:C_BD + 128]
        sel65 = cst[:, C_SEL:C_SEL + 64]
        ident = cst[:, C_ID:C_ID + 128]

        def V(l, name, j=0, n=1):
            o, ln = VL[name]
            return vec[:, l * VLN + o + j: l * VLN + o + j + n]

        def VGl(name, j=0, n=1):
            o, ln = VG[name]
            return vec[:, o + j: o + j + n]

        def DV(l, which, kind, c):
            o = ((l * 2 + which) * 6 + kind) * 8 + c
            return dv[:, o:o + 1]

        class Carver:
            def __init__(self):
                self.off = 0

            def take(self, nbytes, dtype, name):
                nb = (nbytes + 63) // 64 * 64
                a = arena[:, self.off // 2:(self.off + nb) // 2]
                self.off += nb
                assert self.off <= ARENA * 2, (name, self.off)
                if dtype is F32:
                    a = a.bitcast(F32)
                    return a[:, 0:nbytes // 4]
                return a[:, 0:nbytes // 2]

        epsc = k.sb("epsc", [128, 1], F32); Beps = Buf("epsc")
        fz = k.sb("fz", [128, 1], F32); Bfz = Buf("fz")
        k.op("dve", lambda e: e.memset(epsc[:], EPS), writes=[Beps])

        def rsqrt_from(out, Bout, src_, Bsrc, scale, P):
            k.op("act", lambda e: e.activation(out, src_, AF.Ln, bias=epsc[0:P, :], scale=scale), reads=[Bsrc, Beps],
                 writes=[Bout])
            k.op("act", lambda e: e.activation(out, out, AF.Exp, scale=-0.5), reads=[Bout], writes=[Bout])

        def mm(bank_ap, lhsT, rhs, start, stop, reads, Bbank, skip=False):
            k.op("pe", lambda e: e.matmul(bank_ap, lhsT, rhs, start=start, stop=stop, skip_group_check=skip), reads=reads,
                 writes=[Bbank])

        def mod_phase(ws, l):
            bank, Bb = banks[0]
            for g in range(12):
                wt, Bw = ws.get(fm(w_mod[l])[:, :, g * 512:(g + 1) * 512], 8, 512)
                for nl in range(4):
                    j = g * 4 + nl
                    for c in range(8):
                        mm(bank[:, 2 * j:2 * j + 2], wt[:, c, nl * 128:(nl + 1) * 128], csb[:, 2 * c:2 * c + 2],
                           c == 0, c == 7, [Bw, Bcsb], Bb)
            mv = modT[:, l * 96:(l + 1) * 96].rearrange("p (j w) -> p j w", w=2)
            bv = bank[:, 0:96].rearrange("p (j w) -> p j w", w=2)
            for w in range(2):
                k.op("dve", lambda e: e.tensor_tensor(mv[:, :, w], bv[:, :, w], V(l, "b_mod", 0, 48), ALU.add),
                     reads=[Bb, Bvec], writes=[Bmod])
            for w in range(2):
                for sub, (sh, sc, gt, ng) in enumerate(((0, 1, 2, "n1g"), (3, 4, 5, "n2g"))):
                    o = ((l * 2 + w) * 6 + sub * 3) * 8
                    k.op("dve", lambda e: e.scalar_tensor_tensor(dv[:, o:o + 8], mv[:, sc * 8:(sc + 1) * 8, w], 1.0,
                                                                 V(l, ng, 0, 8), ALU.add, ALU.mult),
                         reads=[Bmod, Bvec], writes=[Bdv])
                    k.op("dve", lambda e: e.tensor_copy(dv[:, o + 8:o + 16], mv[:, sh * 8:(sh + 1) * 8, w]),
                         reads=[Bmod], writes=[Bdv])
                    k.op("dve", lambda e: e.tensor_copy(dv[:, o + 16:o + 24], mv[:, gt * 8:(gt + 1) * 8, w]),
                         reads=[Bmod], writes=[Bdv])
            lam_init = 0.8 - 0.6 * math.exp(-0.3 * l)
            lo = l * 8
            tmp = lamt[:, DEPTH * 8:DEPTH * 8 + 64]
            for j in range(2):
                k.op("dve", lambda e: e.tensor_tensor(tmp, V(l, "lamv", j * 128, 64), V(l, "lamv", j * 128 + 64, 64),
                                                      ALU.mult), reads=[Bvec], writes=[Blam])
                k.op("dve", lambda e: e.reduce_sum(lamt[:, lo + j:lo + j + 1], tmp, AX.X), reads=[Blam], writes=[Blam])
            k.op("act", lambda e: e.activation(lamt[:, lo + 2:lo + 4], lamt[:, lo:lo + 2], AF.Exp), reads=[Blam],
                 writes=[Blam])
            k.op("dve", lambda e: e.tensor_tensor(lamt[:, lo + 4:lo + 5], lamt[:, lo + 3:lo + 4], lamt[:, lo + 2:lo + 3],
                                                  ALU.subtract), reads=[Blam], writes=[Blam])
            k.op("dve", lambda e: e.tensor_scalar(lamt[:, lo + 5:lo + 6], lamt[:, lo + 4:lo + 5], -lam_init, None,
                                                  ALU.add), reads=[Blam], writes=[Blam])
            k.op("dve", lambda e: e.tensor_scalar(lamt[:, lo + 6:lo + 7], V(l, "subg"), 1.0 - lam_init, None, ALU.mult),
                 reads=[Bvec], writes=[Blam])

        def norm_block(T, xv, Bx, n, Acol, Bcol, out, Bout, out2=None, Bout2=None):
            bank, Bb = T["pp"].next()
            for c in range(8):
                sq, Bs = T["sq"].next()
                k.op("act", lambda e: e.activation(sq[:, :n], xv[:, c, :], AF.Square), reads=[Bx], writes=[Bs])
                mm(bank[:, :n], ones32, sq[:, :n], c == 0, c == 7, [Bs, Bcst], Bb)
            rs, Br = T["rs"].next()
            rsqrt_from(rs[:, :n], Br, bank[:, :n], Bb, 1.0 / D, 128)
            for c in range(8):
                if Bcol is None:
                    k.op("dve", lambda e: e.scalar_tensor_tensor(out[:, c, :], xv[:, c, :], Acol(c), rs[:, :n], ALU.mult,
                                                                 ALU.mult), reads=[Bx, Br, Bdv, Bvec], writes=[Bout])
                    continue
                tmp, Bt = T["tmp"].next()
                k.op("dve", lambda e: e.scalar_tensor_tensor(tmp[:, :n], xv[:, c, :], Acol(c), rs[:, :n], ALU.mult,
                                                             ALU.mult), reads=[Bx, Br, Bdv], writes=[Bt])
                k.op("act", lambda e: e.activation(out[:, c, :], tmp[:, :n], AF.Identity, bias=Bcol(c)),
                     reads=[Bt, Bdv], writes=[Bout])
                if out2 is not None:
                    k.op("act", lambda e: e.activation(out2[:, c, :], tmp[:, :n], AF.Identity, bias=Bcol(c)),
                         reads=[Bt, Bdv], writes=[Bout2])

        def rope_pair(T, bA, BA, bB, BB, P, n, cs, sn, Bcs, norm_g, dests):
            if norm_g is not None:
                gA, gB = norm_g
                sA, BsA = T["sq"].next()
                sB, BsB = T["sq"].next()
                k.op("act", lambda e: e.activation(sA[0:P, :n], bA[0:P, :n], AF.Square), reads=[BA], writes=[BsA])
                k.op("act", lambda e: e.activation(sB[0:P, :n], bB[0:P, :n], AF.Square), reads=[BB], writes=[BsB])
                bank, Bb = T["pp"].next()
                mm(bank[0:P, :n], bd32[0:P, 0:P], sA[0:P, :n], True, False, [BsA, Bcst], Bb)
                mm(bank[0:P, :n], bd32[0:P, 0:P], sB[0:P, :n], False, True, [BsB, Bcst], Bb)
                rs, Br = T["rs"].next()
                rsqrt_from(rs[0:P, :n], Br, bank[0:P, :n], Bb, 1.0 / 64, P)
                nA, BnA = T["tmp"].next()
                nB, BnB = T["tmp"].next()
                k.op("dve", lambda e: e.scalar_tensor_tensor(nA[0:P, :n], bA[0:P, :n], gA[0:P], rs[0:P, :n], ALU.mult,
                                                             ALU.mult), reads=[BA, Br, Bvec], writes=[BnA])
                k.op("dve", lambda e: e.scalar_tensor_tensor(nB[0:P, :n], bB[0:P, :n], gB[0:P], rs[0:P, :n], ALU.mult,
                                                             ALU.mult), reads=[BB, Br, Bvec], writes=[BnB])
                srcA, BsrcA, srcB, BsrcB = nA, BnA, nB, BnB
            else:
                srcA, BsrcA, srcB, BsrcB = bA, BA, bB, BB
            t1, B1 = T["rp"].next()
            t2, B2 = T["rp"].next()
            t3, B3 = T["rp"].next()
            t4, B4 = T["rp"].next()
            k.op("dve", lambda e: e.tensor_tensor(t1[0:P, :n], srcA[0:P, :n], cs[0:P, :n], ALU.mult),
                 reads=[BsrcA, Bcs], writes=[B1])
            k.op("dve", lambda e: e.tensor_tensor(t2[0:P, :n], srcB[0:P, :n], sn[0:P, :n], ALU.mult),
                 reads=[BsrcB, Bcs], writes=[B2])
            k.op("dve", lambda e: e.tensor_tensor(t3[0:P, :n], srcB[0:P, :n], cs[0:P, :n], ALU.mult),
                 reads=[BsrcB, Bcs], writes=[B3])
            k.op("dve", lambda e: e.tensor_tensor(t4[0:P, :n], srcA[0:P, :n], sn[0:P, :n], ALU.mult),
                 reads=[BsrcA, Bcs], writes=[B4])
            for (slo, dl) in dests:
                for (dtile, Bd, dlo) in dl:
                    k.op("pool", lambda e: e.tensor_tensor(dtile[dlo:dlo + 32, :n], t1[slo:slo + 32, :n],
                                                           t2[slo:slo + 32, :n], ALU.subtract),
                         reads=[B1, B2], writes=[Bd])
                    k.op("pool", lambda e: e.tensor_tensor(dtile[dlo + 32:dlo + 64, :n], t3[slo:slo + 32, :n],
                                                           t4[slo:slo + 32, :n], ALU.add),
                         reads=[B3, B4], writes=[Bd])

        def mixer_phase(ws, l, src, sname, qsegs):
            cv = Carver()
            slots = [(cv.take(8192, BF16, "ws%d" % i), Buf("ws%d" % i)) for i in range(3)]
            kTA = [(cv.take(NTOK * 2, BF16, "kTA"), Buf("kTA%d" % i)) for i in range(2)]
            kTC = [(cv.take(NTOK * 2, BF16, "kTC"), Buf("kTC%d" % i)) for i in range(4)]
            VAf = cv.take(NKB * 2 * 65 * 2, BF16, "VA")
            VA = VAf.rearrange("p (b v d) -> p b v d", v=2, d=65); BVA = Buf("VA")
            VC = cv.take(NKB * 512 * 2, BF16, "VC").rearrange("p (b n) -> p b n", n=512); BVC = Buf("VC")
            T = {}
            T["xe"] = Ring([(cv.take(8 * EXT * 4, F32, "xe").rearrange("p (c n) -> p c n", n=EXT), Buf("xe%d" % i))
                            for i in range(1)])
            hT = cv.take(8 * EXT * 2, BF16, "hT").rearrange("p (c n) -> p c n", n=EXT); BhT = Buf("hT")
            cs_t = cv.take(EXT * 4, F32, "cs"); sn_t = cv.take(EXT * 4, F32, "sn"); Bcs = Buf("cs")
            u0 = cv.off
            yx = cv.take(4 * EXT * 4, F32, "yx").rearrange("p (c n) -> p c n", n=EXT); Byx = Buf("yx")
            cacc = cv.take(4 * QB * 4, F32, "cacc").rearrange("p (c n) -> p c n", n=QB); Bcacc = [Buf("cacc%d" % i) for i in range(4)]
            osb_l = [(cv.take(512 * 4, F32, "osb"), Buf("osb%d" % i)) for i in range(2)]
            om_l = [(cv.take(512 * 4, F32, "om"), Buf("om%d" % i)) for i in range(2)]
            assert cv.off - u0 >= 8 * EXT * 6 and u0 % 64 == 0
            xs = arena[:, u0 // 2:(u0 + 8 * EXT * 4) // 2].bitcast(F32).rearrange("p (c n) -> p c n", n=EXT); Bxs = Buf("xs")
            hs = arena[:, (u0 + 8 * EXT * 4) // 2:(u0 + 8 * EXT * 6) // 2].rearrange("p (c n) -> p c n", n=EXT); Bhs = Buf("hs")
            ALIAS = [Byx] + Bcacc + [b_ for (_, b_) in osb_l + om_l]
            qTA = [(cv.take(2 * QB * 2, BF16, "qTA"), Buf("qTA%d" % i)) for i in range(4)]
            qTC = [(cv.take(2 * QB * 2, BF16, "qTC"), Buf("qTC%d" % i)) for i in range(4)]
            oaT = [(cv.take(QB * 2, BF16, "oaT"), Buf("oaT%d" % i)) for i in range(4)]
            obT = [(cv.take(QB * 2, BF16, "obT"), Buf("obT%d" % i)) for i in range(4)]
            ocT = [(cv.take(QB * 2, BF16, "ocT"), Buf("ocT%d" % i)) for i in range(4)]
            mT = cv.take(8 * QB * 2, BF16, "mT").rearrange("p (c n) -> p c n", n=QB); BmT = Buf("mT")
            macc = cv.take(8 * QB * 4, F32, "macc").rearrange("p (c n) -> p c n", n=QB); Bmacc = Buf("macc")
            T["sq"] = Ring([(cv.take(EXT * 4, F32, "sq"), Buf("sq%d" % i)) for i in range(2)])
            T["tmp"] = Ring([(cv.take(EXT * 4, F32, "tmp"), Buf("tmp%d" % i)) for i in range(3)])
            T["rs"] = Ring([(cv.take(EXT * 4, F32, "rs"), Buf("rs%d" % i)) for i in range(2)])
            ri_l = [(cv.take(512 * 4, F32, "ri"), Buf("ri%d" % i)) for i in range(2)]
            T["ri"] = Ring(ri_l)
            T["rp"] = Ring([(ri_l[i // 2][0][:, (i % 2) * QB:(i % 2 + 1) * QB], ri_l[i // 2][1]) for i in range(4)])
            T["pT"] = Ring([(cv.take(512 * 2, BF16, "pT"), Buf("pT%d" % i)) for i in range(4)])
            T["osb"] = Ring(osb_l)
            T["om"] = Ring(om_l)
            T["dd"] = Ring([(cv.take(QB * 4, F32, "dd"), Buf("dd%d" % i)) for i in range(2)])
            T["pp"] = Ring(banks[0:8])
            wl = fm(w_in[l])
            srcv = fm(src)

            k.op("pool", lambda e: e.memset(VA[:, :, :, 64:65], 1.0), writes=[BVA])
            for (t_, B_) in qTA + qTC:
                k.op("pool", lambda e: e.memset(t_[:, :], 0.0), writes=[B_])

            wkv = [(slots[0][0][:, 0:2048].rearrange("p (c n) -> p c n", n=256), Buf("wkv_akv"), wl[:, :, OFF_AK:OFF_AK + 256]),
                   (slots[0][0][:, 2048:4096].rearrange("p (c n) -> p c n", n=256), Buf("wkv_ck0"), wl[:, :, OFF_CK:OFF_CK + 256]),
                   (slots[1][0][:, 0:2048].rearrange("p (c n) -> p c n", n=256), Buf("wkv_ck1"),
                    wl[:, :, OFF_CK + 256:OFF_CK + 512]),
                   (slots[2][0][:, 0:4096].rearrange("p (c n) -> p c n", n=512), Buf("wkv_cv"), wl[:, :, OFF_CV:OFF_CV + 512])]
            for (wt_, Bw_, ap_) in wkv:
                k.dma("pool", wt_, ap_, writes=[Bw_])
            n = QB
            xe0, Bxe0 = T["xe"].items[0]
            xring = [(xe0[:, :, 0:n], Bxe0), (macc[:, :, :], Buf("xe_kv2"))]
            hring = [(hT[:, :, 0:n], BhT), (mT[:, :, :], Buf("hT_kv2"))]
            csring = [(cs_t, sn_t, Bcs), (yx[:, 0, :], yx[:, 1, :], Buf("cs_kv2"))]
            NKV = NTOK // QB

            def kv_load(kb):
                c0 = kb * QB
                xv_, Bx_ = xring[kb % 2]
                k.dma("sp", xv_, srcv[:, :, c0:c0 + n], reads=DB(sname, c0, c0 + n), writes=[Bx_])
                cs_, sn_, Bc_ = csring[kb % 2]
                k.dma("sp", cs_[:, 0:n], cosk[:, c0:c0 + n], writes=[Bc_])
                k.dma("sp", sn_[:, 0:n], sink[:, c0:c0 + n], writes=[Bc_])

            def kv_norm(kb):
                which = 1 if kb * QB >= SEQ else 0
                xv_, Bx_ = xring[kb % 2]
                hv_, Bh_ = hring[kb % 2]
                norm_block(T, xv_, Bx_, n, lambda c: DV(l, which, 0, c), lambda c: DV(l, which, 1, c), hv_, Bh_)

            kv_load(0)
            kv_norm(0)
            for kb in range(NKV):
                c0 = kb * QB
                hv, Bh = hring[kb % 2]
                cs_k, sn_k, Bcs_k = csring[kb % 2]
                if kb + 1 < NKV:
                    kv_load(kb + 1)
                wt, Bw, _ = wkv[0]
                (bA, BA), (bB, BB) = T["pp"].next(), T["pp"].next()
                for part, (bk, Bk) in enumerate(((bA, BA), (bB, BB))):
                    for c in range(8):
                        mm(bk[0:64, :n], wt[:, c, part * 64:(part + 1) * 64], hv[:, c, :], c == 0, c == 7, [Bw, Bh], Bk)
                dests = []
                for kvh in range(2):
                    t, Bt = kTA[kvh]
                    dests.append((32 * kvh, [(t[:, c0:c0 + n], Bt, 0), (t[:, c0:c0 + n], Bt, 64)]))
                rope_pair(T, bA, BA, bB, BB, 64, n, cs_k, sn_k, Bcs_k, (V(l, "kng", 0), V(l, "kng", 1)), dests)
                for sbk in range(n // 128):
                    bk, Bk = T["pp"].next()
                    for c in range(8):
                        mm(bk[:, 0:128], hv[:, c, sbk * 128:(sbk + 1) * 128], wt[:, c, 128:256], c == 0, c == 7, [Bw, Bh], Bk)
                    blk = c0 // 128 + sbk
                    k.op("act", lambda e: e.activation(VA[:, blk, :, 0:64], bk[:, 0:128].rearrange("p (v d) -> p v d", d=64),
                                                       AF.Copy), reads=[Bk], writes=[BVA])
                for pr in range(2):
                    wt, Bw, _ = wkv[1 + pr]
                    (bA, BA), (bB, BB) = T["pp"].next(), T["pp"].next()
                    for part, (bk, Bk) in enumerate(((bA, BA), (bB, BB))):
                        for c in range(8):
                            mm(bk[:, :n], wt[:, c, part * 128:(part + 1) * 128], hv[:, c, :], c == 0, c == 7, [Bw, Bh], Bk)
                    dests = []
                    for ul in range(4):
                        u = pr * 4 + ul
                        t, Bt = kTC[u // 2]
                        dests.append((32 * ul, [(t[:, c0:c0 + n], Bt, 64 * (u % 2))]))
                    rope_pair(T, bA, BA, bB, BB, 128, n, cs_k, sn_k, Bcs_k, None, dests)
                    if pr == 0 and kb + 1 < NKV:
                        kv_norm(kb + 1)
                wt, Bw, _ = wkv[3]
                for sbk in range(n // 128):
                    bk, Bk = T["pp"].next()
                    for c in range(8):
                        mm(bk[:, 0:512], hv[:, c, sbk * 128:(sbk + 1) * 128], wt[:, c, :], c == 0, c == 7, [Bw, Bh], Bk)
                    blk = c0 // 128 + sbk
                    k.op("act", lambda e: e.activation(VC[:, blk, :], bk[:, 0:512], AF.Copy), reads=[Bk], writes=[BVC])

            k.barrier()
            qslots = [(slots[i // 2][0][:, (i % 2) * 2048:(i % 2 + 1) * 2048], Buf("qws%d" % i)) for i in range(6)]
            ws.new_phase(qslots, 4)
            for seg in qsegs:
                which = seg["which"]
                nblk = seg["n"] // QB
                for qb in range(nblk):
                    c0 = seg["c0"] + qb * QB
                    n = QB
                    xe, Bxe = T["xe"].next()

                    def load_x(dst, Bdst, qb_):
                        c0_ = seg["c0"] + qb_ * QB
                        if 0 < qb_ < nblk - 1:
                            k.dma("sp", dst[:, :, :], srcv[:, :, c0_ - 15:c0_ + n + 15], reads=DB(sname, c0_ - 15, c0_ + n + 15),
                                  writes=[Bdst])
                            return
                        k.dma("sp", dst[:, :, 15:15 + n], srcv[:, :, c0_:c0_ + n], reads=DB(sname, c0_, c0_ + n), writes=[Bdst])
                        lcol = seg["hl_col"] if qb_ == 0 else c0_ - 15
                        rcol = seg["hr_col"] if qb_ == nblk - 1 else c0_ + n
                        k.dma("sp", dst[:, :, 0:15], srcv[:, :, lcol:lcol + 15], reads=DB(sname, lcol, lcol + 15), writes=[Bdst])
                        k.dma("sp", dst[:, :, 15 + n:30 + n], srcv[:, :, rcol:rcol + 15], reads=DB(sname, rcol, rcol + 15),
                              writes=[Bdst])

                    if qb == 0:
                        load_x(xe, Bxe, qb)
                    k.dma("sp", cs_t[:, 0:n], cosk[:, c0:c0 + n], writes=[Bcs])
                    k.dma("sp", sn_t[:, 0:n], sink[:, c0:c0 + n], writes=[Bcs])
                    if qb == 0:
                        norm_block(T, xe, Bxe, EXT, lambda c: DV(l, which, 0, c), lambda c: DV(l, which, 1, c), hT, BhT)
                    hm = hT[:, :, 15:15 + n]
                    for j in range(4):
                        if j % 2 == 0:
                            jj = j // 2
                            wa, Bwa = ws.get(wl[:, :, OFF_BZ + jj * 256:OFF_BZ + (jj + 1) * 256], 8, 256, key=(l, "bza", jj))
                            wg, Bwg = ws.get(wl[:, :, OFF_BZ + 512 + jj * 256:OFF_BZ + 512 + (jj + 1) * 256], 8, 256,
                                             key=(l, "bzg", jj))
                        j2 = j % 2
                        (ba, Ba), (bg, Bg) = T["pp"].next(), T["pp"].next()
                        for c in range(8):
                            mm(ba[:, :EXT], wa[:, c, j2 * 128:(j2 + 1) * 128], hT[:, c, :], c == 0, c == 7, [Bwa, BhT], Ba)
                        for c in range(8):
                            mm(bg[:, :EXT], wg[:, c, j2 * 128:(j2 + 1) * 128], hT[:, c, :], c == 0, c == 7, [Bwg, BhT], Bg)
                        sg, Bsg = T["tmp"].next()
                        k.op("act", lambda e: e.activation(sg[:, :EXT], bg[:, :EXT], AF.Sigmoid), reads=[Bg], writes=[Bsg])
                        k.op("dve", lambda e: e.tensor_tensor(yx[:, j, :], ba[:, :EXT], sg[:, :EXT], ALU.mult),
                             reads=[Ba, Bsg], writes=[Byx])
                    if qb == 0:
                        mcol = VGl("hmask", seg["hl_mask"])
                        k.op("pool", lambda e: e.tensor_scalar(yx[:, :, 0:15], yx[:, :, 0:15], mcol, None, ALU.mult),
                             reads=[Byx, Bvec], writes=[Byx])
                    if qb == nblk - 1:
                        mcol = VGl("hmask", seg["hr_mask"])
                        k.op("pool", lambda e: e.tensor_scalar(yx[:, :, 15 + n:30 + n], yx[:, :, 15 + n:30 + n], mcol, None,
                                                               ALU.mult), reads=[Byx, Bvec], writes=[Byx])
                    side = []

                    def conv_tap(j, t):
                        if t == 0:
                            k.op("dve", lambda e: e.tensor_scalar(cacc[:, j, :], yx[:, j, 0:n], V(l, "dww", j * 31),
                                                                  V(l, "dwb", j), ALU.mult, ALU.add),
                                 reads=[Byx, Bvec], writes=[Bcacc[j]])
                        else:
                            k.op("dve", lambda e: e.scalar_tensor_tensor(cacc[:, j, :], yx[:, j, t:t + n],
                                                                         V(l, "dww", j * 31 + t), cacc[:, j, :], ALU.mult,
                                                                         ALU.add), reads=[Byx, Bvec, Bcacc[j]], writes=[Bcacc[j]])
                    for t in range(31):
                        for j in range(4):
                            side.append(lambda j=j, t=t: conv_tap(j, t))
                    mean, rstd, m2, d1 = yx[:, 0, 0:n], yx[:, 1, 0:n], yx[:, 2, 0:n], yx[:, 3, 0:n]

                    lnst = {}

                    def ln_sq(j0):
                        if j0 == 0:
                            lnst["bank"] = T["pp"].next()
                        for j in (j0, j0 + 1):
                            sq, Bs = T["sq"].next()
                            lnst[j] = (sq, Bs)
                            k.op("act", lambda e: e.activation(sq[:, :n], cacc[:, j, :], AF.Square), reads=[Bcacc[j]], writes=[Bs])

                    def ln_mm(j0):
                        bfull, B1 = lnst["bank"]
                        b1, b2, B2 = bfull[:, 0:n], bfull[:, n:2 * n], B1
                        for j in (j0, j0 + 1):
                            sq, Bs = lnst[j]
                            mm(b1[:, :n], ones32, cacc[:, j, :], j == 0, j == 3, [Bcacc[j], Bcst], B1, skip=True)
                            mm(b2[:, :n], ones32, sq[:, :n], False, j == 3, [Bs, Bcst], B2, skip=True)

                    def ln_var():
                        bfull, B1 = lnst["bank"]
                        b1, b2, B2 = bfull[:, 0:n], bfull[:, n:2 * n], B1
                        k.op("dve", lambda e: e.tensor_scalar(mean, b1[:, :n], 1.0 / 512, None, ALU.mult),
                             reads=[B1], writes=[Byx])
                        k.op("dve", lambda e: e.tensor_tensor(m2, mean, mean, ALU.mult), reads=[Byx], writes=[Byx])
                        k.op("dve", lambda e: e.scalar_tensor_tensor(rstd, b2[:, :n], 1.0 / 512, m2, ALU.mult,
                                                                     ALU.subtract), reads=[B2, Byx], writes=[Byx])

                    def ln_rs():
                        rsqrt_from(rstd, Byx, rstd, Byx, 1.0, 128)

                    def ln_sub():
                        for j in range(4):
                            k.op("pool", lambda e: e.tensor_tensor(cacc[:, j, :], cacc[:, j, :], mean, ALU.subtract),
                                 reads=[Bcacc[j], Byx], writes=[Bcacc[j]])

                    def ln_mul():
                        for j in range(4):
                            k.op("dve", lambda e: e.tensor_tensor(cacc[:, j, :], cacc[:, j, :], rstd, ALU.mult),
                                 reads=[Bcacc[j], Byx], writes=[Bcacc[j]])

                    def ln_act():
                        for j in range(4):
                            ot, Bot = obT[j]
                            k.op("act", lambda e: e.activation(ot[:, :n], cacc[:, j, :], AF.Silu, bias=V(l, "lnb", j),
                                                               scale=V(l, "lng", j)), reads=[Bcacc[j], Bvec], writes=[Bot])

                    def ln_mm0_sq2():
                        ln_mm(0)
                        ln_sq(2)
                    side += [None] * 21
                    side += [lambda: ln_sq(0), None, ln_mm0_sq2, None, lambda: ln_mm(2), None, None, ln_var, None, None, ln_rs,
                             None, ln_sub, None, None, ln_mul, None, None, ln_act]
                    for (off, qT, ng) in ((OFF_AQ, qTA, (V(l, "qng", 0), V(l, "qng", 1))), (OFF_CQ, qTC, None)):
                        for pr in range(2):
                            wt, Bw = ws.get(wl[:, :, off + pr * 256:off + (pr + 1) * 256], 8, 256, key=(l, "q", off, pr))
                            (bA, BA), (bB, BB) = T["pp"].next(), T["pp"].next()
                            for part, (bk, Bk) in enumerate(((bA, BA), (bB, BB))):
                                for c in range(8):
                                    mm(bk[:, :n], wt[:, c, part * 128:(part + 1) * 128], hm[:, c, :], c == 0, c == 7,
                                       [Bw, BhT], Bk)
                            dests = []
                            for ul in range(4):
                                u = pr * 4 + ul
                                t, Bt = qT[u // 2]
                                dests.append((32 * ul, [(t[:, (u % 2) * n:(u % 2 + 1) * n], Bt, 64 * (u % 2))]))
                            rope_pair(T, bA, BA, bB, BB, 128, n, cs_t, sn_t, Bcs, ng, dests)
                    kbl = [2 * kp + i for kp in seg["keys"] for i in range(2)]
                    NP = len(kbl)
                    n2 = 2 * n
                    units = [("A", hp) for hp in range(4)] + [("C", hc) for hc in range(4)]
                    flat = [(ui, pi) for ui in range(len(units)) for pi in range(NP)]
                    pp_saved = T["pp"]
                    T["pp"] = Ring(banks[0:1]); T["st"] = Ring(banks[1:4]); T["ac"] = Ring(banks[4:8])
                    stq = {}

                    def emit_qk(s_):
                        ui, pi = flat[s_]
                        kind, a_ = units[ui]
                        (qt, Bq) = qTA[a_] if kind == "A" else qTC[a_]
                        (kt, Bkt) = kTA[a_ // 2] if kind == "A" else kTC[a_]
                        stb, Bst = T["st"].next()
                        kbk = kbl[pi]
                        mm(stb[:, 0:n2], kt[:, kbk * 128:(kbk + 1) * 128], qt[:, 0:n2], True, True, [Bkt, Bq], Bst)
                        stq[s_] = (stb, Bst)

                    pend = []

                    def flush(all_=False):
                        for it in pend:
                            it[0] -= 1
                        while pend and (all_ or pend[0][0] <= 0):
                            pend.pop(0)[1]()

                    def epi_A(hp, acc, Bacc):
                        if pend:
                            flush(True)
                        osb, Bosb = T["osb"].next()
                        k.op("dve", lambda e: e.tensor_copy(osb[0:65, 0:n2], acc[0:65, 0:n2]), reads=[Bacc], writes=[Bosb])
                        st_ = {}

                        def p1():
                            rb_, Brb = T["pp"].next()
                            st_["rb"] = (rb_, Brb)
                            mm(rb_[0:64, 0:n2], sel65[0:65, :], osb[0:65, 0:n2], True, True, [Bosb, Bcst], Brb)

                        def p2():
                            rb_, Brb = st_["rb"]
                            ri, Bri = T["ri"].next()
                            k.op("act", lambda e: e.activation(ri[0:64, 0:n2], rb_[0:64, 0:n2], AF.Ln), reads=[Brb], writes=[Bri])
                            k.op("act", lambda e: e.activation(ri[0:64, 0:n2], ri[0:64, 0:n2], AF.Exp, scale=-1.0),
                                 reads=[Bri], writes=[Bri])
                            ot, Bot = oaT[hp]
                            for hh in range(2):
                                k.op("dve", lambda e: e.tensor_tensor(ot[64 * hh:64 * hh + 64, :n], osb[0:64, hh * n:(hh + 1) * n],
                                                                      ri[0:64, hh * n:(hh + 1) * n], ALU.mult),
                                     reads=[Bosb, Bri], writes=[Bot])
                        pend.append([4, p1])
                        pend.append([6, p2])

                    def epi_C(hc, acc, Bacc, rsk, Brsk):
                        if pend:
                            flush(True)
                        st_ = {}

                        def p0():
                            ri, Bri = T["ri"].next()
                            k.op("act", lambda e: e.activation(ri[:, 0:n2], rsk[:, 0:n2], AF.Ln), reads=[Brsk], writes=[Bri])
                            k.op("act", lambda e: e.activation(ri[:, 0:n2], ri[:, 0:n2], AF.Exp, scale=-1.0), reads=[Bri],
                                 writes=[Bri])
                            om, Bom = T["om"].next()
                            k.op("dve", lambda e: e.tensor_tensor(om[:, 0:n2], acc[:, 0:n2], ri[:, 0:n2], ALU.mult),
                                 reads=[Bacc, Bri], writes=[Bom])
                            dd, Bdd = T["dd"].next()
                            st_["dd"] = (dd, Bdd)
                            k.op("dve", lambda e: e.scalar_tensor_tensor(dd[:, :n], om[:, n:n2], lamt[:, l * 8 + 5:l * 8 + 6],
                                                                         om[:, 0:n], ALU.mult, ALU.add),
                                 reads=[Bom, Blam], writes=[Bdd])
                            sq, Bs = T["sq"].next()
                            st_["sq"] = (sq, Bs)
                            k.op("pool", lambda e: e.tensor_tensor(sq[:, :n], dd[:, :n], dd[:, :n], ALU.mult), reads=[Bdd],
                                 writes=[Bs])

                        def p1():
                            sq, Bs = st_["sq"]
                            bk, Bk = T["pp"].next()
                            st_["bk"] = (bk, Bk)
                            mm(bk[:, :n], ones32, sq[:, :n], True, True, [Bs, Bcst], Bk)

                        def p2():
                            bk, Bk = st_["bk"]
                            dd, Bdd = st_["dd"]
                            rs, Br = T["rs"].next()
                            rsqrt_from(rs[:, :n], Br, bk[:, :n], Bk, 1.0 / 128, 128)
                            ot, Bot = ocT[hc]
                            k.op("dve", lambda e: e.scalar_tensor_tensor(ot[:, :n], dd[:, :n], lamt[:, l * 8 + 6:l * 8 + 7],
                                                                         rs[:, :n], ALU.mult, ALU.mult),
                                 reads=[Bdd, Br, Blam], writes=[Bot])
                        pend.append([1, p0])
                        pend.append([4, p1])
                        pend.append([7, p2])

                    SK = 2
                    for s_ in range(min(SK, len(flat))):
                        emit_qk(s_)
                    per_step = (len(side) + max(1, len(flat) - 6) - 1) // max(1, len(flat) - 6)
                    cur = None
                    for s_, (ui, pi) in enumerate(flat):
                        if s_ + SK < len(flat):
                            emit_qk(s_ + SK)
                        kind, a_ = units[ui]
                        stb, Bst = stq.pop(s_)
                        pT, BpT = T["pT"].next()
                        k.op("act", lambda e: e.activation(pT[:, 0:n2], stb[:, 0:n2], AF.Exp, scale=0.125),
                             reads=[Bst], writes=[BpT])
                        if pi == 0:
                            cur = [T["ac"].next()]
                            if kind == "C":
                                cur.append(T["ac"].next())
                        acc, Bacc = cur[0]
                        kbk = kbl[pi]
                        first = pi == 0
                        lastk = pi == NP - 1
                        if kind == "A":
                            mm(acc[0:65, 0:n2], VA[:, kbk, a_ // 2, :], pT[:, 0:n2], first, lastk, [BVA, BpT], Bacc)
                        else:
                            rsk, Brsk = cur[1]
                            mm(acc[:, 0:n2], VC[:, kbk, a_ * 128:(a_ + 1) * 128], pT[:, 0:n2], first, lastk, [BVC, BpT], Bacc)
                            mm(rsk[:, 0:n2], ones16[:], pT[:, 0:n2], first, lastk, [Bo16, BpT], Brsk)
                        flush()
                        if pi == NP - 1:
                            if kind == "A":
                                epi_A(a_, acc, Bacc)
                            else:
                                epi_C(a_, acc, Bacc, cur[1][0], cur[1][1])
                        for _ in range(per_step):
                            if side:
                                f_ = side.pop(0)
                                if f_ is not None:
                                    f_()
                    flush(True)
                    while side:
                        f_ = side.pop(0)
                        if f_ is not None:
                            f_()
                    T["pp"] = pp_saved
                    staged = qb + 1 < nblk
                    if staged:
                        k.op("pool", lambda e: e.memset(fz[:], 0.0), writes=ALIAS + [Bxs, Bhs, Bfz])
                        load_x(xs, Bxs, qb + 1)
                    for a, (wp, oT) in enumerate(((w_pa, oaT), (w_pb, obT), (w_pc, ocT))):
                        wpv = wp[l].rearrange("(c p) n -> p c n", p=128)
                        if a == 1 and staged:
                            norm_block(T, xs, Bxs, EXT, lambda c: DV(l, which, 0, c), lambda c: DV(l, which, 1, c), hs, Bhs)
                        for ng in range(4):
                            wg, Bwg = ws.get(wl[:, :, OFF_GT + a * 1024 + ng * 256:OFF_GT + a * 1024 + (ng + 1) * 256], 8, 256,
                                             key=(l, "gt", a, ng))
                            wpt, Bwp = ws.get(wpv[:, :, ng * 256:(ng + 1) * 256], 4, 256, key=(l, "wp", a, ng))
                            for nl in range(2):
                                nn = ng * 2 + nl
                                (bg, Bg), (bp, Bp) = T["pp"].next(), T["pp"].next()
                                for c in range(8):
                                    mm(bg[:, :n], wg[:, c, nl * 128:(nl + 1) * 128], hm[:, c, :], c == 0, c == 7, [Bwg, BhT], Bg)
                                for c in range(4):
                                    mm(bp[:, :n], wpt[:, c, nl * 128:(nl + 1) * 128], oT[c][0][:, :n], c == 0, c == 3,
                                       [Bwp, oT[c][1]], Bp)
                                gs, Bgs = T["tmp"].next()
                                k.op("act", lambda e: e.activation(gs[:, :n], bg[:, :n], AF.Sigmoid,
                                                                   bias=V(l, "b_gate", a * 8 + nn)),
                                     reads=[Bg, Bvec], writes=[Bgs])
                                if a == 0:
                                    k.op("dve", lambda e: e.tensor_tensor(macc[:, nn, :], gs[:, :n], bp[:, :n], ALU.mult),
                                         reads=[Bgs, Bp], writes=[Bmacc])
                                else:
                                    k.op("dve", lambda e: e.tensor_tensor(gs[:, :n], gs[:, :n], bp[:, :n], ALU.mult),
                                         reads=[Bgs, Bp], writes=[Bgs])
                                    if a == 1:
                                        k.op("dve", lambda e: e.tensor_tensor(macc[:, nn, :], macc[:, nn, :], gs[:, :n], ALU.add),
                                             reads=[Bgs, Bmacc], writes=[Bmacc])
                                    else:
                                        k.op("dve", lambda e: e.tensor_tensor(mT[:, nn, :], macc[:, nn, :], gs[:, :n], ALU.add),
                                             reads=[Bgs, Bmacc], writes=[BmT])
                    wov = fm(w_out[l])
                    for ng in range(4):
                        wt, Bw = ws.get(wov[:, :, ng * 256:(ng + 1) * 256], 8, 256, key=(l, "wo", ng))
                        for nl in range(2):
                            nn = ng * 2 + nl
                            bk, Bk = T["pp"].next()
                            for c in range(8):
                                mm(bk[:, :n], wt[:, c, nl * 128:(nl + 1) * 128], mT[:, c, :], c == 0, c == 7, [Bw, BmT], Bk)
                            k.op("dve", lambda e: e.scalar_tensor_tensor(macc[:, nn, :], bk[:, :n], DV(l, which, 2, nn),
                                                                         xe[:, nn, 15:15 + n], ALU.mult, ALU.add),
                                 reads=[Bk, Bdv, Bxe], writes=[Bmacc])
                    k.dma("sp", fm(xm)[:, :, c0:c0 + n], macc[:, :, :], reads=[Bmacc], writes=DB("xm_scr", c0, c0 + n))
                    if staged:
                        k.op("dve", lambda e: e.tensor_copy(xe[:, :, :], xs[:, :, :]), reads=[Bxs], writes=[Bxe])
                        k.op("act", lambda e: e.activation(hT[:, :, :], hs[:, :, :], AF.Copy), reads=[Bhs], writes=[BhT])
                        k.op("pool", lambda e: e.memset(fz[:], 0.0), writes=ALIAS + [Bxs, Bhs, Bfz])
                    if DEBUG and seg is qsegs[0] and qb == 0 and l == layers[0]:
                        o = 0
                        k.dma("sp", dbg16[:, 0:8 * n].rearrange("p (c n) -> p c n", n=n), hT[:, :, 15:15 + n], reads=[BhT]); o += 8 * n
                        for lst in (oaT, obT, ocT, qTA, qTC):
                            for (t_, B_) in lst:
                                k.dma("sp", dbg16[:, o:o + n], t_[:, :n], reads=[B_]); o += n
                        k.dma("sp", dbg16[:, o:o + 8 * n].rearrange("p (c n) -> p c n", n=n), mT[:, :, :], reads=[BmT])
                        k.dma("sp", dbg32[:, 0:8 * EXT].rearrange("p (c n) -> p c n", n=EXT), xe[:, :, :], reads=[Bxe])
                        k.dma("sp", dbg32[:, 8 * EXT:12 * EXT].rearrange("p (c n) -> p c n", n=EXT), yx[:, :, :], reads=[Byx])
                        k.dma("sp", dbg32[:, 12 * EXT:12 * EXT + 4 * n].rearrange("p (c n) -> p c n", n=n), cacc[:, :, :], reads=Bcacc)

        def ffn_phase(ws, l, tsegs, moe, final, dst, dname):
            cv = Carver()
            nslot = 6
            slots = [(cv.take(8192, BF16, "ws%d" % i), Buf("fws%d" % i)) for i in range(nslot)]
            ws.new_phase(slots, 2)
            NT = sum(s["n"] for s in tsegs)
            acc = cv.take(8 * NT * 4, F32, "acc").rearrange("p (c n) -> p c n", n=NT)
            h2T = cv.take(8 * NT * 2, BF16, "h2T").rearrange("p (c n) -> p c n", n=NT)
            nblk256 = NT // 256
            Bacc = [Buf("acc%d" % i) for i in range(nblk256)]
            Bh2 = [Buf("h2_%d" % i) for i in range(nblk256)]
            T = {}
            T["sq"] = Ring([(cv.take(512 * 4, F32, "sq"), Buf("fsq%d" % i)) for i in range(2)])
            T["tmp"] = Ring([(cv.take(512 * 4, F32, "tmp"), Buf("ftmp%d" % i)) for i in range(3)])
            T["rs"] = Ring([(cv.take(512 * 4, F32, "rs"), Buf("frs%d" % i)) for i in range(2)])
            hid = Ring([(cv.take(4 * 512 * 2, BF16, "hid").rearrange("p (c n) -> p c n", n=512), Buf("hid%d" % i))
                        for i in range(2)])
            T["pp"] = Ring(banks[0:2])
            pa = Ring(banks[0:2]); pb = Ring(banks[2:4]); py = Ring(banks[4:8])
            if moe:
                h2f = cv.take(8 * 256 * 4, F32, "h2f").rearrange("p (c n) -> p c n", n=256); Bh2f = Buf("h2f")
                gT = cv.take(NT * 4, F32, "gT"); BgT = Buf("gT")
                Ge = cv.take(NT * 4, F32, "Ge"); BGe = Buf("Ge")
                wr = cv.take(8 * 8 * 4, F32, "wr").rearrange("p (c n) -> p c n", n=8); Bwr = Buf("wr")
                sm = cv.take(64 * 4, F32, "sm"); Bsm = Buf("sm")
                k.dma("sp", wr, fm(moe_r[0]), writes=[Bwr])
            blocks = []
            lc = 0
            for s in tsegs:
                for j in range(s["n"] // 256):
                    blocks.append((lc, s, s["c0"] + j * 256, None if s["dcol"] is None else s["dcol"] + j * 256))
                    lc += 256
            xmv = fm(xm)
            for bi, (lc, s, c0, dcol) in enumerate(blocks):
                which = s["which"]
                k.dma("sp", acc[:, :, lc:lc + 256], xmv[:, :, c0:c0 + 256], reads=DB("xm_scr", c0, c0 + 256), writes=[Bacc[bi]])
                norm_block(T, acc[:, :, lc:lc + 256], Bacc[bi], 256, lambda c: DV(l, which, 3, c), lambda c: DV(l, which, 4, c),
                           h2T[:, :, lc:lc + 256], Bh2[bi], out2=h2f if moe else None, Bout2=Bh2f if moe else None)
                if moe:
                    for sbk in range(2):
                        bk, Bk = T["pp"].next()
                        for c in range(8):
                            mm(bk[:, 0:8], h2f[:, c, sbk * 128:(sbk + 1) * 128], wr[:, c, :], c == 0, c == 7, [Bh2f, Bwr], Bk)
                        lg = sm[:, 0:8]; m1 = sm[:, 8:9]; eq = sm[:, 16:24]; l2 = sm[:, 24:32]; m2 = sm[:, 9:10]
                        sel = sm[:, 32:40]; ex = sm[:, 40:48]; nm1 = sm[:, 10:11]; ssum = sm[:, 11:12]; gg = sm[:, 48:56]
                        R, W = [Bsm], [Bsm]
                        k.op("dve", lambda e: e.tensor_tensor(lg, bk[:, 0:8], VGl("rb", 0, 8), ALU.add), reads=[Bk, Bvec], writes=W)
                        k.op("dve", lambda e: e.reduce_max(m1, lg, AX.X), reads=R, writes=W)
                        k.op("dve", lambda e: e.tensor_scalar(eq, lg, m1, None, ALU.is_equal), reads=R, writes=W)
                        k.op("dve", lambda e: e.scalar_tensor_tensor(l2, eq, -1.0e30, lg, ALU.mult, ALU.add), reads=R, writes=W)
                        k.op("dve", lambda e: e.reduce_max(m2, l2, AX.X), reads=R, writes=W)
                        k.op("dve", lambda e: e.tensor_scalar(sel, lg, m2, None, ALU.is_ge), reads=R, writes=W)
                        k.op("dve", lambda e: e.tensor_scalar(nm1, m1, -1.0, None, ALU.mult), reads=R, writes=W)
                        k.op("act", lambda e: e.activation(ex, lg, AF.Exp, bias=nm1), reads=R, writes=W)
                        k.op("dve", lambda e: e.tensor_tensor(gg, sel, ex, ALU.mult), reads=R, writes=W)
                        k.op("dve", lambda e: e.reduce_sum(ssum, gg, AX.X), reads=R, writes=W)
                        k.op("dve", lambda e: e.reciprocal(ssum, ssum), reads=R, writes=W)
                        k.op("dve", lambda e: e.tensor_scalar(gg, gg, ssum, None, ALU.mult), reads=R, writes=W)
                        bt, Bbt = T["pp"].next()
                        k.op("pe", lambda e: e.transpose(bt[0:8, 0:128], gg, ident), reads=[Bsm, Bcst], writes=[Bbt])
                        cc = lc + sbk * 128
                        k.op("act", lambda e: e.activation(gT[0:8, cc:cc + 128], bt[0:8, 0:128], AF.Copy), reads=[Bbt], writes=[BgT])
            tbl = []
            lc = 0
            for s in tsegs:
                o = 0
                while o < s["n"]:
                    w = min(512, s["n"] - o)
                    tbl.append((lc + o, w, s["which"]))
                    o += w
                lc += s["n"]
            nexp = N_EXP if moe else 1
            dff = D_FFE if moe else D_FF
            work = []
            for ex_i in range(nexp):
                f0 = 0
                while f0 < dff:
                    fw = min(512, dff - f0)
                    for ti, (lc, w, which) in enumerate(tbl):
                        work.append((ex_i, f0, fw, ti, lc, w, which))
                    f0 += fw
            wstate = {}

            def stage_a(item):
                ex_i, f0, fw, ti, lc, w, which = item
                nfc = fw // 128
                if ti == 0:
                    if moe:
                        if f0 == 0:
                            for (lc_, w_, which_) in tbl:
                                bk, Bk = py.next()
                                mm(bk[:, :w_], cst[0:8, C_OH + ex_i * 128:C_OH + (ex_i + 1) * 128], gT[0:8, lc_:lc_ + w_], True, True,
                                   [BgT, Bcst], Bk)
                                k.op("act", lambda e: e.activation(Ge[:, lc_:lc_ + w_], bk[:, :w_], AF.Copy), reads=[Bk],
                                     writes=[BGe])
                        w1v, w3v, w2v = fm(moe_w1[0, ex_i]), fm(moe_w3[0, ex_i]), fm(moe_w2[0, ex_i])
                    else:
                        w1v, w3v, w2v = fm(ffn_w1[0]), fm(ffn_w3[0]), fm(ffn_w2[0])
                    w1t, Bw1 = ws.get(w1v[:, :, f0:f0 + fw], 8, fw)
                    w3t, Bw3 = ws.get(w3v[:, :, f0:f0 + fw], 8, fw)
                    w2t, Bw2 = ws.get(w2v[:, f0 // 128:f0 // 128 + nfc, :], nfc, 1024)
                    wstate["w"] = (w1t, Bw1, w3t, Bw3, w2t, Bw2)
                w1t, Bw1, w3t, Bw3, w2t, Bw2 = wstate["w"]
                bis = list(range(lc // 256, (lc + w) // 256))
                hd, Bhd = hid.next()
                for fc in range(nfc):
                    (ba, Ba), (bb, Bb) = pa.next(), pb.next()
                    for c in range(8):
                        mm(ba[:, :w], w1t[:, c, fc * 128:(fc + 1) * 128], h2T[:, c, lc:lc + w], c == 0, c == 7,
                           [Bw1] + [Bh2[i] for i in bis], Ba)
                    for c in range(8):
                        mm(bb[:, :w], w3t[:, c, fc * 128:(fc + 1) * 128], h2T[:, c, lc:lc + w], c == 0, c == 7,
                           [Bw3] + [Bh2[i] for i in bis], Bb)
                    sa, Bsa = T["tmp"].next()
                    k.op("act", lambda e: e.activation(sa[:, :w], ba[:, :w], AF.Silu), reads=[Ba], writes=[Bsa])
                    if moe:
                        k.op("dve", lambda e: e.tensor_tensor(sa[:, :w], sa[:, :w], bb[:, :w], ALU.mult),
                             reads=[Bsa, Bb], writes=[Bsa])
                        k.op("pool", lambda e: e.tensor_tensor(hd[:, fc, :w], sa[:, :w], Ge[:, lc:lc + w], ALU.mult),
                             reads=[Bsa, BGe], writes=[Bhd])
                    else:
                        k.op("dve", lambda e: e.tensor_tensor(hd[:, fc, :w], sa[:, :w], bb[:, :w], ALU.mult),
                             reads=[Bsa, Bb], writes=[Bhd])
                return (hd, Bhd, w2t, Bw2, nfc, lc, w, which, bis)

            def stage_b(ctx_):
                hd, Bhd, w2t, Bw2, nfc, lc, w, which, bis = ctx_
                for nn in range(8):
                    by, By = py.next()
                    for fc in range(nfc):
                        mm(by[:, :w], w2t[:, fc, nn * 128:(nn + 1) * 128], hd[:, fc, :w], fc == 0, fc == nfc - 1,
                           [Bw2, Bhd], By)
                    k.op("dve", lambda e: e.scalar_tensor_tensor(acc[:, nn, lc:lc + w], by[:, :w], DV(l, which, 5, nn),
                                                                 acc[:, nn, lc:lc + w], ALU.mult, ALU.add),
                         reads=[By, Bdv] + [Bacc[i] for i in bis], writes=[Bacc[i] for i in bis])

            prev_ = None
            for item in work:
                cur_ = stage_a(item)
                if prev_ is not None:
                    stage_b(prev_)
                prev_ = cur_
            stage_b(prev_)
            if final:
                k.barrier()
                Bstg = Buf("stg")
            for bi, (lc, s, c0, dcol) in enumerate(blocks):
                if dcol is None:
                    continue
                if final:
                    stg = h2f
                    norm_block(T, acc[:, :, lc:lc + 256], Bacc[bi], 256, lambda c: VGl("fing", c), None, stg, Bstg)
                    k.dma("sp", fm(dst)[:, :, dcol:dcol + 256], stg, reads=[Bstg], writes=DB(dname, dcol, dcol + 256))
                else:
                    k.dma("sp", fm(dst)[:, :, dcol:dcol + 256], acc[:, :, lc:lc + 256], reads=[Bacc[bi]],
                          writes=DB(dname, dcol, dcol + 256))

        all_keys = list(range(NKB // 2))
        ctx_keys = [SEQ // 256]
        seg_own = dict(c0=0, n=HALF, which=0, hl_col=SEQ - 15, hl_mask=0, hr_col=HALF, hr_mask=1, keys=all_keys)
        seg_oth = dict(c0=HALF, n=HALF, which=0, hl_col=HALF - 15, hl_mask=2, hr_col=0, hr_mask=3, keys=all_keys)
        seg_ctx = dict(c0=SEQ, n=CTX, which=1, hl_col=SEQ, hl_mask=4, hr_col=SEQ, hr_mask=4, keys=ctx_keys)

        def whole(ws):
            k.op("act", lambda e: e.activation(csb[:], VGl("cfm", 0, 16), AF.Silu), reads=[Bvec], writes=[Bcsb])
            cvm = Carver()
            ws.new_phase([(cvm.take(8192, BF16, "mws%d" % i), Buf("mws%d" % i)) for i in range(3)], 2)
            for l in layers:
                mod_phase(ws, l)
            for li, l in enumerate(layers):
                k.barrier()
                src, sname = (xT, "xT") if li == 0 else (xb, "xb_scr")
                lastl = l == DEPTH - 1
                if lastl:
                    qsegs = [seg_own]
                elif fused and len(layers) > 1:
                    qsegs = [seg_own, seg_oth, seg_ctx]
                else:
                    qsegs = [seg_own, seg_ctx]
                mixer_phase(ws, l, src, sname, qsegs)
                if lastl:
                    sbs = [[dict(c0=0, n=HALF, which=0, dcol=0)]]
                    dst, dname = outT, "outT"
                elif fused and len(layers) > 1:
                    sbs = [[dict(c0=0, n=HALF, which=0, dcol=0)],
                           [dict(c0=HALF, n=HALF, which=0, dcol=HALF), dict(c0=SEQ, n=CTX, which=1, dcol=SEQ)]]
                    dst, dname = xb, "xb_scr"
                else:
                    sbs = [[dict(c0=0, n=HALF, which=0, dcol=0), dict(c0=SEQ, n=CTX, which=1, dcol=HALF)]]
                    dst, dname = outT, "outT"
                for sb_ in sbs:
                    k.barrier()
                    ffn_phase(ws, l, sb_, moe=(l % 2 == 1), final=lastl, dst=dst, dname=dname)
            k.finish()

        ws = WStream(k, pf=2)
        ws.scr = wsc
        k.dry = True
        whole(ws)
        k.dry = False
        ws.rewind()
        whole(ws)
        print("program: nins=%d nwait=%d" % (k.nins, k.nwait))
    return nc


def _fm(v):
    v = np.asarray(v, np.float32)
    return np.ascontiguousarray(v.reshape(-1, 128).T)


def _qk_perm(nunits):
    cols = []
    for g0 in range(0, nunits, 4):
        us = list(range(g0, min(g0 + 4, nunits)))
        for half in range(2):
            for u in us:
                for axis in range(2):
                    for f in range(16):
                        cols.append(u * 64 + axis * 32 + half * 16 + f)
    return np.array(cols)


def _consts():
    c = np.zeros((128, NCONST), np.float32)
    c[:, C_ONES:C_ONES + 128] = 1.0
    for b in range(4):
        c[32 * b:32 * b + 32, C_BD + 32 * b:C_BD + 32 * b + 32] = 1.0
    c[64, C_SEL:C_SEL + 64] = 1.0
    c[:, C_ID:C_ID + 128] = np.eye(128, dtype=np.float32)
    for e in range(8):
        c[e, C_OH + e * 128:C_OH + (e + 1) * 128] = 1.0
    return c


def _tables(tok_pos):
    tok_pos = np.asarray(tok_pos)
    n_freq = 16
    inv = (10000.0 ** (-np.arange(n_freq, dtype=np.float32) / n_freq)).astype(np.float32)
    row = (tok_pos // 64).astype(np.float32)
    col = (tok_pos % 64).astype(np.float32)
    cos = np.ones((128, len(tok_pos)), np.float32)
    sin = np.zeros((128, len(tok_pos)), np.float32)
    valid = tok_pos >= 0
    for p in range(128):
        axis = (p % 32) // 16
        f = p % 16
        pos = row if axis == 0 else col
        ang = (pos * inv[f]).astype(np.float32)
        cos[p, valid] = np.cos(ang[valid])
        sin[p, valid] = np.sin(ang[valid])
    return cos, sin


def _prep_shared(inp):
    sh = {}
    w_in = np.asarray(inp["w_in"], np.float32)
    perm = np.arange(6400)
    perm[OFF_AQ:OFF_AQ + 512] = OFF_AQ + _qk_perm(8)
    perm[OFF_AK:OFF_AK + 128] = OFF_AK + _qk_perm(2)
    perm[OFF_CQ:OFF_CQ + 512] = OFF_CQ + _qk_perm(8)
    perm[OFF_CK:OFF_CK + 512] = OFF_CK + _qk_perm(8)
    sh["w_in"] = np.ascontiguousarray(w_in[:, :, perm])
    for nme in ("w_mod", "w_pa", "w_pb", "w_pc", "w_out", "ffn_w1", "ffn_w3", "ffn_w2", "moe_router", "moe_w1", "moe_w3",
                "moe_w2"):
        sh[nme] = np.ascontiguousarray(np.asarray(inp[nme], np.float32))
    sh["consts"] = _consts()
    return sh


def _vecs(inp, b, hf):
    v = np.zeros((128, NV), np.float32)
    p = np.arange(128)
    gidx = ((p % 32) // 16) * 32 + (p % 16)
    for l in range(DEPTH):
        o = l * VLN

        def put(name, arr):
            a, n = VL[name]
            v[:, o + a:o + a + n] = arr.reshape(128, n)
        put("b_mod", _fm(inp["b_mod"][l]))
        put("n1g", _fm(inp["norm1_g"][l]))
        put("n2g", _fm(inp["norm2_g"][l]))
        put("b_gate", _fm(inp["b_gate"][l]))
        qg = np.asarray(inp["a_qn_g"][l], np.float32)
        kg = np.asarray(inp["a_kn_g"][l], np.float32)
        put("qng", np.stack([qg[gidx], qg[gidx + 16]], axis=1))
        put("kng", np.stack([kg[gidx], kg[gidx + 16]], axis=1))
        dw = np.asarray(inp["b_dw_w"][l], np.float32)
        put("dww", np.ascontiguousarray(dw.T.reshape(4, 128, 31).transpose(1, 0, 2)))
        put("dwb", _fm(inp["b_dw_b"][l]))
        put("lng", _fm(inp["b_ln_g"][l]))
        put("lnb", _fm(inp["b_ln_b"][l]))
        put("subg", _fm(inp["c_subln_g"][l]))
        lv = np.concatenate([np.asarray(inp[n_][l], np.float32) for n_ in ("c_lq1", "c_lk1", "c_lq2", "c_lk2")])
        put("lamv", np.broadcast_to(lv[None, :], (128, 256)))
    a, n = VG["hmask"]
    hm = np.array([0, 1, 1, 0, 0] if hf == 0 else [1, 0, 0, 1, 0], np.float32)
    v[:, a:a + n] = hm[None, :]
    a, n = VG["fing"]
    v[:, a:a + n] = _fm(inp["final_g"])
    a, n = VG["cfm"]
    cf = np.stack([_fm(inp["c"][b]), _fm(inp["c_ctx"])], axis=2)
    v[:, a:a + n] = cf.reshape(128, 16)
    a, n = VG["rb"]
    v[:, a:a + n] = np.broadcast_to(np.asarray(inp["moe_router_b"][0], np.float32)[None, :], (128, 8))
    return v


def _core_order(hf):
    own = np.arange(hf * HALF, (hf + 1) * HALF)
    oth = np.arange((1 - hf) * HALF, (2 - hf) * HALF)
    return own, oth


_PROG_CACHE = {}


def _get_prog(layers, fused):
    key = (tuple(layers), fused)
    if key not in _PROG_CACHE:
        _PROG_CACHE[key] = build_program(list(layers), fused)
    return _PROG_CACHE[key]


def _run(layers, fused, inp, sh, xT_cores):
    in_maps = []
    for core in range(8):
        b, hf = core // 2, core % 2
        own, oth = _core_order(hf)
        pos = np.concatenate([own, oth, -np.ones(CTX, np.int64)])
        cos, sin = _tables(pos)
        m = dict(xT=xT_cores[core], cosk=cos, sink=sin, vecs=_vecs(inp, b, hf), consts=sh["consts"])
        for nme in ("w_mod", "w_in", "w_pa", "w_pb", "w_pc", "w_out"):
            m[nme] = sh[nme]
        if 0 in layers:
            for nme in ("ffn_w1", "ffn_w3", "ffn_w2"):
                m[nme] = sh[nme]
        if 1 in layers:
            for nme in ("moe_router", "moe_w1", "moe_w3", "moe_w2"):
                m[nme] = sh[nme]
        in_maps.append(m)
    nc = _get_prog(layers, fused)
    res = run_bass_kernel_spmd(nc, in_maps, core_ids=list(range(8)))
    return res.results


def kernel(**inp):
    x = np.asarray(inp["x"], np.float32)
    ctx = np.asarray(inp["ctx"], np.float32)
    sh = _prep_shared(inp)
    xT_cores = []
    for core in range(8):
        b, hf = core // 2, core % 2
        own, oth = _core_order(hf)
        xt = np.concatenate([x[b][own], x[b][oth], ctx[b]], axis=0)
        xT_cores.append(np.ascontiguousarray(xt.T))
    if FUSED:
        res = _run([0, 1], True, inp, sh, xT_cores)
    else:
        r0 = _run([0], False, inp, sh, xT_cores)
        xT1 = []
        for core in range(8):
            mate = core ^ 1
            xT1.append(np.ascontiguousarray(np.concatenate(
                [r0[core]["x1T"][:, :HALF], r0[mate]["x1T"][:, :HALF], r0[core]["x1T"][:, HALF:]], axis=1)))
        res = _run([1], False, inp, sh, xT1)
    out = np.zeros((4, SEQ, D), np.float32)
    for core in range(8):
        b, hf = core // 2, core % 2
        out[b, hf * HALF:(hf + 1) * HALF, :] = res[core]["outT"].T
    return out
```

```python
import math
import numpy as np
from contextlib import ExitStack
import concourse.bass as bass
import concourse.mybir as mybir
from concourse.bass_utils import run_bass_kernel_spmd

F32 = mybir.dt.float32
BF16 = mybir.dt.bfloat16
AF = mybir.ActivationFunctionType
ALU = mybir.AluOpType
AX = mybir.AxisListType

D = 1024
SEQ = 4096
HALF = 2048
CTX = 256
NTOK = SEQ + CTX
DEPTH = 2
EPS = 1e-6
QB = 256
EXT = QB + 30
NKB = NTOK // 128
D_FF = 2816
N_EXP = 8
D_FFE = 3584
OFF_AQ, OFF_AK, OFF_AV, OFF_BZ, OFF_CQ, OFF_CK, OFF_CV, OFF_GT = 0, 512, 640, 768, 1792, 2304, 2816, 3328
FUSED = True
DEBUG = False
ARENA = 99500

VL = dict(b_mod=(0, 48), n1g=(48, 8), n2g=(56, 8), b_gate=(64, 24), qng=(88, 2), kng=(90, 2), dww=(92, 124),
          dwb=(216, 4), lng=(220, 4), lnb=(224, 4), subg=(228, 1), lamv=(229, 256))
VLN = 485
VG = dict(hmask=(2 * VLN, 5), fing=(2 * VLN + 5, 8), cfm=(2 * VLN + 13, 16), rb=(2 * VLN + 29, 8))
NV = 2 * VLN + 37
C_ONES, C_BD, C_SEL, C_ID, C_OH = 0, 128, 256, 320, 448
NCONST = 448 + 1024


class Buf:
    __slots__ = ("name", "w", "r")

    def __init__(self, name):
        self.name = name
        self.w = None
        self.r = {}


class Ev:
    __slots__ = ("sem", "val", "clk")

    def __init__(self, sem, val, clk):
        self.sem = sem
        self.val = val
        self.clk = clk


class K:
    ENGS = ("pe", "act", "dve", "pool", "sp")

    def __init__(self, nc, stack, n_dma_sems=32):
        self.nc = nc
        self.stack = stack
        self.e = dict(pe=nc.tensor, act=nc.scalar, dve=nc.vector, pool=nc.gpsimd, sp=nc.sync)
        self.sem, self.cnt, self.clk = {}, {}, {}
        for k in self.ENGS:
            self.sem[k] = stack.enter_context(nc.semaphore("s_" + k))
            self.cnt[k] = 0
            self.clk[k] = {}
        self.dsem = [stack.enter_context(nc.semaphore("d%d" % i)) for i in range(n_dma_sems)]
        self.dcnt = [0] * n_dma_sems
        self.dlast = [None] * n_dma_sems
        self.dnext = 0
        self.nwait = 0
        self.nins = 0
        self.dry = False

    def sb(self, name, shape, dt):
        return self.stack.enter_context(self.nc.sbuf_tensor(name, list(shape), dt))

    def ps(self, name, shape, dt=F32):
        return self.stack.enter_context(self.nc.psum_tensor(name, list(shape), dt))

    def _need(self, eng, ev):
        if ev is None:
            return
        c = self.clk[eng]
        if c.get(ev.sem, 0) >= ev.val:
            return
        if eng == "pe" and ev.sem is self.sem["pe"]:
            return
        self.e[eng].wait_ge(ev.sem, ev.val)
        self.nwait += 1
        for s, v in ev.clk.items():
            if c.get(s, 0) < v:
                c[s] = v
        if c.get(ev.sem, 0) < ev.val:
            c[ev.sem] = ev.val

    def _deps(self, eng, reads, writes):
        for b in reads:
            self._need(eng, b.w)
        for b in writes:
            self._need(eng, b.w)
            for ev in b.r.values():
                self._need(eng, ev)

    def _commit(self, ev, reads, writes):
        for b in writes:
            b.w = ev
            b.r = {}
        for b in reads:
            if b.w is ev:
                continue
            b.r[ev.sem] = ev

    def op(self, eng, fn, reads=(), writes=()):
        if self.dry:
            return None
        self._deps(eng, reads, writes)
        ins = fn(self.e[eng])
        self.cnt[eng] += 1
        ins.then_inc(self.sem[eng], 1)
        self.nins += 1
        clk = dict(self.clk[eng])
        clk[self.sem[eng]] = self.cnt[eng]
        ev = Ev(self.sem[eng], self.cnt[eng], clk)
        self._commit(ev, reads, writes)
        return ev

    def dma(self, eng, out, in_, reads=(), writes=(), **kw):
        if self.dry:
            return None
        i = self.dnext
        self.dnext = (self.dnext + 1) % len(self.dsem)
        self._need(eng, self.dlast[i])
        self._deps(eng, reads, writes)
        ins = self.e[eng].dma_start(out=out, in_=in_, **kw)
        self.dcnt[i] += 16
        ins.then_inc(self.dsem[i], 16)
        self.nins += 1
        clk = dict(self.clk[eng])
        clk[self.dsem[i]] = self.dcnt[i]
        ev = Ev(self.dsem[i], self.dcnt[i], clk)
        self.dlast[i] = ev
        self._commit(ev, reads, writes)
        return ev

    def barrier(self, engs=None):
        if self.dry:
            return
        for e in (engs or self.ENGS):
            for f in self.ENGS:
                if f != e and self.cnt[f] > 0:
                    self._need(e, Ev(self.sem[f], self.cnt[f], {}))
            for ev in self.dlast:
                self._need(e, ev)

    def finish(self):
        self.barrier(engs=("sp",))


class Ring:
    def __init__(self, items):
        self.items = items
        self.i = 0

    def next(self):
        it = self.items[self.i % len(self.items)]
        self.i += 1
        return it


class WStream:
    def __init__(self, k, pf):
        self.k = k
        self.pf = pf
        self.reqs = []
        self.issued = 0
        self.pos = 0
        self.phase = 0
        self.pstart = 0
        self.slots = None
        self.scr = None
        self.sidx = {}
        self.sbuf = {}
        self.done = set()

    def rewind(self):
        self.issued = 0
        self.pos = 0
        self.phase = 0
        self.pstart = 0
        self.pend = {}
        for j, r in enumerate(self.reqs):
            self.pend[r[3]] = j + 1
        self.done = set()

    def new_phase(self, slots, pf):
        self.phase += 1
        self.pf = pf
        self.slots = slots
        self.pstart = self.pos
        assert self.issued <= self.pos or self.k.dry

    def _issue(self, j):
        ap, kc, n, ph, key = self.reqs[j]
        assert ph == self.phase
        t, B = self.slots[(j - self.pstart) % len(self.slots)]
        view = t[:, 0:kc * n].rearrange("p (c n) -> p c n", n=n)
        if key is None:
            self.k.dma("pool", view, ap, writes=[B])
            return
        if key not in self.sidx:
            self.sidx[key] = len(self.sidx)
            self.sbuf[key] = Buf("wsc_%d" % self.sidx[key])
        SB = self.sbuf[key]
        sap = self.scr[self.sidx[key]][:, 0:kc * n]
        if key in self.done:
            self.k.dma("sp", t[:, 0:kc * n], sap, reads=[SB], writes=[B])
        else:
            self.k.dma("pool", view, ap, writes=[B])
            self.k.dma("sp", sap, t[:, 0:kc * n], reads=[B], writes=[SB])
            self.done.add(key)

    def get(self, ap, kc, n, key=None):
        if self.k.dry:
            self.reqs.append((ap, kc, n, self.phase, key))
            self.pos += 1
            t, B = self.slots[0]
            return t[:, 0:kc * n].rearrange("p (c n) -> p c n", n=n), B
        j = self.pos
        self.pos += 1
        lim = min(self.pend[self.phase], j + 1 + self.pf)
        while self.issued < lim:
            self._issue(self.issued)
            self.issued += 1
        t, B = self.slots[(j - self.pstart) % len(self.slots)]
        return t[:, 0:kc * n].rearrange("p (c n) -> p c n", n=n), B


def build_program(layers, fused):
    nc = bass.Bass("TRN2", target_bir_lowering=False)
    dt = nc.dram_tensor
    I = {}

    def inp(name, shape):
        I[name] = dt(name, list(shape), F32, kind="ExternalInput").ap()
        return I[name]

    xT = inp("xT", [D, NTOK])
    cosk = inp("cosk", [128, NTOK])
    sink = inp("sink", [128, NTOK])
    vecs = inp("vecs", [128, NV])
    consts = inp("consts", [128, NCONST])
    w_mod = inp("w_mod", [DEPTH, D, 6 * D])
    w_in = inp("w_in", [DEPTH, D, 6400])
    w_pa = inp("w_pa", [DEPTH, 512, D])
    w_pb = inp("w_pb", [DEPTH, 512, D])
    w_pc = inp("w_pc", [DEPTH, 512, D])
    w_out = inp("w_out", [DEPTH, D, D])
    if 0 in layers:
        ffn_w1 = inp("ffn_w1", [1, D, D_FF])
        ffn_w3 = inp("ffn_w3", [1, D, D_FF])
        ffn_w2 = inp("ffn_w2", [1, D_FF, D])
    if 1 in layers:
        moe_r = inp("moe_router", [1, D, N_EXP])
        moe_w1 = inp("moe_w1", [1, N_EXP, D, D_FFE])
        moe_w3 = inp("moe_w3", [1, N_EXP, D, D_FFE])
        moe_w2 = inp("moe_w2", [1, N_EXP, D_FFE, D])
    last = layers[-1] == DEPTH - 1
    if last:
        outT = dt("outT", [D, HALF], F32, kind="ExternalOutput").ap()
    else:
        outT = dt("x1T", [D, HALF + CTX], F32, kind="ExternalOutput").ap()
    if DEBUG:
        dbg16 = dt("dbg16", [128, 36 * QB], BF16, kind="ExternalOutput").ap()
        dbg32 = dt("dbg32", [128, 16 * EXT], F32, kind="ExternalOutput").ap()
    xm = dt("xm_scr", [D, NTOK], F32).ap()
    xb = dt("xb_scr", [D, NTOK], F32).ap()
    wsc = dt("wsc_scr", [DEPTH * 44, 128, 4096], BF16).ap()

    dbuf = {}

    def DB(name, c0, c1):
        out = []
        for c in range(c0 // 128, (c1 + 127) // 128):
            key = (name, c)
            if key not in dbuf:
                dbuf[key] = Buf("%s_%d" % key)
            out.append(dbuf[key])
        return out

    def fm(ap2d):
        return ap2d.rearrange("(c p) n -> p c n", p=128)

    with ExitStack() as st:
        k = K(nc, st)
        vec = k.sb("vec", [128, NV], F32); Bvec = Buf("vec")
        cst = k.sb("cst", [128, NCONST], F32); Bcst = Buf("cst")
        ones16 = k.sb("ones16", [128, 128], BF16); Bo16 = Buf("ones16")
        modT = k.sb("modT", [128, DEPTH * 96], F32); Bmod = Buf("modT")
        dv = k.sb("dv", [128, DEPTH * 2 * 6 * 8], F32); Bdv = Buf("dv")
        lamt = k.sb("lamt", [128, DEPTH * 8 + 64], F32); Blam = Buf("lamt")
        csb = k.sb("csb", [128, 16], BF16); Bcsb = Buf("csb")
        arena = k.sb("arena", [128, ARENA], BF16)
        banks = [(k.ps("bank%d" % i, [128, 512]), Buf("bank%d" % i)) for i in range(8)]

        k.dma("sp", vec[:], vecs, writes=[Bvec])
        k.dma("sp", cst[:], consts, writes=[Bcst])
        k.op("dve", lambda e: e.memset(ones16[:], 1.0), writes=[Bo16])
        ones32 = cst[:, C_ONES:C_ONES + 128]
        bd32 = cst[:, C_BD:C_BD + 128]
        sel65 = cst[:, C_SEL:C_SEL + 64]
        ident = cst[:, C_ID:C_ID + 128]

        def V(l, name, j=0, n=1):
            o, ln = VL[name]
            return vec[:, l * VLN + o + j: l * VLN + o + j + n]

        def VGl(name, j=0, n=1):
            o, ln = VG[name]
            return vec[:, o + j: o + j + n]

        def DV(l, which, kind, c):
            o = ((l * 2 + which) * 6 + kind) * 8 + c
            return dv[:, o:o + 1]

        class Carver:
            def __init__(self):
                self.off = 0

            def take(self, nbytes, dtype, name):
                nb = (nbytes + 63) // 64 * 64
                a = arena[:, self.off // 2:(self.off + nb) // 2]
                self.off += nb
                assert self.off <= ARENA * 2, (name, self.off)
                if dtype is F32:
                    a = a.bitcast(F32)
                    return a[:, 0:nbytes // 4]
                return a[:, 0:nbytes // 2]

        epsc = k.sb("epsc", [128, 1], F32); Beps = Buf("epsc")
        fz = k.sb("fz", [128, 1], F32); Bfz = Buf("fz")
        k.op("dve", lambda e: e.memset(epsc[:], EPS), writes=[Beps])

        def rsqrt_from(out, Bout, src_, Bsrc, scale, P):
            k.op("act", lambda e: e.activation(out, src_, AF.Ln, bias=epsc[0:P, :], scale=scale), reads=[Bsrc, Beps],
                 writes=[Bout])
            k.op("act", lambda e: e.activation(out, out, AF.Exp, scale=-0.5), reads=[Bout], writes=[Bout])

        def mm(bank_ap, lhsT, rhs, start, stop, reads, Bbank, skip=False):
            k.op("pe", lambda e: e.matmul(bank_ap, lhsT, rhs, start=start, stop=stop, skip_group_check=skip), reads=reads,
                 writes=[Bbank])

        def mod_phase(ws, l):
            bank, Bb = banks[0]
            for g in range(12):
                wt, Bw = ws.get(fm(w_mod[l])[:, :, g * 512:(g + 1) * 512], 8, 512)
                for nl in range(4):
                    j = g * 4 + nl
                    for c in range(8):
                        mm(bank[:, 2 * j:2 * j + 2], wt[:, c, nl * 128:(nl + 1) * 128], csb[:, 2 * c:2 * c + 2],
                           c == 0, c == 7, [Bw, Bcsb], Bb)
            mv = modT[:, l * 96:(l + 1) * 96].rearrange("p (j w) -> p j w", w=2)
            bv = bank[:, 0:96].rearrange("p (j w) -> p j w", w=2)
            for w in range(2):
                k.op("dve", lambda e: e.tensor_tensor(mv[:, :, w], bv[:, :, w], V(l, "b_mod", 0, 48), ALU.add),
                     reads=[Bb, Bvec], writes=[Bmod])
            for w in range(2):
                for sub, (sh, sc, gt, ng) in enumerate(((0, 1, 2, "n1g"), (3, 4, 5, "n2g"))):
                    o = ((l * 2 + w) * 6 + sub * 3) * 8
                    k.op("dve", lambda e: e.scalar_tensor_tensor(dv[:, o:o + 8], mv[:, sc * 8:(sc + 1) * 8, w], 1.0,
                                                                 V(l, ng, 0, 8), ALU.add, ALU.mult),
                         reads=[Bmod, Bvec], writes=[Bdv])
                    k.op("dve", lambda e: e.tensor_copy(dv[:, o + 8:o + 16], mv[:, sh * 8:(sh + 1) * 8, w]),
                         reads=[Bmod], writes=[Bdv])
                    k.op("dve", lambda e: e.tensor_copy(dv[:, o + 16:o + 24], mv[:, gt * 8:(gt + 1) * 8, w]),
                         reads=[Bmod], writes=[Bdv])
            lam_init = 0.8 - 0.6 * math.exp(-0.3 * l)
            lo = l * 8
            tmp = lamt[:, DEPTH * 8:DEPTH * 8 + 64]
            for j in range(2):
                k.op("dve", lambda e: e.tensor_tensor(tmp, V(l, "lamv", j * 128, 64), V(l, "lamv", j * 128 + 64, 64),
                                                      ALU.mult), reads=[Bvec], writes=[Blam])
                k.op("dve", lambda e: e.reduce_sum(lamt[:, lo + j:lo + j + 1], tmp, AX.X), reads=[Blam], writes=[Blam])
            k.op("act", lambda e: e.activation(lamt[:, lo + 2:lo + 4], lamt[:, lo:lo + 2], AF.Exp), reads=[Blam],
                 writes=[Blam])
            k.op("dve", lambda e: e.tensor_tensor(lamt[:, lo + 4:lo + 5], lamt[:, lo + 3:lo + 4], lamt[:, lo + 2:lo + 3],
                                                  ALU.subtract), reads=[Blam], writes=[Blam])
            k.op("dve", lambda e: e.tensor_scalar(lamt[:, lo + 5:lo + 6], lamt[:, lo + 4:lo + 5], -lam_init, None,
                                                  ALU.add), reads=[Blam], writes=[Blam])
            k.op("dve", lambda e: e.tensor_scalar(lamt[:, lo + 6:lo + 7], V(l, "subg"), 1.0 - lam_init, None, ALU.mult),
                 reads=[Bvec], writes=[Blam])

        def norm_block(T, xv, Bx, n, Acol, Bcol, out, Bout, out2=None, Bout2=None):
            bank, Bb = T["pp"].next()
            for c in range(8):
                sq, Bs = T["sq"].next()
                k.op("act", lambda e: e.activation(sq[:, :n], xv[:, c, :], AF.Square), reads=[Bx], writes=[Bs])
                mm(bank[:, :n], ones32, sq[:, :n], c == 0, c == 7, [Bs, Bcst], Bb)
            rs, Br = T["rs"].next()
            rsqrt_from(rs[:, :n], Br, bank[:, :n], Bb, 1.0 / D, 128)
            for c in range(8):
                if Bcol is None:
                    k.op("dve", lambda e: e.scalar_tensor_tensor(out[:, c, :], xv[:, c, :], Acol(c), rs[:, :n], ALU.mult,
                                                                 ALU.mult), reads=[Bx, Br, Bdv, Bvec], writes=[Bout])
                    continue
                tmp, Bt = T["tmp"].next()
                k.op("dve", lambda e: e.scalar_tensor_tensor(tmp[:, :n], xv[:, c, :], Acol(c), rs[:, :n], ALU.mult,
                                                             ALU.mult), reads=[Bx, Br, Bdv], writes=[Bt])
                k.op("act", lambda e: e.activation(out[:, c, :], tmp[:, :n], AF.Identity, bias=Bcol(c)),
                     reads=[Bt, Bdv], writes=[Bout])
                if out2 is not None:
                    k.op("act", lambda e: e.activation(out2[:, c, :], tmp[:, :n], AF.Identity, bias=Bcol(c)),
                         reads=[Bt, Bdv], writes=[Bout2])

        def rope_pair(T, bA, BA, bB, BB, P, n, cs, sn, Bcs, norm_g, dests):
            if norm_g is not None:
                gA, gB = norm_g
                sA, BsA = T["sq"].next()
                sB, BsB = T["sq"].next()
                k.op("act", lambda e: e.activation(sA[0:P, :n], bA[0:P, :n], AF.Square), reads=[BA], writes=[BsA])
                k.op("act", lambda e: e.activation(sB[0:P, :n], bB[0:P, :n], AF.Square), reads=[BB], writes=[BsB])
                bank, Bb = T["pp"].next()
                mm(bank[0:P, :n], bd32[0:P, 0:P], sA[0:P, :n], True, False, [BsA, Bcst], Bb)
                mm(bank[0:P, :n], bd32[0:P, 0:P], sB[0:P, :n], False, True, [BsB, Bcst], Bb)
                rs, Br = T["rs"].next()
                rsqrt_from(rs[0:P, :n], Br, bank[0:P, :n], Bb, 1.0 / 64, P)
                nA, BnA = T["tmp"].next()
                nB, BnB = T["tmp"].next()
                k.op("dve", lambda e: e.scalar_tensor_tensor(nA[0:P, :n], bA[0:P, :n], gA[0:P], rs[0:P, :n], ALU.mult,
                                                             ALU.mult), reads=[BA, Br, Bvec], writes=[BnA])
                k.op("dve", lambda e: e.scalar_tensor_tensor(nB[0:P, :n], bB[0:P, :n], gB[0:P], rs[0:P, :n], ALU.mult,
                                                             ALU.mult), reads=[BB, Br, Bvec], writes=[BnB])
                srcA, BsrcA, srcB, BsrcB = nA, BnA, nB, BnB
            else:
                srcA, BsrcA, srcB, BsrcB = bA, BA, bB, BB
            t1, B1 = T["rp"].next()
            t2, B2 = T["rp"].next()
            t3, B3 = T["rp"].next()
            t4, B4 = T["rp"].next()
            k.op("dve", lambda e: e.tensor_tensor(t1[0:P, :n], srcA[0:P, :n], cs[0:P, :n], ALU.mult),
                 reads=[BsrcA, Bcs], writes=[B1])
            k.op("dve", lambda e: e.tensor_tensor(t2[0:P, :n], srcB[0:P, :n], sn[0:P, :n], ALU.mult),
                 reads=[BsrcB, Bcs], writes=[B2])
            k.op("dve", lambda e: e.tensor_tensor(t3[0:P, :n], srcB[0:P, :n], cs[0:P, :n], ALU.mult),
                 reads=[BsrcB, Bcs], writes=[B3])
            k.op("dve", lambda e: e.tensor_tensor(t4[0:P, :n], srcA[0:P, :n], sn[0:P, :n], ALU.mult),
                 reads=[BsrcA, Bcs], writes=[B4])
            for (slo, dl) in dests:
                for (dtile, Bd, dlo) in dl:
                    k.op("pool", lambda e: e.tensor_tensor(dtile[dlo:dlo + 32, :n], t1[slo:slo + 32, :n],
                                                           t2[slo:slo + 32, :n], ALU.subtract),
                         reads=[B1, B2], writes=[Bd])
                    k.op("pool", lambda e: e.tensor_tensor(dtile[dlo + 32:dlo + 64, :n], t3[slo:slo + 32, :n],
                                                           t4[slo:slo + 32, :n], ALU.add),
                         reads=[B3, B4], writes=[Bd])

        def mixer_phase(ws, l, src, sname, qsegs):
            cv = Carver()
            slots = [(cv.take(8192, BF16, "ws%d" % i), Buf("ws%d" % i)) for i in range(3)]
            kTA = [(cv.take(NTOK * 2, BF16, "kTA"), Buf("kTA%d" % i)) for i in range(2)]
            kTC = [(cv.take(NTOK * 2, BF16, "kTC"), Buf("kTC%d" % i)) for i in range(4)]
            VAf = cv.take(NKB * 2 * 65 * 2, BF16, "VA")
            VA = VAf.rearrange("p (b v d) -> p b v d", v=2, d=65); BVA = Buf("VA")
            VC = cv.take(NKB * 512 * 2, BF16, "VC").rearrange("p (b n) -> p b n", n=512); BVC = Buf("VC")
            T = {}
            T["xe"] = Ring([(cv.take(8 * EXT * 4, F32, "xe").rearrange("p (c n) -> p c n", n=EXT), Buf("xe%d" % i))
                            for i in range(1)])
            hT = cv.take(8 * EXT * 2, BF16, "hT").rearrange("p (c n) -> p c n", n=EXT); BhT = Buf("hT")
            cs_t = cv.take(EXT * 4, F32, "cs"); sn_t = cv.take(EXT * 4, F32, "sn"); Bcs = Buf("cs")
            u0 = cv.off
            yx = cv.take(4 * EXT * 4, F32, "yx").rearrange("p (c n) -> p c n", n=EXT); Byx = Buf("yx")
            cacc = cv.take(4 * QB * 4, F32, "cacc").rearrange("p (c n) -> p c n", n=QB); Bcacc = [Buf("cacc%d" % i) for i in range(4)]
            osb_l = [(cv.take(512 * 4, F32, "osb"), Buf("osb%d" % i)) for i in range(2)]
            om_l = [(cv.take(512 * 4, F32, "om"), Buf("om%d" % i)) for i in range(2)]
            assert cv.off - u0 >= 8 * EXT * 6 and u0 % 64 == 0
            xs = arena[:, u0 // 2:(u0 + 8 * EXT * 4) // 2].bitcast(F32).rearrange("p (c n) -> p c n", n=EXT); Bxs = Buf("xs")
            hs = arena[:, (u0 + 8 * EXT * 4) // 2:(u0 + 8 * EXT * 6) // 2].rearrange("p (c n) -> p c n", n=EXT); Bhs = Buf("hs")
            ALIAS = [Byx] + Bcacc + [b_ for (_, b_) in osb_l + om_l]
            qTA = [(cv.take(2 * QB * 2, BF16, "qTA"), Buf("qTA%d" % i)) for i in range(4)]
            qTC = [(cv.take(2 * QB * 2, BF16, "qTC"), Buf("qTC%d" % i)) for i in range(4)]
            oaT = [(cv.take(QB * 2, BF16, "oaT"), Buf("oaT%d" % i)) for i in range(4)]
            obT = [(cv.take(QB * 2, BF16, "obT"), Buf("obT%d" % i)) for i in range(4)]
            ocT = [(cv.take(QB * 2, BF16, "ocT"), Buf("ocT%d" % i)) for i in range(4)]
            mT = cv.take(8 * QB * 2, BF16, "mT").rearrange("p (c n) -> p c n", n=QB); BmT = Buf("mT")
            macc = cv.take(8 * QB * 4, F32, "macc").rearrange("p (c n) -> p c n", n=QB); Bmacc = Buf("macc")
            T["sq"] = Ring([(cv.take(EXT * 4, F32, "sq"), Buf("sq%d" % i)) for i in range(2)])
            T["tmp"] = Ring([(cv.take(EXT * 4, F32, "tmp"), Buf("tmp%d" % i)) for i in range(3)])
            T["rs"] = Ring([(cv.take(EXT * 4, F32, "rs"), Buf("rs%d" % i)) for i in range(2)])
            ri_l = [(cv.take(512 * 4, F32, "ri"), Buf("ri%d" % i)) for i in range(2)]
            T["ri"] = Ring(ri_l)
            T["rp"] = Ring([(ri_l[i // 2][0][:, (i % 2) * QB:(i % 2 + 1) * QB], ri_l[i // 2][1]) for i in range(4)])
            T["pT"] = Ring([(cv.take(512 * 2, BF16, "pT"), Buf("pT%d" % i)) for i in range(4)])
            T["osb"] = Ring(osb_l)
            T["om"] = Ring(om_l)
            T["dd"] = Ring([(cv.take(QB * 4, F32, "dd"), Buf("dd%d" % i)) for i in range(2)])
            T["pp"] = Ring(banks[0:8])
            wl = fm(w_in[l])
            srcv = fm(src)

            k.op("pool", lambda e: e.memset(VA[:, :, :, 64:65], 1.0), writes=[BVA])
            for (t_, B_) in qTA + qTC:
                k.op("pool", lambda e: e.memset(t_[:, :], 0.0), writes=[B_])

            wkv = [(slots[0][0][:, 0:2048].rearrange("p (c n) -> p c n", n=256), Buf("wkv_akv"), wl[:, :, OFF_AK:OFF_AK + 256]),
                   (slots[0][0][:, 2048:4096].rearrange("p (c n) -> p c n", n=256), Buf("wkv_ck0"), wl[:, :, OFF_CK:OFF_CK + 256]),
                   (slots[1][0][:, 0:2048].rearrange("p (c n) -> p c n", n=256), Buf("wkv_ck1"),
                    wl[:, :, OFF_CK + 256:OFF_CK + 512]),
                   (slots[2][0][:, 0:4096].rearrange("p (c n) -> p c n", n=512), Buf("wkv_cv"), wl[:, :, OFF_CV:OFF_CV + 512])]
            for (wt_, Bw_, ap_) in wkv:
                k.dma("pool", wt_, ap_, writes=[Bw_])
            n = QB
            xe0, Bxe0 = T["xe"].items[0]
            xring = [(xe0[:, :, 0:n], Bxe0), (macc[:, :, :], Buf("xe_kv2"))]
            hring = [(hT[:, :, 0:n], BhT), (mT[:, :, :], Buf("hT_kv2"))]
            csring = [(cs_t, sn_t, Bcs), (yx[:, 0, :], yx[:, 1, :], Buf("cs_kv2"))]
            NKV = NTOK // QB

            def kv_load(kb):
                c0 = kb * QB
                xv_, Bx_ = xring[kb % 2]
                k.dma("sp", xv_, srcv[:, :, c0:c0 + n], reads=DB(sname, c0, c0 + n), writes=[Bx_])
                cs_, sn_, Bc_ = csring[kb % 2]
                k.dma("sp", cs_[:, 0:n], cosk[:, c0:c0 + n], writes=[Bc_])
                k.dma("sp", sn_[:, 0:n], sink[:, c0:c0 + n], writes=[Bc_])

            def kv_norm(kb):
                which = 1 if kb * QB >= SEQ else 0
                xv_, Bx_ = xring[kb % 2]
                hv_, Bh_ = hring[kb % 2]
                norm_block(T, xv_, Bx_, n, lambda c: DV(l, which, 0, c), lambda c: DV(l, which, 1, c), hv_, Bh_)

            kv_load(0)
            kv_norm(0)
            for kb in range(NKV):
                c0 = kb * QB
                hv, Bh = hring[kb % 2]
                cs_k, sn_k, Bcs_k = csring[kb % 2]
                if kb + 1 < NKV:
                    kv_load(kb + 1)
                wt, Bw, _ = wkv[0]
                (bA, BA), (bB, BB) = T["pp"].next(), T["pp"].next()
                for part, (bk, Bk) in enumerate(((bA, BA), (bB, BB))):
                    for c in range(8):
                        mm(bk[0:64, :n], wt[:, c, part * 64:(part + 1) * 64], hv[:, c, :], c == 0, c == 7, [Bw, Bh], Bk)
                dests = []
                for kvh in range(2):
                    t, Bt = kTA[kvh]
                    dests.append((32 * kvh, [(t[:, c0:c0 + n], Bt, 0), (t[:, c0:c0 + n], Bt, 64)]))
                rope_pair(T, bA, BA, bB, BB, 64, n, cs_k, sn_k, Bcs_k, (V(l, "kng", 0), V(l, "kng", 1)), dests)
                for sbk in range(n // 128):
                    bk, Bk = T["pp"].next()
                    for c in range(8):
                        mm(bk[:, 0:128], hv[:, c, sbk * 128:(sbk + 1) * 128], wt[:, c, 128:256], c == 0, c == 7, [Bw, Bh], Bk)
                    blk = c0 // 128 + sbk
                    k.op("act", lambda e: e.activation(VA[:, blk, :, 0:64], bk[:, 0:128].rearrange("p (v d) -> p v d", d=64),
                                                       AF.Copy), reads=[Bk], writes=[BVA])
                for pr in range(2):
                    wt, Bw, _ = wkv[1 + pr]
                    (bA, BA), (bB, BB) = T["pp"].next(), T["pp"].next()
                    for part, (bk, Bk) in enumerate(((bA, BA), (bB, BB))):
                        for c in range(8):
                            mm(bk[:, :n], wt[:, c, part * 128:(part + 1) * 128], hv[:, c, :], c == 0, c == 7, [Bw, Bh], Bk)
                    dests = []
                    for ul in range(4):
                        u = pr * 4 + ul
                        t, Bt = kTC[u // 2]
                        dests.append((32 * ul, [(t[:, c0:c0 + n], Bt, 64 * (u % 2))]))
                    rope_pair(T, bA, BA, bB, BB, 128, n, cs_k, sn_k, Bcs_k, None, dests)
                    if pr == 0 and kb + 1 < NKV:
                        kv_norm(kb + 1)
                wt, Bw, _ = wkv[3]
                for sbk in range(n // 128):
                    bk, Bk = T["pp"].next()
                    for c in range(8):
                        mm(bk[:, 0:512], hv[:, c, sbk * 128:(sbk + 1) * 128], wt[:, c, :], c == 0, c == 7, [Bw, Bh], Bk)
                    blk = c0 // 128 + sbk
                    k.op("act", lambda e: e.activation(VC[:, blk, :], bk[:, 0:512], AF.Copy), reads=[Bk], writes=[BVC])

            k.barrier()
            qslots = [(slots[i // 2][0][:, (i % 2) * 2048:(i % 2 + 1) * 2048], Buf("qws%d" % i)) for i in range(6)]
            ws.new_phase(qslots, 4)
            for seg in qsegs:
                which = seg["which"]
                nblk = seg["n"] // QB
                for qb in range(nblk):
                    c0 = seg["c0"] + qb * QB
                    n = QB
                    xe, Bxe = T["xe"].next()

                    def load_x(dst, Bdst, qb_, q_="sp"):
                        c0_ = seg["c0"] + qb_ * QB
                        if 0 < qb_ < nblk - 1:
                            k.dma(q_, dst[:, :, :], srcv[:, :, c0_ - 15:c0_ + n + 15], reads=DB(sname, c0_ - 15, c0_ + n + 15),
                                  writes=[Bdst])
                            return
                        k.dma(q_, dst[:, :, 15:15 + n], srcv[:, :, c0_:c0_ + n], reads=DB(sname, c0_, c0_ + n), writes=[Bdst])
                        lcol = seg["hl_col"] if qb_ == 0 else c0_ - 15
                        rcol = seg["hr_col"] if qb_ == nblk - 1 else c0_ + n
                        k.dma(q_, dst[:, :, 0:15], srcv[:, :, lcol:lcol + 15], reads=DB(sname, lcol, lcol + 15), writes=[Bdst])
                        k.dma(q_, dst[:, :, 15 + n:30 + n], srcv[:, :, rcol:rcol + 15], reads=DB(sname, rcol, rcol + 15),
                              writes=[Bdst])

                    if qb == 0:
                        load_x(xe, Bxe, qb)
                    k.dma("sp", cs_t[:, 0:n], cosk[:, c0:c0 + n], writes=[Bcs])
                    k.dma("sp", sn_t[:, 0:n], sink[:, c0:c0 + n], writes=[Bcs])
                    if qb == 0:
                        norm_block(T, xe, Bxe, EXT, lambda c: DV(l, which, 0, c), lambda c: DV(l, which, 1, c), hT, BhT)
                    hm = hT[:, :, 15:15 + n]
                    for j in range(4):
                        if j % 2 == 0:
                            jj = j // 2
                            wa, Bwa = ws.get(wl[:, :, OFF_BZ + jj * 256:OFF_BZ + (jj + 1) * 256], 8, 256, key=(l, "bza", jj))
                            wg, Bwg = ws.get(wl[:, :, OFF_BZ + 512 + jj * 256:OFF_BZ + 512 + (jj + 1) * 256], 8, 256,
                                             key=(l, "bzg", jj))
                        j2 = j % 2
                        (ba, Ba), (bg, Bg) = T["pp"].next(), T["pp"].next()
                        for c in range(8):
                            mm(ba[:, :EXT], wa[:, c, j2 * 128:(j2 + 1) * 128], hT[:, c, :], c == 0, c == 7, [Bwa, BhT], Ba)
                        for c in range(8):
                            mm(bg[:, :EXT], wg[:, c, j2 * 128:(j2 + 1) * 128], hT[:, c, :], c == 0, c == 7, [Bwg, BhT], Bg)
                        sg, Bsg = T["tmp"].next()
                        k.op("act", lambda e: e.activation(sg[:, :EXT], bg[:, :EXT], AF.Sigmoid), reads=[Bg], writes=[Bsg])
                        k.op("dve", lambda e: e.tensor_tensor(yx[:, j, :], ba[:, :EXT], sg[:, :EXT], ALU.mult),
                             reads=[Ba, Bsg], writes=[Byx])
                    if qb == 0:
                        mcol = VGl("hmask", seg["hl_mask"])
                        k.op("pool", lambda e: e.tensor_scalar(yx[:, :, 0:15], yx[:, :, 0:15], mcol, None, ALU.mult),
                             reads=[Byx, Bvec], writes=[Byx])
                    if qb == nblk - 1:
                        mcol = VGl("hmask", seg["hr_mask"])
                        k.op("pool", lambda e: e.tensor_scalar(yx[:, :, 15 + n:30 + n], yx[:, :, 15 + n:30 + n], mcol, None,
                                                               ALU.mult), reads=[Byx, Bvec], writes=[Byx])
                    side = []

                    def conv_tap(j, t):
                        if t == 0:
                            k.op("dve", lambda e: e.tensor_scalar(cacc[:, j, :], yx[:, j, 0:n], V(l, "dww", j * 31),
                                                                  V(l, "dwb", j), ALU.mult, ALU.add),
                                 reads=[Byx, Bvec], writes=[Bcacc[j]])
                        else:
                            k.op("dve", lambda e: e.scalar_tensor_tensor(cacc[:, j, :], yx[:, j, t:t + n],
                                                                         V(l, "dww", j * 31 + t), cacc[:, j, :], ALU.mult,
                                                                         ALU.add), reads=[Byx, Bvec, Bcacc[j]], writes=[Bcacc[j]])
                    for t in range(31):
                        for j in range(4):
                            side.append(lambda j=j, t=t: conv_tap(j, t))
                    mean, rstd, m2, d1 = yx[:, 0, 0:n], yx[:, 1, 0:n], yx[:, 2, 0:n], yx[:, 3, 0:n]

                    lnst = {}

                    def ln_sq(j0):
                        if j0 == 0:
                            lnst["bank"] = T["pp"].next()
                        for j in (j0, j0 + 1):
                            sq, Bs = T["sq"].next()
                            lnst[j] = (sq, Bs)
                            k.op("act", lambda e: e.activation(sq[:, :n], cacc[:, j, :], AF.Square), reads=[Bcacc[j]], writes=[Bs])

                    def ln_mm(j0):
                        bfull, B1 = lnst["bank"]
                        b1, b2, B2 = bfull[:, 0:n], bfull[:, n:2 * n], B1
                        for j in (j0, j0 + 1):
                            sq, Bs = lnst[j]
                            mm(b1[:, :n], ones32, cacc[:, j, :], j == 0, j == 3, [Bcacc[j], Bcst], B1, skip=True)
                            mm(b2[:, :n], ones32, sq[:, :n], False, j == 3, [Bs, Bcst], B2, skip=True)

                    def ln_var():
                        bfull, B1 = lnst["bank"]
                        b1, b2, B2 = bfull[:, 0:n], bfull[:, n:2 * n], B1
                        k.op("dve", lambda e: e.tensor_scalar(mean, b1[:, :n], 1.0 / 512, None, ALU.mult),
                             reads=[B1], writes=[Byx])
                        k.op("dve", lambda e: e.tensor_tensor(m2, mean, mean, ALU.mult), reads=[Byx], writes=[Byx])
                        k.op("dve", lambda e: e.scalar_tensor_tensor(rstd, b2[:, :n], 1.0 / 512, m2, ALU.mult,
                                                                     ALU.subtract), reads=[B2, Byx], writes=[Byx])

                    def ln_rs():
                        rsqrt_from(rstd, Byx, rstd, Byx, 1.0, 128)

                    def ln_sub():
                        for j in range(4):
                            k.op("pool", lambda e: e.tensor_tensor(cacc[:, j, :], cacc[:, j, :], mean, ALU.subtract),
                                 reads=[Bcacc[j], Byx], writes=[Bcacc[j]])

                    def ln_mul():
                        for j in range(4):
                            k.op("dve", lambda e: e.tensor_tensor(cacc[:, j, :], cacc[:, j, :], rstd, ALU.mult),
                                 reads=[Bcacc[j], Byx], writes=[Bcacc[j]])

                    def ln_act():
                        for j in range(4):
                            ot, Bot = obT[j]
                            k.op("act", lambda e: e.activation(ot[:, :n], cacc[:, j, :], AF.Silu, bias=V(l, "lnb", j),
                                                               scale=V(l, "lng", j)), reads=[Bcacc[j], Bvec], writes=[Bot])

                    def ln_mm0_sq2():
                        ln_mm(0)
                        ln_sq(2)
                    side += [None] * 21
                    side += [lambda: ln_sq(0), None, ln_mm0_sq2, None, lambda: ln_mm(2), None, None, ln_var, None, None, ln_rs,
                             None, ln_sub, None, None, ln_mul, None, None, ln_act]
                    for (off, qT, ng) in ((OFF_AQ, qTA, (V(l, "qng", 0), V(l, "qng", 1))), (OFF_CQ, qTC, None)):
                        for pr in range(2):
                            wt, Bw = ws.get(wl[:, :, off + pr * 256:off + (pr + 1) * 256], 8, 256, key=(l, "q", off, pr))
                            (bA, BA), (bB, BB) = T["pp"].next(), T["pp"].next()
                            for part, (bk, Bk) in enumerate(((bA, BA), (bB, BB))):
                                for c in range(8):
                                    mm(bk[:, :n], wt[:, c, part * 128:(part + 1) * 128], hm[:, c, :], c == 0, c == 7,
                                       [Bw, BhT], Bk)
                            dests = []
                            for ul in range(4):
                                u = pr * 4 + ul
                                t, Bt = qT[u // 2]
                                dests.append((32 * ul, [(t[:, (u % 2) * n:(u % 2 + 1) * n], Bt, 64 * (u % 2))]))
                            rope_pair(T, bA, BA, bB, BB, 128, n, cs_t, sn_t, Bcs, ng, dests)
                    kbl = [2 * kp + i for kp in seg["keys"] for i in range(2)]
                    NP = len(kbl)
                    n2 = 2 * n
                    units = [("A", hp) for hp in range(4)] + [("C", hc) for hc in range(4)]
                    flat = [(ui, pi) for ui in range(len(units)) for pi in range(NP)]
                    pp_saved = T["pp"]
                    T["pp"] = Ring(banks[0:1]); T["st"] = Ring(banks[1:4]); T["ac"] = Ring(banks[4:8])
                    stq = {}

                    def emit_qk(s_):
                        ui, pi = flat[s_]
                        kind, a_ = units[ui]
                        (qt, Bq) = qTA[a_] if kind == "A" else qTC[a_]
                        (kt, Bkt) = kTA[a_ // 2] if kind == "A" else kTC[a_]
                        stb, Bst = T["st"].next()
                        kbk = kbl[pi]
                        mm(stb[:, 0:n2], kt[:, kbk * 128:(kbk + 1) * 128], qt[:, 0:n2], True, True, [Bkt, Bq], Bst)
                        stq[s_] = (stb, Bst)

                    pend = []

                    def flush(all_=False):
                        for it in pend:
                            it[0] -= 1
                        while pend and (all_ or pend[0][0] <= 0):
                            pend.pop(0)[1]()

                    def epi_A(hp, acc, Bacc):
                        if pend:
                            flush(True)
                        osb, Bosb = T["osb"].next()
                        k.op("dve", lambda e: e.tensor_copy(osb[0:65, 0:n2], acc[0:65, 0:n2]), reads=[Bacc], writes=[Bosb])
                        st_ = {}

                        def p1():
                            rb_, Brb = T["pp"].next()
                            st_["rb"] = (rb_, Brb)
                            mm(rb_[0:64, 0:n2], sel65[0:65, :], osb[0:65, 0:n2], True, True, [Bosb, Bcst], Brb)

                        def p2():
                            rb_, Brb = st_["rb"]
                            ri, Bri = T["ri"].next()
                            k.op("act", lambda e: e.activation(ri[0:64, 0:n2], rb_[0:64, 0:n2], AF.Ln), reads=[Brb], writes=[Bri])
                            k.op("act", lambda e: e.activation(ri[0:64, 0:n2], ri[0:64, 0:n2], AF.Exp, scale=-1.0),
                                 reads=[Bri], writes=[Bri])
                            ot, Bot = oaT[hp]
                            for hh in range(2):
                                k.op("dve", lambda e: e.tensor_tensor(ot[64 * hh:64 * hh + 64, :n], osb[0:64, hh * n:(hh + 1) * n],
                                                                      ri[0:64, hh * n:(hh + 1) * n], ALU.mult),
                                     reads=[Bosb, Bri], writes=[Bot])
                        pend.append([4, p1])
                        pend.append([6, p2])

                    def epi_C(hc, acc, Bacc, rsk, Brsk):
                        if pend:
                            flush(True)
                        st_ = {}

                        def p0():
                            ri, Bri = T["ri"].next()
                            k.op("act", lambda e: e.activation(ri[:, 0:n2], rsk[:, 0:n2], AF.Ln), reads=[Brsk], writes=[Bri])
                            k.op("act", lambda e: e.activation(ri[:, 0:n2], ri[:, 0:n2], AF.Exp, scale=-1.0), reads=[Bri],
                                 writes=[Bri])
                            om, Bom = T["om"].next()
                            k.op("dve", lambda e: e.tensor_tensor(om[:, 0:n2], acc[:, 0:n2], ri[:, 0:n2], ALU.mult),
                                 reads=[Bacc, Bri], writes=[Bom])
                            dd, Bdd = T["dd"].next()
                            st_["dd"] = (dd, Bdd)
                            k.op("dve", lambda e: e.scalar_tensor_tensor(dd[:, :n], om[:, n:n2], lamt[:, l * 8 + 5:l * 8 + 6],
                                                                         om[:, 0:n], ALU.mult, ALU.add),
                                 reads=[Bom, Blam], writes=[Bdd])
                            sq, Bs = T["sq"].next()
                            st_["sq"] = (sq, Bs)
                            k.op("pool", lambda e: e.tensor_tensor(sq[:, :n], dd[:, :n], dd[:, :n], ALU.mult), reads=[Bdd],
                                 writes=[Bs])

                        def p1():
                            sq, Bs = st_["sq"]
                            bk, Bk = T["pp"].next()
                            st_["bk"] = (bk, Bk)
                            mm(bk[:, :n], ones32, sq[:, :n], True, True, [Bs, Bcst], Bk)

                        def p2():
                            bk, Bk = st_["bk"]
                            dd, Bdd = st_["dd"]
                            rs, Br = T["rs"].next()
                            rsqrt_from(rs[:, :n], Br, bk[:, :n], Bk, 1.0 / 128, 128)
                            ot, Bot = ocT[hc]
                            k.op("dve", lambda e: e.scalar_tensor_tensor(ot[:, :n], dd[:, :n], lamt[:, l * 8 + 6:l * 8 + 7],
                                                                         rs[:, :n], ALU.mult, ALU.mult),
                                 reads=[Bdd, Br, Blam], writes=[Bot])
                        pend.append([1, p0])
                        pend.append([4, p1])
                        pend.append([7, p2])

                    SK = 2
                    for s_ in range(min(SK, len(flat))):
                        emit_qk(s_)
                    per_step = (len(side) + max(1, len(flat) - 6) - 1) // max(1, len(flat) - 6)
                    cur = None
                    for s_, (ui, pi) in enumerate(flat):
                        if s_ + SK < len(flat):
                            emit_qk(s_ + SK)
                        kind, a_ = units[ui]
                        stb, Bst = stq.pop(s_)
                        pT, BpT = T["pT"].next()
                        k.op("act", lambda e: e.activation(pT[:, 0:n2], stb[:, 0:n2], AF.Exp, scale=0.125),
                             reads=[Bst], writes=[BpT])
                        if pi == 0:
                            cur = [T["ac"].next()]
                            if kind == "C":
                                cur.append(T["ac"].next())
                        acc, Bacc = cur[0]
                        kbk = kbl[pi]
                        first = pi == 0
                        lastk = pi == NP - 1
                        if kind == "A":
                            mm(acc[0:65, 0:n2], VA[:, kbk, a_ // 2, :], pT[:, 0:n2], first, lastk, [BVA, BpT], Bacc)
                        else:
                            rsk, Brsk = cur[1]
                            mm(acc[:, 0:n2], VC[:, kbk, a_ * 128:(a_ + 1) * 128], pT[:, 0:n2], first, lastk, [BVC, BpT], Bacc)
                            mm(rsk[:, 0:n2], ones16[:], pT[:, 0:n2], first, lastk, [Bo16, BpT], Brsk)
                        flush()
                        if pi == NP - 1:
                            if kind == "A":
                                epi_A(a_, acc, Bacc)
                            else:
                                epi_C(a_, acc, Bacc, cur[1][0], cur[1][1])
                        for _ in range(per_step):
                            if side:
                                f_ = side.pop(0)
                                if f_ is not None:
                                    f_()
                    flush(True)
                    while side:
                        f_ = side.pop(0)
                        if f_ is not None:
                            f_()
                    T["pp"] = pp_saved
                    staged = qb + 1 < nblk
                    if staged:
                        k.op("pool", lambda e: e.memset(fz[:], 0.0), writes=ALIAS + [Bxs, Bhs, Bfz])
                        load_x(xs, Bxs, qb + 1, "pool")
                    for a, (wp, oT) in enumerate(((w_pa, oaT), (w_pb, obT), (w_pc, ocT))):
                        wpv = wp[l].rearrange("(c p) n -> p c n", p=128)
                        if a == 1 and staged:
                            norm_block(T, xs, Bxs, EXT, lambda c: DV(l, which, 0, c), lambda c: DV(l, which, 1, c), hs, Bhs)
                        for ng in range(4):
                            wg, Bwg = ws.get(wl[:, :, OFF_GT + a * 1024 + ng * 256:OFF_GT + a * 1024 + (ng + 1) * 256], 8, 256,
                                             key=(l, "gt", a, ng))
                            wpt, Bwp = ws.get(wpv[:, :, ng * 256:(ng + 1) * 256], 4, 256, key=(l, "wp", a, ng))
                            for nl in range(2):
                                nn = ng * 2 + nl
                                (bg, Bg), (bp, Bp) = T["pp"].next(), T["pp"].next()
                                for c in range(8):
                                    mm(bg[:, :n], wg[:, c, nl * 128:(nl + 1) * 128], hm[:, c, :], c == 0, c == 7, [Bwg, BhT], Bg)
                                for c in range(4):
                                    mm(bp[:, :n], wpt[:, c, nl * 128:(nl + 1) * 128], oT[c][0][:, :n], c == 0, c == 3,
                                       [Bwp, oT[c][1]], Bp)
                                gs, Bgs = T["tmp"].next()
                                k.op("act", lambda e: e.activation(gs[:, :n], bg[:, :n], AF.Sigmoid,
                                                                   bias=V(l, "b_gate", a * 8 + nn)),
                                     reads=[Bg, Bvec], writes=[Bgs])
                                if a == 0:
                                    k.op("dve", lambda e: e.tensor_tensor(macc[:, nn, :], gs[:, :n], bp[:, :n], ALU.mult),
                                         reads=[Bgs, Bp], writes=[Bmacc])
                                else:
                                    k.op("dve", lambda e: e.tensor_tensor(gs[:, :n], gs[:, :n], bp[:, :n], ALU.mult),
                                         reads=[Bgs, Bp], writes=[Bgs])
                                    if a == 1:
                                        k.op("dve", lambda e: e.tensor_tensor(macc[:, nn, :], macc[:, nn, :], gs[:, :n], ALU.add),
                                             reads=[Bgs, Bmacc], writes=[Bmacc])
                                    else:
                                        k.op("dve", lambda e: e.tensor_tensor(mT[:, nn, :], macc[:, nn, :], gs[:, :n], ALU.add),
                                             reads=[Bgs, Bmacc], writes=[BmT])
                    wov = fm(w_out[l])
                    for ng in range(4):
                        wt, Bw = ws.get(wov[:, :, ng * 256:(ng + 1) * 256], 8, 256, key=(l, "wo", ng))
                        for nl in range(2):
                            nn = ng * 2 + nl
                            bk, Bk = T["pp"].next()
                            for c in range(8):
                                mm(bk[:, :n], wt[:, c, nl * 128:(nl + 1) * 128], mT[:, c, :], c == 0, c == 7, [Bw, BmT], Bk)
                            k.op("dve", lambda e: e.scalar_tensor_tensor(macc[:, nn, :], bk[:, :n], DV(l, which, 2, nn),
                                                                         xe[:, nn, 15:15 + n], ALU.mult, ALU.add),
                                 reads=[Bk, Bdv, Bxe], writes=[Bmacc])
                    k.dma("sp", fm(xm)[:, :, c0:c0 + n], macc[:, :, :], reads=[Bmacc], writes=DB("xm_scr", c0, c0 + n))
                    if staged:
                        k.op("dve", lambda e: e.tensor_copy(xe[:, :, :], xs[:, :, :]), reads=[Bxs], writes=[Bxe])
                        k.op("act", lambda e: e.activation(hT[:, :, :], hs[:, :, :], AF.Copy), reads=[Bhs], writes=[BhT])
                        k.op("pool", lambda e: e.memset(fz[:], 0.0), writes=ALIAS + [Bxs, Bhs, Bfz])
                    if DEBUG and seg is qsegs[0] and qb == 0 and l == layers[0]:
                        o = 0
                        k.dma("sp", dbg16[:, 0:8 * n].rearrange("p (c n) -> p c n", n=n), hT[:, :, 15:15 + n], reads=[BhT]); o += 8 * n
                        for lst in (oaT, obT, ocT, qTA, qTC):
                            for (t_, B_) in lst:
                                k.dma("sp", dbg16[:, o:o + n], t_[:, :n], reads=[B_]); o += n
                        k.dma("sp", dbg16[:, o:o + 8 * n].rearrange("p (c n) -> p c n", n=n), mT[:, :, :], reads=[BmT])
                        k.dma("sp", dbg32[:, 0:8 * EXT].rearrange("p (c n) -> p c n", n=EXT), xe[:, :, :], reads=[Bxe])
                        k.dma("sp", dbg32[:, 8 * EXT:12 * EXT].rearrange("p (c n) -> p c n", n=EXT), yx[:, :, :], reads=[Byx])
                        k.dma("sp", dbg32[:, 12 * EXT:12 * EXT + 4 * n].rearrange("p (c n) -> p c n", n=n), cacc[:, :, :], reads=Bcacc)

        def ffn_phase(ws, l, tsegs, moe, final, dst, dname):
            cv = Carver()
            nslot = 6
            slots = [(cv.take(8192, BF16, "ws%d" % i), Buf("fws%d" % i)) for i in range(nslot)]
            ws.new_phase(slots, 2)
            NT = sum(s["n"] for s in tsegs)
            acc = cv.take(8 * NT * 4, F32, "acc").rearrange("p (c n) -> p c n", n=NT)
            h2T = cv.take(8 * NT * 2, BF16, "h2T").rearrange("p (c n) -> p c n", n=NT)
            nblk256 = NT // 256
            Bacc = [Buf("acc%d" % i) for i in range(nblk256)]
            Bh2 = [Buf("h2_%d" % i) for i in range(nblk256)]
            T = {}
            T["sq"] = Ring([(cv.take(512 * 4, F32, "sq"), Buf("fsq%d" % i)) for i in range(2)])
            T["tmp"] = Ring([(cv.take(512 * 4, F32, "tmp"), Buf("ftmp%d" % i)) for i in range(3)])
            T["rs"] = Ring([(cv.take(512 * 4, F32, "rs"), Buf("frs%d" % i)) for i in range(2)])
            hid = Ring([(cv.take(4 * 512 * 2, BF16, "hid").rearrange("p (c n) -> p c n", n=512), Buf("hid%d" % i))
                        for i in range(2)])
            T["pp"] = Ring(banks[0:2])
            pa = Ring(banks[0:2]); pb = Ring(banks[2:4]); py = Ring(banks[4:8])
            if moe:
                h2f = cv.take(8 * 256 * 4, F32, "h2f").rearrange("p (c n) -> p c n", n=256); Bh2f = Buf("h2f")
                gT = cv.take(NT * 4, F32, "gT"); BgT = Buf("gT")
                Ge = cv.take(NT * 4, F32, "Ge"); BGe = Buf("Ge")
                wr = cv.take(8 * 8 * 4, F32, "wr").rearrange("p (c n) -> p c n", n=8); Bwr = Buf("wr")
                sm = cv.take(64 * 4, F32, "sm"); Bsm = Buf("sm")
                k.dma("sp", wr, fm(moe_r[0]), writes=[Bwr])
            blocks = []
            lc = 0
            for s in tsegs:
                for j in range(s["n"] // 256):
                    blocks.append((lc, s, s["c0"] + j * 256, None if s["dcol"] is None else s["dcol"] + j * 256))
                    lc += 256
            xmv = fm(xm)
            for bi, (lc, s, c0, dcol) in enumerate(blocks):
                which = s["which"]
                k.dma("sp", acc[:, :, lc:lc + 256], xmv[:, :, c0:c0 + 256], reads=DB("xm_scr", c0, c0 + 256), writes=[Bacc[bi]])
                norm_block(T, acc[:, :, lc:lc + 256], Bacc[bi], 256, lambda c: DV(l, which, 3, c), lambda c: DV(l, which, 4, c),
                           h2T[:, :, lc:lc + 256], Bh2[bi], out2=h2f if moe else None, Bout2=Bh2f if moe else None)
                if moe:
                    for sbk in range(2):
                        bk, Bk = T["pp"].next()
                        for c in range(8):
                            mm(bk[:, 0:8], h2f[:, c, sbk * 128:(sbk + 1) * 128], wr[:, c, :], c == 0, c == 7, [Bh2f, Bwr], Bk)
                        lg = sm[:, 0:8]; m1 = sm[:, 8:9]; eq = sm[:, 16:24]; l2 = sm[:, 24:32]; m2 = sm[:, 9:10]
                        sel = sm[:, 32:40]; ex = sm[:, 40:48]; nm1 = sm[:, 10:11]; ssum = sm[:, 11:12]; gg = sm[:, 48:56]
                        R, W = [Bsm], [Bsm]
                        k.op("dve", lambda e: e.tensor_tensor(lg, bk[:, 0:8], VGl("rb", 0, 8), ALU.add), reads=[Bk, Bvec], writes=W)
                        k.op("dve", lambda e: e.reduce_max(m1, lg, AX.X), reads=R, writes=W)
                        k.op("dve", lambda e: e.tensor_scalar(eq, lg, m1, None, ALU.is_equal), reads=R, writes=W)
                        k.op("dve", lambda e: e.scalar_tensor_tensor(l2, eq, -1.0e30, lg, ALU.mult, ALU.add), reads=R, writes=W)
                        k.op("dve", lambda e: e.reduce_max(m2, l2, AX.X), reads=R, writes=W)
                        k.op("dve", lambda e: e.tensor_scalar(sel, lg, m2, None, ALU.is_ge), reads=R, writes=W)
                        k.op("dve", lambda e: e.tensor_scalar(nm1, m1, -1.0, None, ALU.mult), reads=R, writes=W)
                        k.op("act", lambda e: e.activation(ex, lg, AF.Exp, bias=nm1), reads=R, writes=W)
                        k.op("dve", lambda e: e.tensor_tensor(gg, sel, ex, ALU.mult), reads=R, writes=W)
                        k.op("dve", lambda e: e.reduce_sum(ssum, gg, AX.X), reads=R, writes=W)
                        k.op("dve", lambda e: e.reciprocal(ssum, ssum), reads=R, writes=W)
                        k.op("dve", lambda e: e.tensor_scalar(gg, gg, ssum, None, ALU.mult), reads=R, writes=W)
                        bt, Bbt = T["pp"].next()
                        k.op("pe", lambda e: e.transpose(bt[0:8, 0:128], gg, ident), reads=[Bsm, Bcst], writes=[Bbt])
                        cc = lc + sbk * 128
                        k.op("act", lambda e: e.activation(gT[0:8, cc:cc + 128], bt[0:8, 0:128], AF.Copy), reads=[Bbt], writes=[BgT])
            tbl = []
            lc = 0
            for s in tsegs:
                o = 0
                while o < s["n"]:
                    w = min(512, s["n"] - o)
                    tbl.append((lc + o, w, s["which"]))
                    o += w
                lc += s["n"]
            nexp = N_EXP if moe else 1
            dff = D_FFE if moe else D_FF
            work = []
            for ex_i in range(nexp):
                f0 = 0
                while f0 < dff:
                    fw = min(512, dff - f0)
                    for ti, (lc, w, which) in enumerate(tbl):
                        work.append((ex_i, f0, fw, ti, lc, w, which))
                    f0 += fw
            wstate = {}

            def stage_a(item):
                ex_i, f0, fw, ti, lc, w, which = item
                nfc = fw // 128
                if ti == 0:
                    if moe:
                        if f0 == 0:
                            for (lc_, w_, which_) in tbl:
                                bk, Bk = py.next()
                                mm(bk[:, :w_], cst[0:8, C_OH + ex_i * 128:C_OH + (ex_i + 1) * 128], gT[0:8, lc_:lc_ + w_], True, True,
                                   [BgT, Bcst], Bk)
                                k.op("act", lambda e: e.activation(Ge[:, lc_:lc_ + w_], bk[:, :w_], AF.Copy), reads=[Bk],
                                     writes=[BGe])
                        w1v, w3v, w2v = fm(moe_w1[0, ex_i]), fm(moe_w3[0, ex_i]), fm(moe_w2[0, ex_i])
                    else:
                        w1v, w3v, w2v = fm(ffn_w1[0]), fm(ffn_w3[0]), fm(ffn_w2[0])
                    w1t, Bw1 = ws.get(w1v[:, :, f0:f0 + fw], 8, fw)
                    w3t, Bw3 = ws.get(w3v[:, :, f0:f0 + fw], 8, fw)
                    w2t, Bw2 = ws.get(w2v[:, f0 // 128:f0 // 128 + nfc, :], nfc, 1024)
                    wstate["w"] = (w1t, Bw1, w3t, Bw3, w2t, Bw2)
                w1t, Bw1, w3t, Bw3, w2t, Bw2 = wstate["w"]
                bis = list(range(lc // 256, (lc + w) // 256))
                hd, Bhd = hid.next()
                for fc in range(nfc):
                    (ba, Ba), (bb, Bb) = pa.next(), pb.next()
                    for c in range(8):
                        mm(ba[:, :w], w1t[:, c, fc * 128:(fc + 1) * 128], h2T[:, c, lc:lc + w], c == 0, c == 7,
                           [Bw1] + [Bh2[i] for i in bis], Ba)
                    for c in range(8):
                        mm(bb[:, :w], w3t[:, c, fc * 128:(fc + 1) * 128], h2T[:, c, lc:lc + w], c == 0, c == 7,
                           [Bw3] + [Bh2[i] for i in bis], Bb)
                    sa, Bsa = T["tmp"].next()
                    k.op("act", lambda e: e.activation(sa[:, :w], ba[:, :w], AF.Silu), reads=[Ba], writes=[Bsa])
                    if moe:
                        k.op("dve", lambda e: e.tensor_tensor(sa[:, :w], sa[:, :w], bb[:, :w], ALU.mult),
                             reads=[Bsa, Bb], writes=[Bsa])
                        k.op("pool", lambda e: e.tensor_tensor(hd[:, fc, :w], sa[:, :w], Ge[:, lc:lc + w], ALU.mult),
                             reads=[Bsa, BGe], writes=[Bhd])
                    else:
                        k.op("dve", lambda e: e.tensor_tensor(hd[:, fc, :w], sa[:, :w], bb[:, :w], ALU.mult),
                             reads=[Bsa, Bb], writes=[Bhd])
                return (hd, Bhd, w2t, Bw2, nfc, lc, w, which, bis)

            def stage_b(ctx_):
                hd, Bhd, w2t, Bw2, nfc, lc, w, which, bis = ctx_
                for nn in range(8):
                    by, By = py.next()
                    for fc in range(nfc):
                        mm(by[:, :w], w2t[:, fc, nn * 128:(nn + 1) * 128], hd[:, fc, :w], fc == 0, fc == nfc - 1,
                           [Bw2, Bhd], By)
                    k.op("dve", lambda e: e.scalar_tensor_tensor(acc[:, nn, lc:lc + w], by[:, :w], DV(l, which, 5, nn),
                                                                 acc[:, nn, lc:lc + w], ALU.mult, ALU.add),
                         reads=[By, Bdv] + [Bacc[i] for i in bis], writes=[Bacc[i] for i in bis])

            prev_ = None
            for item in work:
                cur_ = stage_a(item)
                if prev_ is not None:
                    stage_b(prev_)
                prev_ = cur_
            stage_b(prev_)
            if final:
                k.barrier()
                Bstg = Buf("stg")
            for bi, (lc, s, c0, dcol) in enumerate(blocks):
                if dcol is None:
                    continue
                if final:
                    stg = h2f
                    norm_block(T, acc[:, :, lc:lc + 256], Bacc[bi], 256, lambda c: VGl("fing", c), None, stg, Bstg)
                    k.dma("sp", fm(dst)[:, :, dcol:dcol + 256], stg, reads=[Bstg], writes=DB(dname, dcol, dcol + 256))
                else:
                    k.dma("sp", fm(dst)[:, :, dcol:dcol + 256], acc[:, :, lc:lc + 256], reads=[Bacc[bi]],
                          writes=DB(dname, dcol, dcol + 256))

        all_keys = list(range(NKB // 2))
        ctx_keys = [SEQ // 256]
        seg_own = dict(c0=0, n=HALF, which=0, hl_col=SEQ - 15, hl_mask=0, hr_col=HALF, hr_mask=1, keys=all_keys)
        seg_oth = dict(c0=HALF, n=HALF, which=0, hl_col=HALF - 15, hl_mask=2, hr_col=0, hr_mask=3, keys=all_keys)
        seg_ctx = dict(c0=SEQ, n=CTX, which=1, hl_col=SEQ, hl_mask=4, hr_col=SEQ, hr_mask=4, keys=ctx_keys)

        def whole(ws):
            k.op("act", lambda e: e.activation(csb[:], VGl("cfm", 0, 16), AF.Silu), reads=[Bvec], writes=[Bcsb])
            cvm = Carver()
            ws.new_phase([(cvm.take(8192, BF16, "mws%d" % i), Buf("mws%d" % i)) for i in range(3)], 2)
            for l in layers:
                mod_phase(ws, l)
            for li, l in enumerate(layers):
                k.barrier()
                src, sname = (xT, "xT") if li == 0 else (xb, "xb_scr")
                lastl = l == DEPTH - 1
                if lastl:
                    qsegs = [seg_own]
                elif fused and len(layers) > 1:
                    qsegs = [seg_own, seg_oth, seg_ctx]
                else:
                    qsegs = [seg_own, seg_ctx]
                mixer_phase(ws, l, src, sname, qsegs)
                if lastl:
                    sbs = [[dict(c0=0, n=HALF, which=0, dcol=0)]]
                    dst, dname = outT, "outT"
                elif fused and len(layers) > 1:
                    sbs = [[dict(c0=0, n=HALF, which=0, dcol=0)],
                           [dict(c0=HALF, n=HALF, which=0, dcol=HALF), dict(c0=SEQ, n=CTX, which=1, dcol=SEQ)]]
                    dst, dname = xb, "xb_scr"
                else:
                    sbs = [[dict(c0=0, n=HALF, which=0, dcol=0), dict(c0=SEQ, n=CTX, which=1, dcol=HALF)]]
                    dst, dname = outT, "outT"
                for sb_ in sbs:
                    k.barrier()
                    ffn_phase(ws, l, sb_, moe=(l % 2 == 1), final=lastl, dst=dst, dname=dname)
            k.finish()

        ws = WStream(k, pf=2)
        ws.scr = wsc
        k.dry = True
        whole(ws)
        k.dry = False
        ws.rewind()
        whole(ws)
        print("program: nins=%d nwait=%d" % (k.nins, k.nwait))
    return nc


def _fm(v):
    v = np.asarray(v, np.float32)
    return np.ascontiguousarray(v.reshape(-1, 128).T)


def _qk_perm(nunits):
    cols = []
    for g0 in range(0, nunits, 4):
        us = list(range(g0, min(g0 + 4, nunits)))
        for half in range(2):
            for u in us:
                for axis in range(2):
                    for f in range(16):
                        cols.append(u * 64 + axis * 32 + half * 16 + f)
    return np.array(cols)


def _consts():
    c = np.zeros((128, NCONST), np.float32)
    c[:, C_ONES:C_ONES + 128] = 1.0
    for b in range(4):
        c[32 * b:32 * b + 32, C_BD + 32 * b:C_BD + 32 * b + 32] = 1.0
    c[64, C_SEL:C_SEL + 64] = 1.0
    c[:, C_ID:C_ID + 128] = np.eye(128, dtype=np.float32)
    for e in range(8):
        c[e, C_OH + e * 128:C_OH + (e + 1) * 128] = 1.0
    return c


def _tables(tok_pos):
    tok_pos = np.asarray(tok_pos)
    n_freq = 16
    inv = (10000.0 ** (-np.arange(n_freq, dtype=np.float32) / n_freq)).astype(np.float32)
    row = (tok_pos // 64).astype(np.float32)
    col = (tok_pos % 64).astype(np.float32)
    cos = np.ones((128, len(tok_pos)), np.float32)
    sin = np.zeros((128, len(tok_pos)), np.float32)
    valid = tok_pos >= 0
    for p in range(128):
        axis = (p % 32) // 16
        f = p % 16
        pos = row if axis == 0 else col
        ang = (pos * inv[f]).astype(np.float32)
        cos[p, valid] = np.cos(ang[valid])
        sin[p, valid] = np.sin(ang[valid])
    return cos, sin


def _prep_shared(inp):
    sh = {}
    w_in = np.asarray(inp["w_in"], np.float32)
    perm = np.arange(6400)
    perm[OFF_AQ:OFF_AQ + 512] = OFF_AQ + _qk_perm(8)
    perm[OFF_AK:OFF_AK + 128] = OFF_AK + _qk_perm(2)
    perm[OFF_CQ:OFF_CQ + 512] = OFF_CQ + _qk_perm(8)
    perm[OFF_CK:OFF_CK + 512] = OFF_CK + _qk_perm(8)
    sh["w_in"] = np.ascontiguousarray(w_in[:, :, perm])
    for nme in ("w_mod", "w_pa", "w_pb", "w_pc", "w_out", "ffn_w1", "ffn_w3", "ffn_w2", "moe_router", "moe_w1", "moe_w3",
                "moe_w2"):
        sh[nme] = np.ascontiguousarray(np.asarray(inp[nme], np.float32))
    sh["consts"] = _consts()
    return sh


def _vecs(inp, b, hf):
    v = np.zeros((128, NV), np.float32)
    p = np.arange(128)
    gidx = ((p % 32) // 16) * 32 + (p % 16)
    for l in range(DEPTH):
        o = l * VLN

        def put(name, arr):
            a, n = VL[name]
            v[:, o + a:o + a + n] = arr.reshape(128, n)
        put("b_mod", _fm(inp["b_mod"][l]))
        put("n1g", _fm(inp["norm1_g"][l]))
        put("n2g", _fm(inp["norm2_g"][l]))
        put("b_gate", _fm(inp["b_gate"][l]))
        qg = np.asarray(inp["a_qn_g"][l], np.float32)
        kg = np.asarray(inp["a_kn_g"][l], np.float32)
        put("qng", np.stack([qg[gidx], qg[gidx + 16]], axis=1))
        put("kng", np.stack([kg[gidx], kg[gidx + 16]], axis=1))
        dw = np.asarray(inp["b_dw_w"][l], np.float32)
        put("dww", np.ascontiguousarray(dw.T.reshape(4, 128, 31).transpose(1, 0, 2)))
        put("dwb", _fm(inp["b_dw_b"][l]))
        put("lng", _fm(inp["b_ln_g"][l]))
        put("lnb", _fm(inp["b_ln_b"][l]))
        put("subg", _fm(inp["c_subln_g"][l]))
        lv = np.concatenate([np.asarray(inp[n_][l], np.float32) for n_ in ("c_lq1", "c_lk1", "c_lq2", "c_lk2")])
        put("lamv", np.broadcast_to(lv[None, :], (128, 256)))
    a, n = VG["hmask"]
    hm = np.array([0, 1, 1, 0, 0] if hf == 0 else [1, 0, 0, 1, 0], np.float32)
    v[:, a:a + n] = hm[None, :]
    a, n = VG["fing"]
    v[:, a:a + n] = _fm(inp["final_g"])
    a, n = VG["cfm"]
    cf = np.stack([_fm(inp["c"][b]), _fm(inp["c_ctx"])], axis=2)
    v[:, a:a + n] = cf.reshape(128, 16)
    a, n = VG["rb"]
    v[:, a:a + n] = np.broadcast_to(np.asarray(inp["moe_router_b"][0], np.float32)[None, :], (128, 8))
    return v


def _core_order(hf):
    own = np.arange(hf * HALF, (hf + 1) * HALF)
    oth = np.arange((1 - hf) * HALF, (2 - hf) * HALF)
    return own, oth


_PROG_CACHE = {}


def _get_prog(layers, fused):
    key = (tuple(layers), fused)
    if key not in _PROG_CACHE:
        _PROG_CACHE[key] = build_program(list(layers), fused)
    return _PROG_CACHE[key]


def _run(layers, fused, inp, sh, xT_cores):
    in_maps = []
    for core in range(8):
        b, hf = core // 2, core % 2
        own, oth = _core_order(hf)
        pos = np.concatenate([own, oth, -np.ones(CTX, np.int64)])
        cos, sin = _tables(pos)
        m = dict(xT=xT_cores[core], cosk=cos, sink=sin, vecs=_vecs(inp, b, hf), consts=sh["consts"])
        for nme in ("w_mod", "w_in", "w_pa", "w_pb", "w_pc", "w_out"):
            m[nme] = sh[nme]
        if 0 in layers:
            for nme in ("ffn_w1", "ffn_w3", "ffn_w2"):
                m[nme] = sh[nme]
        if 1 in layers:
            for nme in ("moe_router", "moe_w1", "moe_w3", "moe_w2"):
                m[nme] = sh[nme]
        in_maps.append(m)
    nc = _get_prog(layers, fused)
    res = run_bass_kernel_spmd(nc, in_maps, core_ids=list(range(8)))
    return res.results


def kernel(**inp):
    x = np.asarray(inp["x"], np.float32)
    ctx = np.asarray(inp["ctx"], np.float32)
    sh = _prep_shared(inp)
    xT_cores = []
    for core in range(8):
        b, hf = core // 2, core % 2
        own, oth = _core_order(hf)
        xt = np.concatenate([x[b][own], x[b][oth], ctx[b]], axis=0)
        xT_cores.append(np.ascontiguousarray(xt.T))
    if FUSED:
        res = _run([0, 1], True, inp, sh, xT_cores)
    else:
        r0 = _run([0], False, inp, sh, xT_cores)
        xT1 = []
        for core in range(8):
            mate = core ^ 1
            xT1.append(np.ascontiguousarray(np.concatenate(
                [r0[core]["x1T"][:, :HALF], r0[mate]["x1T"][:, :HALF], r0[core]["x1T"][:, HALF:]], axis=1)))
        res = _run([1], False, inp, sh, xT1)
    out = np.zeros((4, SEQ, D), np.float32)
    for core in range(8):
        b, hf = core // 2, core % 2
        out[b, hf * HALF:(hf + 1) * HALF, :] = res[core]["outT"].T
    return out
```

```python
import math
import numpy as np
from contextlib import ExitStack
import concourse.bass as bass
import concourse.mybir as mybir
from concourse.bass_utils import run_bass_kernel_spmd

F32 = mybir.dt.float32
BF16 = mybir.dt.bfloat16
AF = mybir.ActivationFunctionType
ALU = mybir.AluOpType
AX = mybir.AxisListType

D = 1024
SEQ = 4096
HALF = 2048
CTX = 256
NTOK = SEQ + CTX
DEPTH = 2
EPS = 1e-6
QB = 256
EXT = QB + 30
NKB = NTOK // 128
D_FF = 2816
N_EXP = 8
D_FFE = 3584
OFF_AQ, OFF_AK, OFF_AV, OFF_BZ, OFF_CQ, OFF_CK, OFF_CV, OFF_GT = 0, 512, 640, 768, 1792, 2304, 2816, 3328
FUSED = True
DEBUG = False
ARENA = 99500

VL = dict(b_mod=(0, 48), n1g=(48, 8), n2g=(56, 8), b_gate=(64, 24), qng=(88, 2), kng=(90, 2), dww=(92, 124),
          dwb=(216, 4), lng=(220, 4), lnb=(224, 4), subg=(228, 1), lamv=(229, 256))
VLN = 485
VG = dict(hmask=(2 * VLN, 5), fing=(2 * VLN + 5, 8), cfm=(2 * VLN + 13, 16), rb=(2 * VLN + 29, 8))
NV = 2 * VLN + 37
C_ONES, C_BD, C_SEL, C_ID, C_OH = 0, 128, 256, 320, 448
NCONST = 448 + 1024


class Buf:
    __slots__ = ("name", "w", "r")

    def __init__(self, name):
        self.name = name
        self.w = None
        self.r = {}


class Ev:
    __slots__ = ("sem", "val", "clk")

    def __init__(self, sem, val, clk):
        self.sem = sem
        self.val = val
        self.clk = clk


class K:
    ENGS = ("pe", "act", "dve", "pool", "sp")

    def __init__(self, nc, stack, n_dma_sems=32):
        self.nc = nc
        self.stack = stack
        self.e = dict(pe=nc.tensor, act=nc.scalar, dve=nc.vector, pool=nc.gpsimd, sp=nc.sync)
        self.sem, self.cnt, self.clk = {}, {}, {}
        for k in self.ENGS:
            self.sem[k] = stack.enter_context(nc.semaphore("s_" + k))
            self.cnt[k] = 0
            self.clk[k] = {}
        self.dsem = [stack.enter_context(nc.semaphore("d%d" % i)) for i in range(n_dma_sems)]
        self.dcnt = [0] * n_dma_sems
        self.dlast = [None] * n_dma_sems
        self.dnext = 0
        self.nwait = 0
        self.nins = 0
        self.dry = False

    def sb(self, name, shape, dt):
        return self.stack.enter_context(self.nc.sbuf_tensor(name, list(shape), dt))

    def ps(self, name, shape, dt=F32):
        return self.stack.enter_context(self.nc.psum_tensor(name, list(shape), dt))

    def _need(self, eng, ev):
        if ev is None:
            return
        c = self.clk[eng]
        if c.get(ev.sem, 0) >= ev.val:
            return
        if eng == "pe" and ev.sem is self.sem["pe"]:
            return
        self.e[eng].wait_ge(ev.sem, ev.val)
        self.nwait += 1
        for s, v in ev.clk.items():
            if c.get(s, 0) < v:
                c[s] = v
        if c.get(ev.sem, 0) < ev.val:
            c[ev.sem] = ev.val

    def _deps(self, eng, reads, writes):
        for b in reads:
            self._need(eng, b.w)
        for b in writes:
            self._need(eng, b.w)
            for ev in b.r.values():
                self._need(eng, ev)

    def _commit(self, ev, reads, writes):
        for b in writes:
            b.w = ev
            b.r = {}
        for b in reads:
            if b.w is ev:
                continue
            b.r[ev.sem] = ev

    def op(self, eng, fn, reads=(), writes=()):
        if self.dry:
            return None
        self._deps(eng, reads, writes)
        ins = fn(self.e[eng])
        self.cnt[eng] += 1
        ins.then_inc(self.sem[eng], 1)
        self.nins += 1
        clk = dict(self.clk[eng])
        clk[self.sem[eng]] = self.cnt[eng]
        ev = Ev(self.sem[eng], self.cnt[eng], clk)
        self._commit(ev, reads, writes)
        return ev

    def dma(self, eng, out, in_, reads=(), writes=(), **kw):
        if self.dry:
            return None
        i = self.dnext
        self.dnext = (self.dnext + 1) % len(self.dsem)
        self._need(eng, self.dlast[i])
        self._deps(eng, reads, writes)
        ins = self.e[eng].dma_start(out=out, in_=in_, **kw)
        self.dcnt[i] += 16
        ins.then_inc(self.dsem[i], 16)
        self.nins += 1
        clk = dict(self.clk[eng])
        clk[self.dsem[i]] = self.dcnt[i]
        ev = Ev(self.dsem[i], self.dcnt[i], clk)
        self.dlast[i] = ev
        self._commit(ev, reads, writes)
        return ev

    def barrier(self, engs=None):
        if self.dry:
            return
        for e in (engs or self.ENGS):
            for f in self.ENGS:
                if f != e and self.cnt[f] > 0:
                    self._need(e, Ev(self.sem[f], self.cnt[f], {}))
            for ev in self.dlast:
                self._need(e, ev)

    def finish(self):
        self.barrier(engs=("sp",))


class Ring:
    def __init__(self, items):
        self.items = items
        self.i = 0

    def next(self):
        it = self.items[self.i % len(self.items)]
        self.i += 1
        return it


class WStream:
    def __init__(self, k, pf):
        self.k = k
        self.pf = pf
        self.reqs = []
        self.issued = 0
        self.pos = 0
        self.phase = 0
        self.pstart = 0
        self.slots = None
        self.scr = None
        self.sidx = {}
        self.sbuf = {}
        self.done = set()

    def rewind(self):
        self.issued = 0
        self.pos = 0
        self.phase = 0
        self.pstart = 0
        self.pend = {}
        for j, r in enumerate(self.reqs):
            self.pend[r[3]] = j + 1
        self.done = set()

    def new_phase(self, slots, pf):
        self.phase += 1
        self.pf = pf
        self.slots = slots
        self.pstart = self.pos
        assert self.issued <= self.pos or self.k.dry

    def _issue(self, j):
        ap, kc, n, ph, key = self.reqs[j]
        assert ph == self.phase
        t, B = self.slots[(j - self.pstart) % len(self.slots)]
        view = t[:, 0:kc * n].rearrange("p (c n) -> p c n", n=n)
        if key is None:
            self.k.dma("pool", view, ap, writes=[B])
            return
        if key not in self.sidx:
            self.sidx[key] = len(self.sidx)
            self.sbuf[key] = Buf("wsc_%d" % self.sidx[key])
        SB = self.sbuf[key]
        sap = self.scr[self.sidx[key]][:, 0:kc * n]
        if key in self.done:
            self.k.dma("sp", t[:, 0:kc * n], sap, reads=[SB], writes=[B])
        else:
            self.k.dma("pool", view, ap, writes=[B])
            self.k.dma("sp", sap, t[:, 0:kc * n], reads=[B], writes=[SB])
            self.done.add(key)

    def get(self, ap, kc, n, key=None):
        if self.k.dry:
            self.reqs.append((ap, kc, n, self.phase, key))
            self.pos += 1
            t, B = self.slots[0]
            return t[:, 0:kc * n].rearrange("p (c n) -> p c n", n=n), B
        j = self.pos
        self.pos += 1
        lim = min(self.pend[self.phase], j + 1 + self.pf)
        while self.issued < lim:
            self._issue(self.issued)
            self.issued += 1
        t, B = self.slots[(j - self.pstart) % len(self.slots)]
        return t[:, 0:kc * n].rearrange("p (c n) -> p c n", n=n), B


def build_program(layers, fused):
    nc = bass.Bass("TRN2", target_bir_lowering=False)
    dt = nc.dram_tensor
    I = {}

    def inp(name, shape):
        I[name] = dt(name, list(shape), F32, kind="ExternalInput").ap()
        return I[name]

    xT = inp("xT", [D, NTOK])
    cosk = inp("cosk", [128, NTOK])
    sink = inp("sink", [128, NTOK])
    vecs = inp("vecs", [128, NV])
    consts = inp("consts", [128, NCONST])
    w_mod = inp("w_mod", [DEPTH, D, 6 * D])
    w_in = inp("w_in", [DEPTH, D, 6400])
    w_pa = inp("w_pa", [DEPTH, 512, D])
    w_pb = inp("w_pb", [DEPTH, 512, D])
    w_pc = inp("w_pc", [DEPTH, 512, D])
    w_out = inp("w_out", [DEPTH, D, D])
    if 0 in layers:
        ffn_w1 = inp("ffn_w1", [1, D, D_FF])
        ffn_w3 = inp("ffn_w3", [1, D, D_FF])
        ffn_w2 = inp("ffn_w2", [1, D_FF, D])
    if 1 in layers:
        moe_r = inp("moe_router", [1, D, N_EXP])
        moe_w1 = inp("moe_w1", [1, N_EXP, D, D_FFE])
        moe_w3 = inp("moe_w3", [1, N_EXP, D, D_FFE])
        moe_w2 = inp("moe_w2", [1, N_EXP, D_FFE, D])
    last = layers[-1] == DEPTH - 1
    if last:
        outT = dt("outT", [D, HALF], F32, kind="ExternalOutput").ap()
    else:
        outT = dt("x1T", [D, HALF + CTX], F32, kind="ExternalOutput").ap()
    if DEBUG:
        dbg16 = dt("dbg16", [128, 36 * QB], BF16, kind="ExternalOutput").ap()
        dbg32 = dt("dbg32", [128, 16 * EXT], F32, kind="ExternalOutput").ap()
    xm = dt("xm_scr", [D, NTOK], F32).ap()
    xb = dt("xb_scr", [D, NTOK], F32).ap()
    wsc = dt("wsc_scr", [DEPTH * 44, 128, 4096], BF16).ap()

    dbuf = {}

    def DB(name, c0, c1):
        out = []
        for c in range(c0 // 128, (c1 + 127) // 128):
            key = (name, c)
            if key not in dbuf:
                dbuf[key] = Buf("%s_%d" % key)
            out.append(dbuf[key])
        return out

    def fm(ap2d):
        return ap2d.rearrange("(c p) n -> p c n", p=128)

    with ExitStack() as st:
        k = K(nc, st)
        vec = k.sb("vec", [128, NV], F32); Bvec = Buf("vec")
        cst = k.sb("cst", [128, NCONST], F32); Bcst = Buf("cst")
        ones16 = k.sb("ones16", [128, 128], BF16); Bo16 = Buf("ones16")
        modT = k.sb("modT", [128, DEPTH * 96], F32); Bmod = Buf("modT")
        dv = k.sb("dv", [128, DEPTH * 2 * 6 * 8], F32); Bdv = Buf("dv")
        lamt = k.sb("lamt", [128, DEPTH * 8 + 64], F32); Blam = Buf("lamt")
        csb = k.sb("csb", [128, 16], BF16); Bcsb = Buf("csb")
        arena = k.sb("arena", [128, ARENA], BF16)
        banks = [(k.ps("bank%d" % i, [128, 512]), Buf("bank%d" % i)) for i in range(8)]

        k.dma("sp", vec[:], vecs, writes=[Bvec])
        k.dma("sp", cst[:], consts, writes=[Bcst])
        k.op("dve", lambda e: e.memset(ones16[:], 1.0), writes=[Bo16])
        ones32 = cst[:, C_ONES:C_ONES + 128]
        bd32 = cst[:, C_BD:C_BD + 128]
        sel65 = cst[:, C_SEL:C_SEL + 64]
        ident = cst[:, C_ID:C_ID + 128]

        def V(l, name, j=0, n=1):
            o, ln = VL[name]
            return vec[:, l * VLN + o + j: l * VLN + o + j + n]

        def VGl(name, j=0, n=1):
            o, ln = VG[name]
            return vec[:, o + j: o + j + n]

        def DV(l, which, kind, c):
            o = ((l * 2 + which) * 6 + kind) * 8 + c
            return dv[:, o:o + 1]

        class Carver:
            def __init__(self):
                self.off = 0

            def take(self, nbytes, dtype, name):
                nb = (nbytes + 63) // 64 * 64
                a = arena[:, self.off // 2:(self.off + nb) // 2]
                self.off += nb
                assert self.off <= ARENA * 2, (name, self.off)
                if dtype is F32:
                    a = a.bitcast(F32)
                    return a[:, 0:nbytes // 4]
                return a[:, 0:nbytes // 2]

        epsc = k.sb("epsc", [128, 1], F32); Beps = Buf("epsc")
        fz = k.sb("fz", [128, 1], F32); Bfz = Buf("fz")
        k.op("dve", lambda e: e.memset(epsc[:], EPS), writes=[Beps])

        def rsqrt_from(out, Bout, src_, Bsrc, scale, P):
            k.op("act", lambda e: e.activation(out, src_, AF.Ln, bias=epsc[0:P, :], scale=scale), reads=[Bsrc, Beps],
                 writes=[Bout])
            k.op("act", lambda e: e.activation(out, out, AF.Exp, scale=-0.5), reads=[Bout], writes=[Bout])

        def mm(bank_ap, lhsT, rhs, start, stop, reads, Bbank, skip=False):
            k.op("pe", lambda e: e.matmul(bank_ap, lhsT, rhs, start=start, stop=stop, skip_group_check=skip), reads=reads,
                 writes=[Bbank])

        def mod_phase(ws, l):
            bank, Bb = banks[0]
            for g in range(12):
                wt, Bw = ws.get(fm(w_mod[l])[:, :, g * 512:(g + 1) * 512], 8, 512)
                for nl in range(4):
                    j = g * 4 + nl
                    for c in range(8):
                        mm(bank[:, 2 * j:2 * j + 2], wt[:, c, nl * 128:(nl + 1) * 128], csb[:, 2 * c:2 * c + 2],
                           c == 0, c == 7, [Bw, Bcsb], Bb)
            mv = modT[:, l * 96:(l + 1) * 96].rearrange("p (j w) -> p j w", w=2)
            bv = bank[:, 0:96].rearrange("p (j w) -> p j w", w=2)
            for w in range(2):
                k.op("dve", lambda e: e.tensor_tensor(mv[:, :, w], bv[:, :, w], V(l, "b_mod", 0, 48), ALU.add),
                     reads=[Bb, Bvec], writes=[Bmod])
            for w in range(2):
                for sub, (sh, sc, gt, ng) in enumerate(((0, 1, 2, "n1g"), (3, 4, 5, "n2g"))):
                    o = ((l * 2 + w) * 6 + sub * 3) * 8
                    k.op("dve", lambda e: e.scalar_tensor_tensor(dv[:, o:o + 8], mv[:, sc * 8:(sc + 1) * 8, w], 1.0,
                                                                 V(l, ng, 0, 8), ALU.add, ALU.mult),
                         reads=[Bmod, Bvec], writes=[Bdv])
                    k.op("dve", lambda e: e.tensor_copy(dv[:, o + 8:o + 16], mv[:, sh * 8:(sh + 1) * 8, w]),
                         reads=[Bmod], writes=[Bdv])
                    k.op("dve", lambda e: e.tensor_copy(dv[:, o + 16:o + 24], mv[:, gt * 8:(gt + 1) * 8, w]),
                         reads=[Bmod], writes=[Bdv])
            lam_init = 0.8 - 0.6 * math.exp(-0.3 * l)
            lo = l * 8
            tmp = lamt[:, DEPTH * 8:DEPTH * 8 + 64]
            for j in range(2):
                k.op("dve", lambda e: e.tensor_tensor(tmp, V(l, "lamv", j * 128, 64), V(l, "lamv", j * 128 + 64, 64),
                                                      ALU.mult), reads=[Bvec], writes=[Blam])
                k.op("dve", lambda e: e.reduce_sum(lamt[:, lo + j:lo + j + 1], tmp, AX.X), reads=[Blam], writes=[Blam])
            k.op("act", lambda e: e.activation(lamt[:, lo + 2:lo + 4], lamt[:, lo:lo + 2], AF.Exp), reads=[Blam],
                 writes=[Blam])
            k.op("dve", lambda e: e.tensor_tensor(lamt[:, lo + 4:lo + 5], lamt[:, lo + 3:lo + 4], lamt[:, lo + 2:lo + 3],
                                                  ALU.subtract), reads=[Blam], writes=[Blam])
            k.op("dve", lambda e: e.tensor_scalar(lamt[:, lo + 5:lo + 6], lamt[:, lo + 4:lo + 5], -lam_init, None,
                                                  ALU.add), reads=[Blam], writes=[Blam])
            k.op("dve", lambda e: e.tensor_scalar(lamt[:, lo + 6:lo + 7], V(l, "subg"), 1.0 - lam_init, None, ALU.mult),
                 reads=[Bvec], writes=[Blam])

        def norm_stats(T, xv, Bx, n):
            bank, Bb = T["pp"].next()
            for c in range(8):
                sq, Bs = T["sq"].next()
                k.op("act", lambda e: e.activation(sq[:, :n], xv[:, c, :], AF.Square), reads=[Bx], writes=[Bs])
                mm(bank[:, :n], ones32, sq[:, :n], c == 0, c == 7, [Bs, Bcst], Bb)
            rs, Br = T["rs"].next()
            rsqrt_from(rs[:, :n], Br, bank[:, :n], Bb, 1.0 / D, 128)
            return rs, Br

        def norm_block(T, xv, Bx, n, Acol, Bcol, out, Bout, out2=None, Bout2=None, rs_pre=None):
            rs, Br = rs_pre if rs_pre is not None else norm_stats(T, xv, Bx, n)
            for c in range(8):
                if Bcol is None:
                    k.op("dve", lambda e: e.scalar_tensor_tensor(out[:, c, :], xv[:, c, :], Acol(c), rs[:, :n], ALU.mult,
                                                                 ALU.mult), reads=[Bx, Br, Bdv, Bvec], writes=[Bout])
                    continue
                tmp, Bt = T["tmp"].next()
                k.op("dve", lambda e: e.scalar_tensor_tensor(tmp[:, :n], xv[:, c, :], Acol(c), rs[:, :n], ALU.mult,
                                                             ALU.mult), reads=[Bx, Br, Bdv], writes=[Bt])
                k.op("act", lambda e: e.activation(out[:, c, :], tmp[:, :n], AF.Identity, bias=Bcol(c)),
                     reads=[Bt, Bdv], writes=[Bout])
                if out2 is not None:
                    k.op("act", lambda e: e.activation(out2[:, c, :], tmp[:, :n], AF.Identity, bias=Bcol(c)),
                         reads=[Bt, Bdv], writes=[Bout2])

        def rope_pair(T, bA, BA, bB, BB, P, n, cs, sn, Bcs, norm_g, dests):
            if norm_g is not None:
                gA, gB = norm_g
                sA, BsA = T["sq"].next()
                sB, BsB = T["sq"].next()
                k.op("act", lambda e: e.activation(sA[0:P, :n], bA[0:P, :n], AF.Square), reads=[BA], writes=[BsA])
                k.op("act", lambda e: e.activation(sB[0:P, :n], bB[0:P, :n], AF.Square), reads=[BB], writes=[BsB])
                bank, Bb = T["pp"].next()
                mm(bank[0:P, :n], bd32[0:P, 0:P], sA[0:P, :n], True, False, [BsA, Bcst], Bb)
                mm(bank[0:P, :n], bd32[0:P, 0:P], sB[0:P, :n], False, True, [BsB, Bcst], Bb)
                rs, Br = T["rs"].next()
                rsqrt_from(rs[0:P, :n], Br, bank[0:P, :n], Bb, 1.0 / 64, P)
                nA, BnA = T["tmp"].next()
                nB, BnB = T["tmp"].next()
                k.op("dve", lambda e: e.scalar_tensor_tensor(nA[0:P, :n], bA[0:P, :n], gA[0:P], rs[0:P, :n], ALU.mult,
                                                             ALU.mult), reads=[BA, Br, Bvec], writes=[BnA])
                k.op("dve", lambda e: e.scalar_tensor_tensor(nB[0:P, :n], bB[0:P, :n], gB[0:P], rs[0:P, :n], ALU.mult,
                                                             ALU.mult), reads=[BB, Br, Bvec], writes=[BnB])
                srcA, BsrcA, srcB, BsrcB = nA, BnA, nB, BnB
            else:
                srcA, BsrcA, srcB, BsrcB = bA, BA, bB, BB
            t1, B1 = T["rp"].next()
            t2, B2 = T["rp"].next()
            t3, B3 = T["rp"].next()
            t4, B4 = T["rp"].next()
            k.op("dve", lambda e: e.tensor_tensor(t1[0:P, :n], srcA[0:P, :n], cs[0:P, :n], ALU.mult),
                 reads=[BsrcA, Bcs], writes=[B1])
            k.op("dve", lambda e: e.tensor_tensor(t2[0:P, :n], srcB[0:P, :n], sn[0:P, :n], ALU.mult),
                 reads=[BsrcB, Bcs], writes=[B2])
            k.op("dve", lambda e: e.tensor_tensor(t3[0:P, :n], srcB[0:P, :n], cs[0:P, :n], ALU.mult),
                 reads=[BsrcB, Bcs], writes=[B3])
            k.op("dve", lambda e: e.tensor_tensor(t4[0:P, :n], srcA[0:P, :n], sn[0:P, :n], ALU.mult),
                 reads=[BsrcA, Bcs], writes=[B4])
            for (slo, dl) in dests:
                for (dtile, Bd, dlo) in dl:
                    e1 = "dve" if (slo != 96 and dlo != 96) else "pool"
                    e2 = "dve" if (slo != 96 and dlo + 32 != 96) else "pool"
                    k.op(e1, lambda e: e.tensor_tensor(dtile[dlo:dlo + 32, :n], t1[slo:slo + 32, :n],
                                                       t2[slo:slo + 32, :n], ALU.subtract),
                         reads=[B1, B2], writes=[Bd])
                    k.op(e2, lambda e: e.tensor_tensor(dtile[dlo + 32:dlo + 64, :n], t3[slo:slo + 32, :n],
                                                       t4[slo:slo + 32, :n], ALU.add),
                         reads=[B3, B4], writes=[Bd])

        def mixer_phase(ws, l, src, sname, qsegs):
            cv = Carver()
            slots = [(cv.take(8192, BF16, "ws%d" % i), Buf("ws%d" % i)) for i in range(3)]
            kTA = [(cv.take(NTOK * 2, BF16, "kTA"), Buf("kTA%d" % i)) for i in range(2)]
            kTC = [(cv.take(NTOK * 2, BF16, "kTC"), Buf("kTC%d" % i)) for i in range(4)]
            VAf = cv.take(NKB * 2 * 65 * 2, BF16, "VA")
            VA = VAf.rearrange("p (b v d) -> p b v d", v=2, d=65); BVA = Buf("VA")
            VC = cv.take(NKB * 512 * 2, BF16, "VC").rearrange("p (b n) -> p b n", n=512); BVC = Buf("VC")
            T = {}
            T["xe"] = Ring([(cv.take(8 * EXT * 4, F32, "xe").rearrange("p (c n) -> p c n", n=EXT), Buf("xe%d" % i))
                            for i in range(1)])
            hT = cv.take(8 * EXT * 2, BF16, "hT").rearrange("p (c n) -> p c n", n=EXT); BhT = Buf("hT")
            cs_t = cv.take(EXT * 4, F32, "cs"); sn_t = cv.take(EXT * 4, F32, "sn"); Bcs = Buf("cs")
            u0 = cv.off
            yx = cv.take(4 * EXT * 4, F32, "yx").rearrange("p (c n) -> p c n", n=EXT); Byx = Buf("yx")
            cacc = cv.take(4 * QB * 4, F32, "cacc").rearrange("p (c n) -> p c n", n=QB); Bcacc = [Buf("cacc%d" % i) for i in range(4)]
            osb_l = [(cv.take(512 * 4, F32, "osb"), Buf("osb%d" % i)) for i in range(2)]
            om_l = [(cv.take(512 * 4, F32, "om"), Buf("om%d" % i)) for i in range(2)]
            assert cv.off - u0 >= 8 * EXT * 6 and u0 % 64 == 0
            xs = arena[:, u0 // 2:(u0 + 8 * EXT * 4) // 2].bitcast(F32).rearrange("p (c n) -> p c n", n=EXT); Bxs = Buf("xs")
            hs = arena[:, (u0 + 8 * EXT * 4) // 2:(u0 + 8 * EXT * 6) // 2].rearrange("p (c n) -> p c n", n=EXT); Bhs = Buf("hs")
            ALIAS = [Byx] + Bcacc + [b_ for (_, b_) in osb_l + om_l]
            qTA = [(cv.take(2 * QB * 2, BF16, "qTA"), Buf("qTA%d" % i)) for i in range(4)]
            qTC = [(cv.take(2 * QB * 2, BF16, "qTC"), Buf("qTC%d" % i)) for i in range(4)]
            oaT = [(cv.take(QB * 2, BF16, "oaT"), Buf("oaT%d" % i)) for i in range(4)]
            obT = [(cv.take(QB * 2, BF16, "obT"), Buf("obT%d" % i)) for i in range(4)]
            ocT = [(cv.take(QB * 2, BF16, "ocT"), Buf("ocT%d" % i)) for i in range(4)]
            mT = cv.take(8 * QB * 2, BF16, "mT").rearrange("p (c n) -> p c n", n=QB); BmT = Buf("mT")
            macc = cv.take(8 * QB * 4, F32, "macc").rearrange("p (c n) -> p c n", n=QB); Bmacc = Buf("macc")
            T["sq"] = Ring([(cv.take(EXT * 4, F32, "sq"), Buf("sq%d" % i)) for i in range(2)])
            T["tmp"] = Ring([(cv.take(EXT * 4, F32, "tmp"), Buf("tmp%d" % i)) for i in range(3)])
            T["rs"] = Ring([(cv.take(EXT * 4, F32, "rs"), Buf("rs%d" % i)) for i in range(2)])
            ri_l = [(cv.take(512 * 4, F32, "ri"), Buf("ri%d" % i)) for i in range(2)]
            T["ri"] = Ring(ri_l)
            T["rp"] = Ring([(ri_l[i // 2][0][:, (i % 2) * QB:(i % 2 + 1) * QB], ri_l[i // 2][1]) for i in range(4)])
            T["pT"] = Ring([(cv.take(512 * 2, BF16, "pT"), Buf("pT%d" % i)) for i in range(4)])
            T["osb"] = Ring(osb_l)
            T["om"] = Ring(om_l)
            T["dd"] = Ring([(cv.take(QB * 4, F32, "dd"), Buf("dd%d" % i)) for i in range(2)])
            T["pp"] = Ring(banks[0:8])
            wl = fm(w_in[l])
            srcv = fm(src)

            k.op("pool", lambda e: e.memset(VA[:, :, :, 64:65], 1.0), writes=[BVA])
            for (t_, B_) in qTA + qTC:
                k.op("pool", lambda e: e.memset(t_[:, :], 0.0), writes=[B_])

            wkv = [(slots[0][0][:, 0:2048].rearrange("p (c n) -> p c n", n=256), Buf("wkv_akv"), wl[:, :, OFF_AK:OFF_AK + 256]),
                   (slots[0][0][:, 2048:4096].rearrange("p (c n) -> p c n", n=256), Buf("wkv_ck0"), wl[:, :, OFF_CK:OFF_CK + 256]),
                   (slots[1][0][:, 0:2048].rearrange("p (c n) -> p c n", n=256), Buf("wkv_ck1"),
                    wl[:, :, OFF_CK + 256:OFF_CK + 512]),
                   (slots[2][0][:, 0:4096].rearrange("p (c n) -> p c n", n=512), Buf("wkv_cv"), wl[:, :, OFF_CV:OFF_CV + 512])]
            for (wt_, Bw_, ap_) in wkv:
                k.dma("pool", wt_, ap_, writes=[Bw_])
            n = QB
            xe0, Bxe0 = T["xe"].items[0]
            xring = [(xe0[:, :, 0:n], Bxe0), (macc[:, :, :], Buf("xe_kv2"))]
            hring = [(hT[:, :, 0:n], BhT), (mT[:, :, :], Buf("hT_kv2"))]
            csring = [(cs_t, sn_t, Bcs), (yx[:, 0, :], yx[:, 1, :], Buf("cs_kv2"))]
            NKV = NTOK // QB

            def kv_load(kb):
                c0 = kb * QB
                xv_, Bx_ = xring[kb % 2]
                k.dma("sp", xv_, srcv[:, :, c0:c0 + n], reads=DB(sname, c0, c0 + n), writes=[Bx_])
                cs_, sn_, Bc_ = csring[kb % 2]
                k.dma("sp", cs_[:, 0:n], cosk[:, c0:c0 + n], writes=[Bc_])
                k.dma("sp", sn_[:, 0:n], sink[:, c0:c0 + n], writes=[Bc_])

            def kv_norm(kb):
                which = 1 if kb * QB >= SEQ else 0
                xv_, Bx_ = xring[kb % 2]
                hv_, Bh_ = hring[kb % 2]
                norm_block(T, xv_, Bx_, n, lambda c: DV(l, which, 0, c), lambda c: DV(l, which, 1, c), hv_, Bh_)

            kv_load(0)
            kv_norm(0)
            for kb in range(NKV):
                c0 = kb * QB
                hv, Bh = hring[kb % 2]
                cs_k, sn_k, Bcs_k = csring[kb % 2]
                if kb + 1 < NKV:
                    kv_load(kb + 1)
                wt, Bw, _ = wkv[0]
                (bA, BA), (bB, BB) = T["pp"].next(), T["pp"].next()
                for part, (bk, Bk) in enumerate(((bA, BA), (bB, BB))):
                    for c in range(8):
                        mm(bk[0:64, :n], wt[:, c, part * 64:(part + 1) * 64], hv[:, c, :], c == 0, c == 7, [Bw, Bh], Bk)
                dests = []
                for kvh in range(2):
                    t, Bt = kTA[kvh]
                    dests.append((32 * kvh, [(t[:, c0:c0 + n], Bt, 0), (t[:, c0:c0 + n], Bt, 64)]))
                rope_pair(T, bA, BA, bB, BB, 64, n, cs_k, sn_k, Bcs_k, (V(l, "kng", 0), V(l, "kng", 1)), dests)
                for sbk in range(n // 128):
                    bk, Bk = T["pp"].next()
                    for c in range(8):
                        mm(bk[:, 0:128], hv[:, c, sbk * 128:(sbk + 1) * 128], wt[:, c, 128:256], c == 0, c == 7, [Bw, Bh], Bk)
                    blk = c0 // 128 + sbk
                    k.op("act", lambda e: e.activation(VA[:, blk, :, 0:64], bk[:, 0:128].rearrange("p (v d) -> p v d", d=64),
                                                       AF.Copy), reads=[Bk], writes=[BVA])
                for pr in range(2):
                    wt, Bw, _ = wkv[1 + pr]
                    (bA, BA), (bB, BB) = T["pp"].next(), T["pp"].next()
                    for part, (bk, Bk) in enumerate(((bA, BA), (bB, BB))):
                        for c in range(8):
                            mm(bk[:, :n], wt[:, c, part * 128:(part + 1) * 128], hv[:, c, :], c == 0, c == 7, [Bw, Bh], Bk)
                    dests = []
                    for ul in range(4):
                        u = pr * 4 + ul
                        t, Bt = kTC[u // 2]
                        dests.append((32 * ul, [(t[:, c0:c0 + n], Bt, 64 * (u % 2))]))
                    rope_pair(T, bA, BA, bB, BB, 128, n, cs_k, sn_k, Bcs_k, None, dests)
                    if pr == 0 and kb + 1 < NKV:
                        kv_norm(kb + 1)
                wt, Bw, _ = wkv[3]
                for sbk in range(n // 128):
                    bk, Bk = T["pp"].next()
                    for c in range(8):
                        mm(bk[:, 0:512], hv[:, c, sbk * 128:(sbk + 1) * 128], wt[:, c, :], c == 0, c == 7, [Bw, Bh], Bk)
                    blk = c0 // 128 + sbk
                    k.op("act", lambda e: e.activation(VC[:, blk, :], bk[:, 0:512], AF.Copy), reads=[Bk], writes=[BVC])

            k.barrier()
            qslots = [(slots[i // 2][0][:, (i % 2) * 2048:(i % 2 + 1) * 2048], Buf("qws%d" % i)) for i in range(6)]
            ws.new_phase(qslots, 4)
            for seg in qsegs:
                which = seg["which"]
                nblk = seg["n"] // QB
                for qb in range(nblk):
                    c0 = seg["c0"] + qb * QB
                    n = QB
                    xe, Bxe = T["xe"].next()

                    def load_x(dst, Bdst, qb_, q_="sp"):
                        c0_ = seg["c0"] + qb_ * QB
                        if 0 < qb_ < nblk - 1:
                            k.dma(q_, dst[:, :, :], srcv[:, :, c0_ - 15:c0_ + n + 15], reads=DB(sname, c0_ - 15, c0_ + n + 15),
                                  writes=[Bdst])
                            return
                        k.dma(q_, dst[:, :, 15:15 + n], srcv[:, :, c0_:c0_ + n], reads=DB(sname, c0_, c0_ + n), writes=[Bdst])
                        lcol = seg["hl_col"] if qb_ == 0 else c0_ - 15
                        rcol = seg["hr_col"] if qb_ == nblk - 1 else c0_ + n
                        k.dma(q_, dst[:, :, 0:15], srcv[:, :, lcol:lcol + 15], reads=DB(sname, lcol, lcol + 15), writes=[Bdst])
                        k.dma(q_, dst[:, :, 15 + n:30 + n], srcv[:, :, rcol:rcol + 15], reads=DB(sname, rcol, rcol + 15),
                              writes=[Bdst])

                    if qb == 0:
                        load_x(xe, Bxe, qb)
                    k.dma("sp", cs_t[:, 0:n], cosk[:, c0:c0 + n], writes=[Bcs])
                    k.dma("sp", sn_t[:, 0:n], sink[:, c0:c0 + n], writes=[Bcs])
                    if qb == 0:
                        norm_block(T, xe, Bxe, EXT, lambda c: DV(l, which, 0, c), lambda c: DV(l, which, 1, c), hT, BhT)
                    hm = hT[:, :, 15:15 + n]
                    for j in range(4):
                        if j % 2 == 0:
                            jj = j // 2
                            wa, Bwa = ws.get(wl[:, :, OFF_BZ + jj * 256:OFF_BZ + (jj + 1) * 256], 8, 256, key=(l, "bza", jj))
                            wg, Bwg = ws.get(wl[:, :, OFF_BZ + 512 + jj * 256:OFF_BZ + 512 + (jj + 1) * 256], 8, 256,
                                             key=(l, "bzg", jj))
                        j2 = j % 2
                        (ba, Ba), (bg, Bg) = T["pp"].next(), T["pp"].next()
                        for c in range(8):
                            mm(ba[:, :EXT], wa[:, c, j2 * 128:(j2 + 1) * 128], hT[:, c, :], c == 0, c == 7, [Bwa, BhT], Ba)
                        for c in range(8):
                            mm(bg[:, :EXT], wg[:, c, j2 * 128:(j2 + 1) * 128], hT[:, c, :], c == 0, c == 7, [Bwg, BhT], Bg)
                        sg, Bsg = T["tmp"].next()
                        k.op("act", lambda e: e.activation(sg[:, :EXT], bg[:, :EXT], AF.Sigmoid), reads=[Bg], writes=[Bsg])
                        k.op("dve", lambda e: e.tensor_tensor(yx[:, j, :], ba[:, :EXT], sg[:, :EXT], ALU.mult),
                             reads=[Ba, Bsg], writes=[Byx])
                    if qb == 0:
                        mcol = VGl("hmask", seg["hl_mask"])
                        k.op("pool", lambda e: e.tensor_scalar(yx[:, :, 0:15], yx[:, :, 0:15], mcol, None, ALU.mult),
                             reads=[Byx, Bvec], writes=[Byx])
                    if qb == nblk - 1:
                        mcol = VGl("hmask", seg["hr_mask"])
                        k.op("pool", lambda e: e.tensor_scalar(yx[:, :, 15 + n:30 + n], yx[:, :, 15 + n:30 + n], mcol, None,
                                                               ALU.mult), reads=[Byx, Bvec], writes=[Byx])
                    side = []

                    def conv_tap(j, t):
                        if t == 0:
                            k.op("dve", lambda e: e.tensor_scalar(cacc[:, j, :], yx[:, j, 0:n], V(l, "dww", j * 31),
                                                                  V(l, "dwb", j), ALU.mult, ALU.add),
                                 reads=[Byx, Bvec], writes=[Bcacc[j]])
                        else:
                            k.op("dve", lambda e: e.scalar_tensor_tensor(cacc[:, j, :], yx[:, j, t:t + n],
                                                                         V(l, "dww", j * 31 + t), cacc[:, j, :], ALU.mult,
                                                                         ALU.add), reads=[Byx, Bvec, Bcacc[j]], writes=[Bcacc[j]])
                    for t in range(31):
                        for j in range(4):
                            side.append(lambda j=j, t=t: conv_tap(j, t))
                    mean, rstd, m2, d1 = yx[:, 0, 0:n], yx[:, 1, 0:n], yx[:, 2, 0:n], yx[:, 3, 0:n]

                    lnst = {}

                    def ln_sq(j0):
                        if j0 == 0:
                            lnst["bank"] = T["pp"].next()
                        for j in (j0, j0 + 1):
                            sq, Bs = T["sq"].next()
                            lnst[j] = (sq, Bs)
                            k.op("act", lambda e: e.activation(sq[:, :n], cacc[:, j, :], AF.Square), reads=[Bcacc[j]], writes=[Bs])

                    def ln_mm(j0):
                        bfull, B1 = lnst["bank"]
                        b1, b2, B2 = bfull[:, 0:n], bfull[:, n:2 * n], B1
                        for j in (j0, j0 + 1):
                            sq, Bs = lnst[j]
                            mm(b1[:, :n], ones32, cacc[:, j, :], j == 0, j == 3, [Bcacc[j], Bcst], B1, skip=True)
                            mm(b2[:, :n], ones32, sq[:, :n], False, j == 3, [Bs, Bcst], B2, skip=True)

                    def ln_var():
                        bfull, B1 = lnst["bank"]
                        b1, b2, B2 = bfull[:, 0:n], bfull[:, n:2 * n], B1
                        k.op("dve", lambda e: e.tensor_scalar(mean, b1[:, :n], 1.0 / 512, None, ALU.mult),
                             reads=[B1], writes=[Byx])
                        k.op("dve", lambda e: e.tensor_tensor(m2, mean, mean, ALU.mult), reads=[Byx], writes=[Byx])
                        k.op("dve", lambda e: e.scalar_tensor_tensor(rstd, b2[:, :n], 1.0 / 512, m2, ALU.mult,
                                                                     ALU.subtract), reads=[B2, Byx], writes=[Byx])

                    def ln_rs():
                        rsqrt_from(rstd, Byx, rstd, Byx, 1.0, 128)

                    def ln_sub():
                        for j in range(4):
                            k.op("pool", lambda e: e.tensor_tensor(cacc[:, j, :], cacc[:, j, :], mean, ALU.subtract),
                                 reads=[Bcacc[j], Byx], writes=[Bcacc[j]])

                    def ln_mul():
                        for j in range(4):
                            k.op("dve", lambda e: e.tensor_tensor(cacc[:, j, :], cacc[:, j, :], rstd, ALU.mult),
                                 reads=[Bcacc[j], Byx], writes=[Bcacc[j]])

                    def ln_act():
                        for j in range(4):
                            ot, Bot = obT[j]
                            k.op("act", lambda e: e.activation(ot[:, :n], cacc[:, j, :], AF.Silu, bias=V(l, "lnb", j),
                                                               scale=V(l, "lng", j)), reads=[Bcacc[j], Bvec], writes=[Bot])

                    def ln_mm0_sq2():
                        ln_mm(0)
                        ln_sq(2)
                    side += [None] * 21
                    side += [lambda: ln_sq(0), None, ln_mm0_sq2, None, lambda: ln_mm(2), None, None, ln_var, None, None, ln_rs,
                             None, ln_sub, None, None, ln_mul, None, None, ln_act]
                    for (off, qT, ng) in ((OFF_AQ, qTA, (V(l, "qng", 0), V(l, "qng", 1))), (OFF_CQ, qTC, None)):
                        for pr in range(2):
                            wt, Bw = ws.get(wl[:, :, off + pr * 256:off + (pr + 1) * 256], 8, 256, key=(l, "q", off, pr))
                            (bA, BA), (bB, BB) = T["pp"].next(), T["pp"].next()
                            for part, (bk, Bk) in enumerate(((bA, BA), (bB, BB))):
                                for c in range(8):
                                    mm(bk[:, :n], wt[:, c, part * 128:(part + 1) * 128], hm[:, c, :], c == 0, c == 7,
                                       [Bw, BhT], Bk)
                            dests = []
                            for ul in range(4):
                                u = pr * 4 + ul
                                t, Bt = qT[u // 2]
                                dests.append((32 * ul, [(t[:, (u % 2) * n:(u % 2 + 1) * n], Bt, 64 * (u % 2))]))
                            rope_pair(T, bA, BA, bB, BB, 128, n, cs_t, sn_t, Bcs, ng, dests)
                    kbl = [2 * kp + i for kp in seg["keys"] for i in range(2)]
                    NP = len(kbl)
                    n2 = 2 * n
                    units = [("A", hp) for hp in range(4)] + [("C", hc) for hc in range(4)]
                    flat = [(ui, pi) for ui in range(len(units)) for pi in range(NP)]
                    pp_saved = T["pp"]
                    T["pp"] = Ring(banks[0:1]); T["st"] = Ring(banks[1:4]); T["ac"] = Ring(banks[4:8])
                    stq = {}

                    def emit_qk(s_):
                        ui, pi = flat[s_]
                        kind, a_ = units[ui]
                        (qt, Bq) = qTA[a_] if kind == "A" else qTC[a_]
                        (kt, Bkt) = kTA[a_ // 2] if kind == "A" else kTC[a_]
                        stb, Bst = T["st"].next()
                        kbk = kbl[pi]
                        mm(stb[:, 0:n2], kt[:, kbk * 128:(kbk + 1) * 128], qt[:, 0:n2], True, True, [Bkt, Bq], Bst)
                        stq[s_] = (stb, Bst)

                    pend = []

                    def flush(all_=False):
                        for it in pend:
                            it[0] -= 1
                        while pend and (all_ or pend[0][0] <= 0):
                            pend.pop(0)[1]()

                    def epi_A(hp, acc, Bacc):
                        if pend:
                            flush(True)
                        osb, Bosb = T["osb"].next()
                        k.op("dve", lambda e: e.tensor_copy(osb[0:65, 0:n2], acc[0:65, 0:n2]), reads=[Bacc], writes=[Bosb])
                        st_ = {}

                        def p1():
                            rb_, Brb = T["pp"].next()
                            st_["rb"] = (rb_, Brb)
                            mm(rb_[0:64, 0:n2], sel65[0:65, :], osb[0:65, 0:n2], True, True, [Bosb, Bcst], Brb)

                        def p2():
                            rb_, Brb = st_["rb"]
                            ri, Bri = T["ri"].next()
                            k.op("act", lambda e: e.activation(ri[0:64, 0:n2], rb_[0:64, 0:n2], AF.Ln), reads=[Brb], writes=[Bri])
                            k.op("act", lambda e: e.activation(ri[0:64, 0:n2], ri[0:64, 0:n2], AF.Exp, scale=-1.0),
                                 reads=[Bri], writes=[Bri])
                            ot, Bot = oaT[hp]
                            for hh in range(2):
                                k.op("dve", lambda e: e.tensor_tensor(ot[64 * hh:64 * hh + 64, :n], osb[0:64, hh * n:(hh + 1) * n],
                                                                      ri[0:64, hh * n:(hh + 1) * n], ALU.mult),
                                     reads=[Bosb, Bri], writes=[Bot])
                        pend.append([4, p1])
                        pend.append([6, p2])

                    def epi_C(hc, acc, Bacc, rsk, Brsk):
                        if pend:
                            flush(True)
                        st_ = {}

                        def p0():
                            ri, Bri = T["ri"].next()
                            k.op("act", lambda e: e.activation(ri[:, 0:n2], rsk[:, 0:n2], AF.Ln), reads=[Brsk], writes=[Bri])
                            k.op("act", lambda e: e.activation(ri[:, 0:n2], ri[:, 0:n2], AF.Exp, scale=-1.0), reads=[Bri],
                                 writes=[Bri])
                            om, Bom = T["om"].next()
                            k.op("dve", lambda e: e.tensor_tensor(om[:, 0:n2], acc[:, 0:n2], ri[:, 0:n2], ALU.mult),
                                 reads=[Bacc, Bri], writes=[Bom])
                            dd, Bdd = T["dd"].next()
                            st_["dd"] = (dd, Bdd)
                            k.op("dve", lambda e: e.scalar_tensor_tensor(dd[:, :n], om[:, n:n2], lamt[:, l * 8 + 5:l * 8 + 6],
                                                                         om[:, 0:n], ALU.mult, ALU.add),
                                 reads=[Bom, Blam], writes=[Bdd])
                            sq, Bs = T["sq"].next()
                            st_["sq"] = (sq, Bs)
                            k.op("pool", lambda e: e.tensor_tensor(sq[:, :n], dd[:, :n], dd[:, :n], ALU.mult), reads=[Bdd],
                                 writes=[Bs])

                        def p1():
                            sq, Bs = st_["sq"]
                            bk, Bk = T["pp"].next()
                            st_["bk"] = (bk, Bk)
                            mm(bk[:, :n], ones32, sq[:, :n], True, True, [Bs, Bcst], Bk)

                        def p2():
                            bk, Bk = st_["bk"]
                            dd, Bdd = st_["dd"]
                            rs, Br = T["rs"].next()
                            rsqrt_from(rs[:, :n], Br, bk[:, :n], Bk, 1.0 / 128, 128)
                            ot, Bot = ocT[hc]
                            k.op("dve", lambda e: e.scalar_tensor_tensor(ot[:, :n], dd[:, :n], lamt[:, l * 8 + 6:l * 8 + 7],
                                                                         rs[:, :n], ALU.mult, ALU.mult),
                                 reads=[Bdd, Br, Blam], writes=[Bot])
                        pend.append([1, p0])
                        pend.append([4, p1])
                        pend.append([7, p2])

                    SK = 2
                    for s_ in range(min(SK, len(flat))):
                        emit_qk(s_)
                    per_step = (len(side) + max(1, len(flat) - 6) - 1) // max(1, len(flat) - 6)
                    cur = None
                    for s_, (ui, pi) in enumerate(flat):
                        if s_ + SK < len(flat):
                            emit_qk(s_ + SK)
                        kind, a_ = units[ui]
                        stb, Bst = stq.pop(s_)
                        pT, BpT = T["pT"].next()
                        k.op("act", lambda e: e.activation(pT[:, 0:n2], stb[:, 0:n2], AF.Exp, scale=0.125),
                             reads=[Bst], writes=[BpT])
                        if pi == 0:
                            cur = [T["ac"].next()]
                            if kind == "C":
                                cur.append(T["ac"].next())
                        acc, Bacc = cur[0]
                        kbk = kbl[pi]
                        first = pi == 0
                        lastk = pi == NP - 1
                        if kind == "A":
                            mm(acc[0:65, 0:n2], VA[:, kbk, a_ // 2, :], pT[:, 0:n2], first, lastk, [BVA, BpT], Bacc)
                        else:
                            rsk, Brsk = cur[1]
                            mm(acc[:, 0:n2], VC[:, kbk, a_ * 128:(a_ + 1) * 128], pT[:, 0:n2], first, lastk, [BVC, BpT], Bacc)
                            mm(rsk[:, 0:n2], ones16[:], pT[:, 0:n2], first, lastk, [Bo16, BpT], Brsk)
                        flush()
                        if pi == NP - 1:
                            if kind == "A":
                                epi_A(a_, acc, Bacc)
                            else:
                                epi_C(a_, acc, Bacc, cur[1][0], cur[1][1])
                        for _ in range(per_step):
                            if side:
                                f_ = side.pop(0)
                                if f_ is not None:
                                    f_()
                    flush(True)
                    while side:
                        f_ = side.pop(0)
                        if f_ is not None:
                            f_()
                    T["pp"] = pp_saved
                    staged = qb + 1 < nblk
                    if staged:
                        k.op("pool", lambda e: e.memset(fz[:], 0.0), writes=ALIAS + [Bxs, Bhs, Bfz])
                        load_x(xs, Bxs, qb + 1, "pool")
                    for a, (wp, oT) in enumerate(((w_pa, oaT), (w_pb, obT), (w_pc, ocT))):
                        wpv = wp[l].rearrange("(c p) n -> p c n", p=128)
                        if a == 1 and staged:
                            norm_block(T, xs, Bxs, EXT, lambda c: DV(l, which, 0, c), lambda c: DV(l, which, 1, c), hs, Bhs)
                        for ng in range(4):
                            wg, Bwg = ws.get(wl[:, :, OFF_GT + a * 1024 + ng * 256:OFF_GT + a * 1024 + (ng + 1) * 256], 8, 256,
                                             key=(l, "gt", a, ng))
                            wpt, Bwp = ws.get(wpv[:, :, ng * 256:(ng + 1) * 256], 4, 256, key=(l, "wp", a, ng))
                            for nl in range(2):
                                nn = ng * 2 + nl
                                (bg, Bg), (bp, Bp) = T["pp"].next(), T["pp"].next()
                                for c in range(8):
                                    mm(bg[:, :n], wg[:, c, nl * 128:(nl + 1) * 128], hm[:, c, :], c == 0, c == 7, [Bwg, BhT], Bg)
                                for c in range(4):
                                    mm(bp[:, :n], wpt[:, c, nl * 128:(nl + 1) * 128], oT[c][0][:, :n], c == 0, c == 3,
                                       [Bwp, oT[c][1]], Bp)
                                gs, Bgs = T["tmp"].next()
                                k.op("act", lambda e: e.activation(gs[:, :n], bg[:, :n], AF.Sigmoid,
                                                                   bias=V(l, "b_gate", a * 8 + nn)),
                                     reads=[Bg, Bvec], writes=[Bgs])
                                if a == 0:
                                    k.op("dve", lambda e: e.tensor_tensor(macc[:, nn, :], gs[:, :n], bp[:, :n], ALU.mult),
                                         reads=[Bgs, Bp], writes=[Bmacc])
                                else:
                                    k.op("dve", lambda e: e.tensor_tensor(gs[:, :n], gs[:, :n], bp[:, :n], ALU.mult),
                                         reads=[Bgs, Bp], writes=[Bgs])
                                    if a == 1:
                                        k.op("dve", lambda e: e.tensor_tensor(macc[:, nn, :], macc[:, nn, :], gs[:, :n], ALU.add),
                                             reads=[Bgs, Bmacc], writes=[Bmacc])
                                    else:
                                        k.op("dve", lambda e: e.tensor_tensor(mT[:, nn, :], macc[:, nn, :], gs[:, :n], ALU.add),
                                             reads=[Bgs, Bmacc], writes=[BmT])
                    wov = fm(w_out[l])
                    for ng in range(4):
                        wt, Bw = ws.get(wov[:, :, ng * 256:(ng + 1) * 256], 8, 256, key=(l, "wo", ng))
                        for nl in range(2):
                            nn = ng * 2 + nl
                            bk, Bk = T["pp"].next()
                            for c in range(8):
                                mm(bk[:, :n], wt[:, c, nl * 128:(nl + 1) * 128], mT[:, c, :], c == 0, c == 7, [Bw, BmT], Bk)
                            k.op("dve", lambda e: e.scalar_tensor_tensor(macc[:, nn, :], bk[:, :n], DV(l, which, 2, nn),
                                                                         xe[:, nn, 15:15 + n], ALU.mult, ALU.add),
                                 reads=[Bk, Bdv, Bxe], writes=[Bmacc])
                    k.dma("sp", fm(xm)[:, :, c0:c0 + n], macc[:, :, :], reads=[Bmacc], writes=DB("xm_scr", c0, c0 + n))
                    if staged:
                        k.op("dve", lambda e: e.tensor_copy(xe[:, :, :], xs[:, :, :]), reads=[Bxs], writes=[Bxe])
                        k.op("act", lambda e: e.activation(hT[:, :, :], hs[:, :, :], AF.Copy), reads=[Bhs], writes=[BhT])
                        k.op("pool", lambda e: e.memset(fz[:], 0.0), writes=ALIAS + [Bxs, Bhs, Bfz])
                    if DEBUG and seg is qsegs[0] and qb == 0 and l == layers[0]:
                        o = 0
                        k.dma("sp", dbg16[:, 0:8 * n].rearrange("p (c n) -> p c n", n=n), hT[:, :, 15:15 + n], reads=[BhT]); o += 8 * n
                        for lst in (oaT, obT, ocT, qTA, qTC):
                            for (t_, B_) in lst:
                                k.dma("sp", dbg16[:, o:o + n], t_[:, :n], reads=[B_]); o += n
                        k.dma("sp", dbg16[:, o:o + 8 * n].rearrange("p (c n) -> p c n", n=n), mT[:, :, :], reads=[BmT])
                        k.dma("sp", dbg32[:, 0:8 * EXT].rearrange("p (c n) -> p c n", n=EXT), xe[:, :, :], reads=[Bxe])
                        k.dma("sp", dbg32[:, 8 * EXT:12 * EXT].rearrange("p (c n) -> p c n", n=EXT), yx[:, :, :], reads=[Byx])
                        k.dma("sp", dbg32[:, 12 * EXT:12 * EXT + 4 * n].rearrange("p (c n) -> p c n", n=n), cacc[:, :, :], reads=Bcacc)

        def ffn_phase(ws, l, tsegs, moe, final, dst, dname):
            cv = Carver()
            nslot = 6
            slots = [(cv.take(8192, BF16, "ws%d" % i), Buf("fws%d" % i)) for i in range(nslot)]
            ws.new_phase(slots, 2)
            NT = sum(s["n"] for s in tsegs)
            acc = cv.take(8 * NT * 4, F32, "acc").rearrange("p (c n) -> p c n", n=NT)
            h2T = cv.take(8 * NT * 2, BF16, "h2T").rearrange("p (c n) -> p c n", n=NT)
            nblk256 = NT // 256
            Bacc = [Buf("acc%d" % i) for i in range(nblk256)]
            Bh2 = [Buf("h2_%d" % i) for i in range(nblk256)]
            T = {}
            T["sq"] = Ring([(cv.take(512 * 4, F32, "sq"), Buf("fsq%d" % i)) for i in range(2)])
            T["tmp"] = Ring([(cv.take(512 * 4, F32, "tmp"), Buf("ftmp%d" % i)) for i in range(3)])
            T["rs"] = Ring([(cv.take(512 * 4, F32, "rs"), Buf("frs%d" % i)) for i in range(2)])
            hid = Ring([(cv.take(4 * 512 * 2, BF16, "hid").rearrange("p (c n) -> p c n", n=512), Buf("hid%d" % i))
                        for i in range(2)])
            T["pp"] = Ring(banks[0:2])
            pa = Ring(banks[0:2]); pb = Ring(banks[2:4]); py = Ring(banks[4:8])
            if moe:
                h2f = cv.take(8 * 256 * 4, F32, "h2f").rearrange("p (c n) -> p c n", n=256); Bh2f = Buf("h2f")
                gT = cv.take(NT * 4, F32, "gT"); BgT = Buf("gT")
                Ge = cv.take(NT * 4, F32, "Ge"); BGe = Buf("Ge")
                wr = cv.take(8 * 8 * 4, F32, "wr").rearrange("p (c n) -> p c n", n=8); Bwr = Buf("wr")
                sm = cv.take(64 * 4, F32, "sm"); Bsm = Buf("sm")
                k.dma("sp", wr, fm(moe_r[0]), writes=[Bwr])
            blocks = []
            lc = 0
            for s in tsegs:
                for j in range(s["n"] // 256):
                    blocks.append((lc, s, s["c0"] + j * 256, None if s["dcol"] is None else s["dcol"] + j * 256))
                    lc += 256
            xmv = fm(xm)
            T["pp"] = Ring(banks[0:4])

            def pro_stats(bi_):
                lc_, s_, c0_, _ = blocks[bi_]
                k.dma("sp", acc[:, :, lc_:lc_ + 256], xmv[:, :, c0_:c0_ + 256], reads=DB("xm_scr", c0_, c0_ + 256),
                      writes=[Bacc[bi_]])
                return norm_stats(T, acc[:, :, lc_:lc_ + 256], Bacc[bi_], 256)

            rs_cur = pro_stats(0)
            for bi, (lc, s, c0, dcol) in enumerate(blocks):
                which = s["which"]
                rs_nxt = pro_stats(bi + 1) if bi + 1 < len(blocks) else None
                norm_block(T, acc[:, :, lc:lc + 256], Bacc[bi], 256, lambda c: DV(l, which, 3, c), lambda c: DV(l, which, 4, c),
                           h2T[:, :, lc:lc + 256], Bh2[bi], out2=h2f if moe else None, Bout2=Bh2f if moe else None,
                           rs_pre=rs_cur)
                rs_cur = rs_nxt
                if moe:
                    for sbk in range(2):
                        bk, Bk = T["pp"].next()
                        for c in range(8):
                            mm(bk[:, 0:8], h2f[:, c, sbk * 128:(sbk + 1) * 128], wr[:, c, :], c == 0, c == 7, [Bh2f, Bwr], Bk)
                        lg = sm[:, 0:8]; m1 = sm[:, 8:9]; eq = sm[:, 16:24]; l2 = sm[:, 24:32]; m2 = sm[:, 9:10]
                        sel = sm[:, 32:40]; ex = sm[:, 40:48]; nm1 = sm[:, 10:11]; ssum = sm[:, 11:12]; gg = sm[:, 48:56]
                        R, W = [Bsm], [Bsm]
                        k.op("dve", lambda e: e.tensor_tensor(lg, bk[:, 0:8], VGl("rb", 0, 8), ALU.add), reads=[Bk, Bvec], writes=W)
                        k.op("dve", lambda e: e.reduce_max(m1, lg, AX.X), reads=R, writes=W)
                        k.op("dve", lambda e: e.tensor_scalar(eq, lg, m1, None, ALU.is_equal), reads=R, writes=W)
                        k.op("dve", lambda e: e.scalar_tensor_tensor(l2, eq, -1.0e30, lg, ALU.mult, ALU.add), reads=R, writes=W)
                        k.op("dve", lambda e: e.reduce_max(m2, l2, AX.X), reads=R, writes=W)
                        k.op("dve", lambda e: e.tensor_scalar(sel, lg, m2, None, ALU.is_ge), reads=R, writes=W)
                        k.op("dve", lambda e: e.tensor_scalar(nm1, m1, -1.0, None, ALU.mult), reads=R, writes=W)
                        k.op("act", lambda e: e.activation(ex, lg, AF.Exp, bias=nm1), reads=R, writes=W)
                        k.op("dve", lambda e: e.tensor_tensor(gg, sel, ex, ALU.mult), reads=R, writes=W)
                        k.op("dve", lambda e: e.reduce_sum(ssum, gg, AX.X), reads=R, writes=W)
                        k.op("dve", lambda e: e.reciprocal(ssum, ssum), reads=R, writes=W)
                        k.op("dve", lambda e: e.tensor_scalar(gg, gg, ssum, None, ALU.mult), reads=R, writes=W)
                        bt, Bbt = T["pp"].next()
                        k.op("pe", lambda e: e.transpose(bt[0:8, 0:128], gg, ident), reads=[Bsm, Bcst], writes=[Bbt])
                        cc = lc + sbk * 128
                        k.op("act", lambda e: e.activation(gT[0:8, cc:cc + 128], bt[0:8, 0:128], AF.Copy), reads=[Bbt], writes=[BgT])
            tbl = []
            lc = 0
            for s in tsegs:
                o = 0
                while o < s["n"]:
                    w = min(512, s["n"] - o)
                    tbl.append((lc + o, w, s["which"]))
                    o += w
                lc += s["n"]
            nexp = N_EXP if moe else 1
            dff = D_FFE if moe else D_FF
            work = []
            for ex_i in range(nexp):
                f0 = 0
                while f0 < dff:
                    fw = min(512, dff - f0)
                    for ti, (lc, w, which) in enumerate(tbl):
                        work.append((ex_i, f0, fw, ti, lc, w, which))
                    f0 += fw
            wstate = {}

            def stage_a(item):
                ex_i, f0, fw, ti, lc, w, which = item
                nfc = fw // 128
                if ti == 0:
                    if moe:
                        if f0 == 0:
                            for (lc_, w_, which_) in tbl:
                                bk, Bk = py.next()
                                mm(bk[:, :w_], cst[0:8, C_OH + ex_i * 128:C_OH + (ex_i + 1) * 128], gT[0:8, lc_:lc_ + w_], True, True,
                                   [BgT, Bcst], Bk)
                                k.op("act", lambda e: e.activation(Ge[:, lc_:lc_ + w_], bk[:, :w_], AF.Copy), reads=[Bk],
                                     writes=[BGe])
                        w1v, w3v, w2v = fm(moe_w1[0, ex_i]), fm(moe_w3[0, ex_i]), fm(moe_w2[0, ex_i])
                    else:
                        w1v, w3v, w2v = fm(ffn_w1[0]), fm(ffn_w3[0]), fm(ffn_w2[0])
                    w1t, Bw1 = ws.get(w1v[:, :, f0:f0 + fw], 8, fw)
                    w3t, Bw3 = ws.get(w3v[:, :, f0:f0 + fw], 8, fw)
                    w2t, Bw2 = ws.get(w2v[:, f0 // 128:f0 // 128 + nfc, :], nfc, 1024)
                    wstate["w"] = (w1t, Bw1, w3t, Bw3, w2t, Bw2)
                w1t, Bw1, w3t, Bw3, w2t, Bw2 = wstate["w"]
                bis = list(range(lc // 256, (lc + w) // 256))
                hd, Bhd = hid.next()
                for fc in range(nfc):
                    (ba, Ba), (bb, Bb) = pa.next(), pb.next()
                    for c in range(8):
                        mm(ba[:, :w], w1t[:, c, fc * 128:(fc + 1) * 128], h2T[:, c, lc:lc + w], c == 0, c == 7,
                           [Bw1] + [Bh2[i] for i in bis], Ba)
                    for c in range(8):
                        mm(bb[:, :w], w3t[:, c, fc * 128:(fc + 1) * 128], h2T[:, c, lc:lc + w], c == 0, c == 7,
                           [Bw3] + [Bh2[i] for i in bis], Bb)
                    sa, Bsa = T["tmp"].next()
                    k.op("act", lambda e: e.activation(sa[:, :w], ba[:, :w], AF.Silu), reads=[Ba], writes=[Bsa])
                    if moe:
                        k.op("dve", lambda e: e.tensor_tensor(sa[:, :w], sa[:, :w], bb[:, :w], ALU.mult),
                             reads=[Bsa, Bb], writes=[Bsa])
                        k.op("pool", lambda e: e.tensor_tensor(hd[:, fc, :w], sa[:, :w], Ge[:, lc:lc + w], ALU.mult),
                             reads=[Bsa, BGe], writes=[Bhd])
                    else:
                        k.op("dve", lambda e: e.tensor_tensor(hd[:, fc, :w], sa[:, :w], bb[:, :w], ALU.mult),
                             reads=[Bsa, Bb], writes=[Bhd])
                return (hd, Bhd, w2t, Bw2, nfc, lc, w, which, bis)

            def stage_b(ctx_):
                hd, Bhd, w2t, Bw2, nfc, lc, w, which, bis = ctx_
                for nn in range(8):
                    by, By = py.next()
                    for fc in range(nfc):
                        mm(by[:, :w], w2t[:, fc, nn * 128:(nn + 1) * 128], hd[:, fc, :w], fc == 0, fc == nfc - 1,
                           [Bw2, Bhd], By)
                    k.op("dve", lambda e: e.scalar_tensor_tensor(acc[:, nn, lc:lc + w], by[:, :w], DV(l, which, 5, nn),
                                                                 acc[:, nn, lc:lc + w], ALU.mult, ALU.add),
                         reads=[By, Bdv] + [Bacc[i] for i in bis], writes=[Bacc[i] for i in bis])

            prev_ = None
            for item in work:
                cur_ = stage_a(item)
                if prev_ is not None:
                    stage_b(prev_)
                prev_ = cur_
            stage_b(prev_)
            if final:
                k.barrier()
                Bstg = Buf("stg")
            for bi, (lc, s, c0, dcol) in enumerate(blocks):
                if dcol is None:
                    continue
                if final:
                    stg = h2f
                    norm_block(T, acc[:, :, lc:lc + 256], Bacc[bi], 256, lambda c: VGl("fing", c), None, stg, Bstg)
                    k.dma("sp", fm(dst)[:, :, dcol:dcol + 256], stg, reads=[Bstg], writes=DB(dname, dcol, dcol + 256))
                else:
                    k.dma("sp", fm(dst)[:, :, dcol:dcol + 256], acc[:, :, lc:lc + 256], reads=[Bacc[bi]],
                          writes=DB(dname, dcol, dcol + 256))

        all_keys = list(range(NKB // 2))
        ctx_keys = [SEQ // 256]
        seg_own = dict(c0=0, n=HALF, which=0, hl_col=SEQ - 15, hl_mask=0, hr_col=HALF, hr_mask=1, keys=all_keys)
        seg_oth = dict(c0=HALF, n=HALF, which=0, hl_col=HALF - 15, hl_mask=2, hr_col=0, hr_mask=3, keys=all_keys)
        seg_ctx = dict(c0=SEQ, n=CTX, which=1, hl_col=SEQ, hl_mask=4, hr_col=SEQ, hr_mask=4, keys=ctx_keys)

        def whole(ws):
            k.op("act", lambda e: e.activation(csb[:], VGl("cfm", 0, 16), AF.Silu), reads=[Bvec], writes=[Bcsb])
            cvm = Carver()
            ws.new_phase([(cvm.take(8192, BF16, "mws%d" % i), Buf("mws%d" % i)) for i in range(3)], 2)
            for l in layers:
                mod_phase(ws, l)
            for li, l in enumerate(layers):
                k.barrier()
                src, sname = (xT, "xT") if li == 0 else (xb, "xb_scr")
                lastl = l == DEPTH - 1
                if lastl:
                    qsegs = [seg_own]
                elif fused and len(layers) > 1:
                    qsegs = [seg_own, seg_oth, seg_ctx]
                else:
                    qsegs = [seg_own, seg_ctx]
                mixer_phase(ws, l, src, sname, qsegs)
                if lastl:
                    sbs = [[dict(c0=0, n=HALF, which=0, dcol=0)]]
                    dst, dname = outT, "outT"
                elif fused and len(layers) > 1:
                    sbs = [[dict(c0=0, n=HALF, which=0, dcol=0)],
                           [dict(c0=HALF, n=HALF, which=0, dcol=HALF), dict(c0=SEQ, n=CTX, which=1, dcol=SEQ)]]
                    dst, dname = xb, "xb_scr"
                else:
                    sbs = [[dict(c0=0, n=HALF, which=0, dcol=0), dict(c0=SEQ, n=CTX, which=1, dcol=HALF)]]
                    dst, dname = outT, "outT"
                for sb_ in sbs:
                    k.barrier()
                    ffn_phase(ws, l, sb_, moe=(l % 2 == 1), final=lastl, dst=dst, dname=dname)
            k.finish()

        ws = WStream(k, pf=2)
        ws.scr = wsc
        k.dry = True
        whole(ws)
        k.dry = False
        ws.rewind()
        whole(ws)
        print("program: nins=%d nwait=%d" % (k.nins, k.nwait))
    return nc


def _fm(v):
    v = np.asarray(v, np.float32)
    return np.ascontiguousarray(v.reshape(-1, 128).T)


def _qk_perm(nunits):
    cols = []
    for g0 in range(0, nunits, 4):
        us = list(range(g0, min(g0 + 4, nunits)))
        for half in range(2):
            for u in us:
                for axis in range(2):
                    for f in range(16):
                        cols.append(u * 64 + axis * 32 + half * 16 + f)
    return np.array(cols)


def _consts():
    c = np.zeros((128, NCONST), np.float32)
    c[:, C_ONES:C_ONES + 128] = 1.0
    for b in range(4):
        c[32 * b:32 * b + 32, C_BD + 32 * b:C_BD + 32 * b + 32] = 1.0
    c[64, C_SEL:C_SEL + 64] = 1.0
    c[:, C_ID:C_ID + 128] = np.eye(128, dtype=np.float32)
    for e in range(8):
        c[e, C_OH + e * 128:C_OH + (e + 1) * 128] = 1.0
    return c


def _tables(tok_pos):
    tok_pos = np.asarray(tok_pos)
    n_freq = 16
    inv = (10000.0 ** (-np.arange(n_freq, dtype=np.float32) / n_freq)).astype(np.float32)
    row = (tok_pos // 64).astype(np.float32)
    col = (tok_pos % 64).astype(np.float32)
    cos = np.ones((128, len(tok_pos)), np.float32)
    sin = np.zeros((128, len(tok_pos)), np.float32)
    valid = tok_pos >= 0
    for p in range(128):
        axis = (p % 32) // 16
        f = p % 16
        pos = row if axis == 0 else col
        ang = (pos * inv[f]).astype(np.float32)
        cos[p, valid] = np.cos(ang[valid])
        sin[p, valid] = np.sin(ang[valid])
    return cos, sin


def _prep_shared(inp):
    sh = {}
    w_in = np.asarray(inp["w_in"], np.float32)
    perm = np.arange(6400)
    perm[OFF_AQ:OFF_AQ + 512] = OFF_AQ + _qk_perm(8)
    perm[OFF_AK:OFF_AK + 128] = OFF_AK + _qk_perm(2)
    perm[OFF_CQ:OFF_CQ + 512] = OFF_CQ + _qk_perm(8)
    perm[OFF_CK:OFF_CK + 512] = OFF_CK + _qk_perm(8)
    sh["w_in"] = np.ascontiguousarray(w_in[:, :, perm])
    for nme in ("w_mod", "w_pa", "w_pb", "w_pc", "w_out", "ffn_w1", "ffn_w3", "ffn_w2", "moe_router", "moe_w1", "moe_w3",
                "moe_w2"):
        sh[nme] = np.ascontiguousarray(np.asarray(inp[nme], np.float32))
    sh["consts"] = _consts()
    return sh


def _vecs(inp, b, hf):
    v = np.zeros((128, NV), np.float32)
    p = np.arange(128)
    gidx = ((p % 32) // 16) * 32 + (p % 16)
    for l in range(DEPTH):
        o = l * VLN

        def put(name, arr):
            a, n = VL[name]
            v[:, o + a:o + a + n] = arr.reshape(128, n)
        put("b_mod", _fm(inp["b_mod"][l]))
        put("n1g", _fm(inp["norm1_g"][l]))
        put("n2g", _fm(inp["norm2_g"][l]))
        put("b_gate", _fm(inp["b_gate"][l]))
        qg = np.asarray(inp["a_qn_g"][l], np.float32)
        kg = np.asarray(inp["a_kn_g"][l], np.float32)
        put("qng", np.stack([qg[gidx], qg[gidx + 16]], axis=1))
        put("kng", np.stack([kg[gidx], kg[gidx + 16]], axis=1))
        dw = np.asarray(inp["b_dw_w"][l], np.float32)
        put("dww", np.ascontiguousarray(dw.T.reshape(4, 128, 31).transpose(1, 0, 2)))
        put("dwb", _fm(inp["b_dw_b"][l]))
        put("lng", _fm(inp["b_ln_g"][l]))
        put("lnb", _fm(inp["b_ln_b"][l]))
        put("subg", _fm(inp["c_subln_g"][l]))
        lv = np.concatenate([np.asarray(inp[n_][l], np.float32) for n_ in ("c_lq1", "c_lk1", "c_lq2", "c_lk2")])
        put("lamv", np.broadcast_to(lv[None, :], (128, 256)))
    a, n = VG["hmask"]
    hm = np.array([0, 1, 1, 0, 0] if hf == 0 else [1, 0, 0, 1, 0], np.float32)
    v[:, a:a + n] = hm[None, :]
    a, n = VG["fing"]
    v[:, a:a + n] = _fm(inp["final_g"])
    a, n = VG["cfm"]
    cf = np.stack([_fm(inp["c"][b]), _fm(inp["c_ctx"])], axis=2)
    v[:, a:a + n] = cf.reshape(128, 16)
    a, n = VG["rb"]
    v[:, a:a + n] = np.broadcast_to(np.asarray(inp["moe_router_b"][0], np.float32)[None, :], (128, 8))
    return v


def _core_order(hf):
    own = np.arange(hf * HALF, (hf + 1) * HALF)
    oth = np.arange((1 - hf) * HALF, (2 - hf) * HALF)
    return own, oth


_PROG_CACHE = {}


def _get_prog(layers, fused):
    key = (tuple(layers), fused)
    if key not in _PROG_CACHE:
        _PROG_CACHE[key] = build_program(list(layers), fused)
    return _PROG_CACHE[key]


def _run(layers, fused, inp, sh, xT_cores):
    in_maps = []
    for core in range(8):
        b, hf = core // 2, core % 2
        own, oth = _core_order(hf)
        pos = np.concatenate([own, oth, -np.ones(CTX, np.int64)])
        cos, sin = _tables(pos)
        m = dict(xT=xT_cores[core], cosk=cos, sink=sin, vecs=_vecs(inp, b, hf), consts=sh["consts"])
        for nme in ("w_mod", "w_in", "w_pa", "w_pb", "w_pc", "w_out"):
            m[nme] = sh[nme]
        if 0 in layers:
            for nme in ("ffn_w1", "ffn_w3", "ffn_w2"):
                m[nme] = sh[nme]
        if 1 in layers:
            for nme in ("moe_router", "moe_w1", "moe_w3", "moe_w2"):
                m[nme] = sh[nme]
        in_maps.append(m)
    nc = _get_prog(layers, fused)
    res = run_bass_kernel_spmd(nc, in_maps, core_ids=list(range(8)))
    return res.results


def kernel(**inp):
    x = np.asarray(inp["x"], np.float32)
    ctx = np.asarray(inp["ctx"], np.float32)
    sh = _prep_shared(inp)
    xT_cores = []
    for core in range(8):
        b, hf = core // 2, core % 2
        own, oth = _core_order(hf)
        xt = np.concatenate([x[b][own], x[b][oth], ctx[b]], axis=0)
        xT_cores.append(np.ascontiguousarray(xt.T))
    if FUSED:
        res = _run([0, 1], True, inp, sh, xT_cores)
    else:
        r0 = _run([0], False, inp, sh, xT_cores)
        xT1 = []
        for core in range(8):
            mate = core ^ 1
            xT1.append(np.ascontiguousarray(np.concatenate(
                [r0[core]["x1T"][:, :HALF], r0[mate]["x1T"][:, :HALF], r0[core]["x1T"][:, HALF:]], axis=1)))
        res = _run([1], False, inp, sh, xT1)
    out = np.zeros((4, SEQ, D), np.float32)
    for core in range(8):
        b, hf = core // 2, core % 2
        out[b, hf * HALF:(hf + 1) * HALF, :] = res[core]["outT"].T
    return out
```
